# Optimizing a Trainium2 kernel written in Bass

```python
import jax, jax.numpy as jnp
from jax import lax
import numpy as np

D_MODEL = 1024
BATCH = 2
SEQ = 8192
DEPTH = 2

GRID_W = 64
CTX_LEN = 256
HEAD_DIM = 64
ROPE_THETA = 10000.0
RMS_EPS = 1e-6
GN_EPS = 1e-5
A_HEADS = 8
A_KV_HEADS = 2
Q_BLOCK = 128
POOL_WINDOWS = (2, 4, 8, 16)
B_GROUP_WIDTH = 128
B_WIDTH = 512
C_HEADS = 4
C_QK_DIM = 64
C_V_DIM = 128
RET_CHUNK = 128
D_HEADS = 8
D_KV_HEADS = 2
WINDOW = 128
BAND_BLOCK = 128
N_EXPERTS = 16
EC_CAPACITY_FACTOR = 2
D_EXPERT = 2 * D_MODEL

AB_LAYOUT = (("a_q", A_HEADS * HEAD_DIM), ("a_k", A_KV_HEADS * HEAD_DIM), ("a_v", A_KV_HEADS * HEAD_DIM), ("b_u", B_WIDTH))
CD_LAYOUT = (("c_q", C_HEADS * C_QK_DIM), ("c_k", C_HEADS * C_QK_DIM), ("c_v", C_HEADS * C_V_DIM), ("c_g", C_HEADS * C_V_DIM),
             ("d_q", D_HEADS * HEAD_DIM), ("d_k", D_KV_HEADS * HEAD_DIM), ("d_v", D_KV_HEADS * HEAD_DIM))
AB_IN = 1280
CD_IN = 2304
AB_OUT = A_HEADS * HEAD_DIM + B_WIDTH
CD_OUT = C_HEADS * C_V_DIM + D_HEADS * HEAD_DIM
N_EVEN = (DEPTH + 1) // 2
N_ODD = DEPTH // 2

kernel_name = "hybrid_dit_attn_pool_retention_swa_ecmoe"

F32 = jnp.float32


def rms_norm(x, g):
    xf = x.astype(F32)
    y = xf * lax.rsqrt(jnp.mean(xf * xf, axis=-1, keepdims=True) + RMS_EPS)
    return (y * g.astype(F32)).astype(x.dtype)


def modulate(x, g, shift, scale):
    return rms_norm(x, g) * (1 + scale) + shift


def axial_rope_tables(rows):
    row = jnp.repeat(jnp.arange(rows, dtype=F32), GRID_W)
    col = jnp.tile(jnp.arange(GRID_W, dtype=F32), rows)
    n_freq = HEAD_DIM // 4
    freqs = ROPE_THETA ** (-jnp.arange(n_freq, dtype=F32) / n_freq)
    ang = jnp.concatenate([row[:, None] * freqs, col[:, None] * freqs], axis=-1)
    return jnp.cos(ang), jnp.sin(ang)


def apply_rope(x, cos, sin):
    xf = x.astype(F32)
    half = x.shape[-1] // 2
    x1, x2 = xf[..., :half], xf[..., half:]
    cs, sn = cos[None, :, None, :], sin[None, :, None, :]
    return jnp.concatenate([x1 * cs - x2 * sn, x1 * sn + x2 * cs], axis=-1).astype(x.dtype)


def project(h, w, layout, names):
    offsets, o = {}, 0
    for name, width in layout:
        offsets[name] = (o, o + width)
        o += width
    if tuple(names) == tuple(nm for nm, _ in layout):
        y = h @ w
    else:
        y = h @ jnp.concatenate([w[:, offsets[nm][0]:offsets[nm][1]] for nm in names], axis=1)
    sizes = [offsets[nm][1] - offsets[nm][0] for nm in names]
    cuts = [int(s) for s in np.cumsum(sizes)[:-1]]
    return dict(zip(names, jnp.split(y, cuts, axis=-1)))


def gqa_attend(q, k, v):
    s = jnp.einsum('bqkgd,bskd->bkgqs', q, k, preferred_element_type=F32) * (q.shape[-1] ** -0.5)
    p = jax.nn.softmax(s, axis=-1).astype(v.dtype)
    return jnp.einsum('bkgqs,bskd->bqkgd', p, v)


def softmax_with_sink(s, sink):
    sk = jnp.broadcast_to(sink.astype(F32)[:, :, None, None], s.shape[:-1] + (1,))
    return jax.nn.softmax(jnp.concatenate([s, sk], axis=-1), axis=-1)[..., :-1]


def multiscale_pool(u, group_w, scale):
    B, L, _ = u.shape
    ng = len(POOL_WINDOWS)
    uf = u.astype(F32).reshape(B, L, ng, B_GROUP_WIDTH)
    cs = jnp.concatenate([jnp.zeros((B, 1, ng, B_GROUP_WIDTH), F32), jnp.cumsum(uf, axis=1)], axis=1)
    win = jnp.array(POOL_WINDOWS, dtype=jnp.int32)
    t = jnp.arange(L, dtype=jnp.int32)[:, None]
    lo = jnp.clip(t - win // 2, 0, L)
    hi = jnp.clip(t - win // 2 + win, 0, L)
    g_idx = jnp.arange(ng)[None, :]
    mean = (cs[:, hi, g_idx] - cs[:, lo, g_idx]) / (hi - lo).astype(F32)[None, :, :, None]
    y = jnp.einsum('blgc,gcd->blgd', mean - uf, group_w.astype(F32))
    return (y.reshape(B, L, ng * B_GROUP_WIDTH) * scale.astype(F32)).astype(u.dtype)


def retention_log_decay(e):
    return jnp.log1p(-jnp.exp2(-e.astype(F32)))


def retention_chunkwise(q, k, v, lg, r0, inclusive):
    B, L, H, _ = q.shape
    dv = v.shape[-1]
    T = RET_CHUNK
    N = L // T

    def chunks(a):
        return a.reshape(B, N, T, H, a.shape[-1]).transpose(1, 0, 3, 2, 4)

    pos = jnp.arange(T, dtype=F32)
    diff = pos[:, None] - pos[None, :]
    mask = (diff >= 0) if inclusive else (diff > 0)
    dmat = jnp.where(mask[None], jnp.exp(lg[:, None, None] * jnp.maximum(diff, 0.0)[None]), 0.0)
    xi = jnp.exp(lg[:, None] * (pos + 1.0))[None, :, :, None]
    zeta = jnp.exp(lg[:, None] * (T - 1.0 - pos))[None, :, :, None]
    chunk_decay = jnp.exp(lg * T)[None, :, None, None]

    def step(r, inp):
        qi, ki, vi = inp
        inner = jnp.einsum('bhtd,bhsd->bhts', qi, ki) * dmat
        o = jnp.einsum('bhts,bhsv->bhtv', inner, vi) + jnp.einsum('bhtd,bhdv->bhtv', qi, r) * xi
        r = r * chunk_decay + jnp.einsum('bhsd,bhsv->bhdv', ki * zeta, vi)
        return r, o

    r, o = lax.scan(step, r0, (chunks(q), chunks(k), chunks(v)))
    return o.transpose(1, 0, 3, 2, 4).reshape(B, L, H, dv), r


def retention_final_state(k, v, lg):
    L = k.shape[1]
    w = jnp.exp(lg[None, :] * (L - 1.0 - jnp.arange(L, dtype=F32)[:, None]))
    return jnp.einsum('blhd,blhv->bhdv', k * w[None, :, :, None], v)


def bidir_retention(q, k, v, lg_f, lg_b, r_f0, r_b0):
    o_f, r_f = retention_chunkwise(q, k, v, lg_f, r_f0, True)
    o_b, r_b = retention_chunkwise(q[:, ::-1], k[:, ::-1], v[:, ::-1], lg_b, r_b0, False)
    return o_f + o_b[:, ::-1], r_f, r_b


def retention_output(o, gn_g, gate):
    B, L, H, dv = o.shape
    mu = jnp.mean(o, axis=-1, keepdims=True)
    var = jnp.mean(jnp.square(o - mu), axis=-1, keepdims=True)
    y = ((o - mu) * lax.rsqrt(var + GN_EPS)).reshape(B, L, H * dv) * gn_g.astype(F32)
    return (jax.nn.silu(gate.astype(F32)) * y).astype(gate.dtype)


def window_sink_attention(q, k, v, kc, vc, sink):
    B, n, KV, G, d = q.shape
    WB = BAND_BLOCK
    NB = n // WB
    scale = d ** -0.5
    pad = ((0, 0), (WB, WB), (0, 0), (0, 0))
    kp = jnp.pad(k, pad).reshape(B, NB + 2, WB, KV, d)
    vp = jnp.pad(v, pad).reshape(B, NB + 2, WB, KV, d)
    kb = jnp.concatenate([kp[:, :-2], kp[:, 1:-1], kp[:, 2:]], axis=2)
    vb = jnp.concatenate([vp[:, :-2], vp[:, 1:-1], vp[:, 2:]], axis=2)
    qb = q.reshape(B, NB, WB, KV, G, d)
    s_loc = jnp.einsum('bnqkgd,bnskd->bnkgqs', qb, kb, preferred_element_type=F32) * scale
    blk = jnp.arange(NB)[:, None]
    qpos = blk * WB + jnp.arange(WB)[None, :]
    kpos = blk * WB - WB + jnp.arange(3 * WB)[None, :]
    valid = ((jnp.abs(qpos[:, :, None] - kpos[:, None, :]) <= WINDOW)
             & (kpos[:, None, :] >= 0) & (kpos[:, None, :] < n))
    s_loc = jnp.where(valid[None, :, None, None], s_loc, -jnp.inf)
    s_ctx = jnp.einsum('bnqkgd,bckd->bnkgqc', qb, kc, preferred_element_type=F32) * scale
    p = softmax_with_sink(jnp.concatenate([s_loc, s_ctx], axis=-1), sink).astype(v.dtype)
    p_loc, p_ctx = p[..., :3 * WB], p[..., 3 * WB:]
    o = (jnp.einsum('bnkgqs,bnskd->bnqkgd', p_loc, vb)
         + jnp.einsum('bnkgqc,bckd->bnqkgd', p_ctx, vc))
    return o.reshape(B, n, KV * G * d)


def mixer_attn_pool(h, hc, w_in, w_out, q_g, k_g, pool_w, pool_scale, cos, sin, ctx_out):
    B, n, _ = h.shape
    Lc = hc.shape[1]
    G = A_HEADS // A_KV_HEADS
    all_names = [nm for nm, _ in AB_LAYOUT]
    p = project(h, w_in, AB_LAYOUT, all_names)
    pc = project(hc, w_in, AB_LAYOUT, all_names if ctx_out else ["a_k", "a_v"])
    q = apply_rope(rms_norm(p["a_q"].reshape(B, n, A_HEADS, HEAD_DIM), q_g), cos, sin)
    q = q.reshape(B, n, A_KV_HEADS, G, HEAD_DIM)
    k = apply_rope(rms_norm(p["a_k"].reshape(B, n, A_KV_HEADS, HEAD_DIM), k_g), cos, sin)
    v = p["a_v"].reshape(B, n, A_KV_HEADS, HEAD_DIM)
    kc = rms_norm(pc["a_k"].reshape(B, Lc, A_KV_HEADS, HEAD_DIM), k_g)
    vc = pc["a_v"].reshape(B, Lc, A_KV_HEADS, HEAD_DIM)
    k_all = jnp.concatenate([kc, k], axis=1)
    v_all = jnp.concatenate([vc, v], axis=1)
    qb = q.reshape(B, n // Q_BLOCK, Q_BLOCK, A_KV_HEADS, G, HEAD_DIM).transpose(1, 0, 2, 3, 4, 5)
    ob = lax.map(lambda qi: gqa_attend(qi, k_all, v_all), qb)
    a = ob.transpose(1, 0, 2, 3, 4, 5).reshape(B, n, A_HEADS * HEAD_DIM)
    b = multiscale_pool(p["b_u"], pool_w, pool_scale)
    out = jnp.concatenate([a, b], axis=-1) @ w_out
    if not ctx_out:
        return out, None
    qc = rms_norm(pc["a_q"].reshape(B, Lc, A_KV_HEADS, G, HEAD_DIM), q_g)
    ac = gqa_attend(qc, kc, vc).reshape(B, Lc, A_HEADS * HEAD_DIM)
    bc = multiscale_pool(pc["b_u"], pool_w, pool_scale)
    out_c = jnp.concatenate([ac, bc], axis=-1) @ w_out
    return out, out_c


def mixer_retention_window(h, hc, w_in, w_out, e_f, e_b, gn_g, sink, cos, sin, ctx_out):
    B, n, _ = h.shape
    Lc = hc.shape[1]
    G = D_HEADS // D_KV_HEADS
    all_names = [nm for nm, _ in CD_LAYOUT]
    p = project(h, w_in, CD_LAYOUT, all_names)
    pc = project(hc, w_in, CD_LAYOUT, all_names if ctx_out else ["c_k", "c_v", "d_k", "d_v"])
    lg_f = retention_log_decay(e_f)
    lg_b = retention_log_decay(e_b)
    k_scale = C_QK_DIM ** -0.5
    kc_r = pc["c_k"].reshape(B, Lc, C_HEADS, C_QK_DIM).astype(F32) * k_scale
    vc_r = pc["c_v"].reshape(B, Lc, C_HEADS, C_V_DIM).astype(F32)
    if ctx_out:
        zero = jnp.zeros((B, C_HEADS, C_QK_DIM, C_V_DIM), F32)
        qc_r = pc["c_q"].reshape(B, Lc, C_HEADS, C_QK_DIM).astype(F32)
        oc_r, rf_ctx, rb_ctx = bidir_retention(qc_r, kc_r, vc_r, lg_f, lg_b, zero, zero)
    else:
        rf_ctx = retention_final_state(kc_r, vc_r, lg_f)
        rb_ctx = retention_final_state(kc_r[:, ::-1], vc_r[:, ::-1], lg_b)
    q_r = apply_rope(p["c_q"].reshape(B, n, C_HEADS, C_QK_DIM), cos, sin).astype(F32)
    k_r = apply_rope(p["c_k"].reshape(B, n, C_HEADS, C_QK_DIM), cos, sin).astype(F32) * k_scale
    v_r = p["c_v"].reshape(B, n, C_HEADS, C_V_DIM).astype(F32)
    o_r, _, _ = bidir_retention(q_r, k_r, v_r, lg_f, lg_b, rf_ctx, rb_ctx)
    c_out = retention_output(o_r, gn_g, p["c_g"])
    sink_g = sink.reshape(D_KV_HEADS, G)
    qd = apply_rope(p["d_q"].reshape(B, n, D_HEADS, HEAD_DIM), cos, sin).reshape(B, n, D_KV_HEADS, G, HEAD_DIM)
    kd = apply_rope(p["d_k"].reshape(B, n, D_KV_HEADS, HEAD_DIM), cos, sin)
    vd = p["d_v"].reshape(B, n, D_KV_HEADS, HEAD_DIM)
    kdc = pc["d_k"].reshape(B, Lc, D_KV_HEADS, HEAD_DIM)
    vdc = pc["d_v"].reshape(B, Lc, D_KV_HEADS, HEAD_DIM)
    d_out = window_sink_attention(qd, kd, vd, kdc, vdc, sink_g)
    out = jnp.concatenate([c_out, d_out], axis=-1) @ w_out
    if not ctx_out:
        return out, None
    c_out_c = retention_output(oc_r, gn_g, pc["c_g"])
    qdc = pc["d_q"].reshape(B, Lc, D_KV_HEADS, G, HEAD_DIM)
    s = jnp.einsum('bqkgd,bskd->bkgqs', qdc, kdc, preferred_element_type=F32) * (HEAD_DIM ** -0.5)
    pcx = softmax_with_sink(s, sink_g).astype(vdc.dtype)
    d_out_c = jnp.einsum('bkgqs,bskd->bqkgd', pcx, vdc).reshape(B, Lc, D_HEADS * HEAD_DIM)
    out_c = jnp.concatenate([c_out_c, d_out_c], axis=-1) @ w_out
    return out, out_c


def expert_choice_ffn(h, router_w, w_gate, w_up, w_down):
    B, n, D = h.shape
    cap = EC_CAPACITY_FACTOR * n // N_EXPERTS
    aff = jax.nn.softmax(jnp.einsum('bnd,de->bne', h, router_w, preferred_element_type=F32), axis=-1)
    gate_vals, idx = lax.top_k(aff.transpose(0, 2, 1), cap)
    xs = jax.vmap(lambda hb, ib: hb[ib])(h, idx)
    a = jnp.einsum('becd,edf->becf', xs, w_gate)
    u = jnp.einsum('becd,edf->becf', xs, w_up)
    y = jnp.einsum('becf,efd->becd', jax.nn.silu(a) * u, w_down) * gate_vals[..., None].astype(h.dtype)
    return jax.vmap(lambda ib, yb: jnp.zeros((n, D), h.dtype).at[ib.reshape(-1)].add(yb.reshape(-1, D)))(idx, y)


def setup_inputs(seed: int = 0) -> dict:
    key = jax.random.key(seed)
    ks = jax.random.split(key, 32)
    nrm = jax.random.normal
    D = D_MODEL
    return {
        "x": nrm(ks[0], (BATCH, SEQ, D), F32),
        "c": nrm(ks[1], (BATCH, D), F32),
        "ctx": nrm(ks[2], (BATCH, CTX_LEN, D), F32),
        "c_ctx": nrm(ks[3], (D,), F32),
        "ada_w": nrm(ks[4], (DEPTH, D, 6 * D), F32) * D ** -0.5,
        "ada_b": nrm(ks[5], (DEPTH, 6 * D), F32) * 0.02,
        "norm_mix_g": 1.0 + 0.1 * nrm(ks[6], (DEPTH, D), F32),
        "norm_ffn_g": 1.0 + 0.1 * nrm(ks[7], (DEPTH, D), F32),
        "final_norm_g": 1.0 + 0.1 * nrm(ks[8], (D,), F32),
        "ab_w_in": nrm(ks[9], (N_EVEN, D, AB_IN), F32) * D ** -0.5,
        "ab_w_out": nrm(ks[10], (N_EVEN, AB_OUT, D), F32) * AB_OUT ** -0.5,
        "a_q_norm_g": 1.0 + 0.1 * nrm(ks[11], (N_EVEN, HEAD_DIM), F32),
        "a_k_norm_g": 1.0 + 0.1 * nrm(ks[12], (N_EVEN, HEAD_DIM), F32),
        "b_group_w": nrm(ks[13], (N_EVEN, len(POOL_WINDOWS), B_GROUP_WIDTH, B_GROUP_WIDTH), F32) * B_GROUP_WIDTH ** -0.5,
        "b_scale": 1.0 + 0.1 * nrm(ks[14], (N_EVEN, B_WIDTH), F32),
        "cd_w_in": nrm(ks[15], (N_ODD, D, CD_IN), F32) * D ** -0.5,
        "cd_w_out": nrm(ks[16], (N_ODD, CD_OUT, D), F32) * CD_OUT ** -0.5,
        "c_decay_fwd": 5.0 + jnp.arange(C_HEADS, dtype=F32)[None, :] + 0.1 * nrm(ks[17], (N_ODD, C_HEADS), F32),
        "c_decay_bwd": 5.0 + jnp.arange(C_HEADS, dtype=F32)[None, :] + 0.1 * nrm(ks[18], (N_ODD, C_HEADS), F32),
        "c_norm_g": 1.0 + 0.1 * nrm(ks[19], (N_ODD, C_HEADS * C_V_DIM), F32),
        "d_sink": 0.5 * nrm(ks[20], (N_ODD, D_HEADS), F32),
        "moe_router": nrm(ks[21], (DEPTH, D, N_EXPERTS), F32) * D ** -0.5,
        "moe_w_gate": nrm(ks[22], (DEPTH, N_EXPERTS, D, D_EXPERT), F32) * D ** -0.5,
        "moe_w_up": nrm(ks[23], (DEPTH, N_EXPERTS, D, D_EXPERT), F32) * D ** -0.5,
        "moe_w_down": nrm(ks[24], (DEPTH, N_EXPERTS, D_EXPERT, D), F32) * D_EXPERT ** -0.5,
    }


def reference(x, c, ctx, c_ctx, ada_w, ada_b, norm_mix_g, norm_ffn_g, final_norm_g,
              ab_w_in, ab_w_out, a_q_norm_g, a_k_norm_g, b_group_w, b_scale,
              cd_w_in, cd_w_out, c_decay_fwd, c_decay_bwd, c_norm_g, d_sink,
              moe_router, moe_w_gate, moe_w_up, moe_w_down):
    rows = x.shape[1] // GRID_W
    cos, sin = axial_rope_tables(rows)
    for i in range(DEPTH):
        last = i == DEPTH - 1
        mod = jnp.einsum('bd,de->be', jax.nn.silu(c), ada_w[i]) + ada_b[i]
        mod_c = jax.nn.silu(c_ctx) @ ada_w[i] + ada_b[i]
        sh1, sc1, g1, sh2, sc2, g2 = jnp.split(mod[:, None, :], 6, axis=-1)
        csh1, csc1, cg1, csh2, csc2, cg2 = jnp.split(mod_c[None, None, :], 6, axis=-1)
        h = modulate(x, norm_mix_g[i], sh1, sc1)
        hc = modulate(ctx, norm_mix_g[i], csh1, csc1)
        j = i // 2
        if i % 2 == 0:
            m, m_c = mixer_attn_pool(h, hc, ab_w_in[j], ab_w_out[j], a_q_norm_g[j], a_k_norm_g[j],
                                     b_group_w[j], b_scale[j], cos, sin, not last)
        else:
            m, m_c = mixer_retention_window(h, hc, cd_w_in[j], cd_w_out[j], c_decay_fwd[j], c_decay_bwd[j],
                                            c_norm_g[j], d_sink[j], cos, sin, not last)
        x = x + g1 * m
        x = x + g2 * expert_choice_ffn(modulate(x, norm_ffn_g[i], sh2, sc2),
                                       moe_router[i], moe_w_gate[i], moe_w_up[i], moe_w_down[i])
        if not last:
            ctx = ctx + cg1 * m_c
            ctx = ctx + cg2 * expert_choice_ffn(modulate(ctx, norm_ffn_g[i], csh2, csc2),
                                                moe_router[i], moe_w_gate[i], moe_w_up[i], moe_w_down[i])
    return rms_norm(x, final_norm_g)
```

```python
import contextlib
import numpy as np
import concourse.bass as bass
import concourse.mybir as mybir
from concourse.bass_utils import run_bass_kernel_spmd

F32 = mybir.dt.float32
BF16 = mybir.dt.bfloat16
ALU = mybir.AluOpType
AF = mybir.ActivationFunctionType
AX = mybir.AxisListType

NCORES = 8
D = 1024
SEQ = 8192
LC = 256
NOWN = 2048
NT = 16
EPS = 1e-6


class Buf:
    __slots__ = ("name", "w", "r")

    def __init__(self, name=""):
        self.name = name
        self.w = None
        self.r = []


class Prog:
    ENGS = ("pe", "act", "dve", "pool", "sp")
    NDMA = 12

    def __init__(self, nc):
        self.nc = nc
        self.ops = {e: [] for e in self.ENGS}
        self.cnt = {e: 0 for e in self.ENGS}
        self.seen = {e: {} for e in self.ENGS}
        self.ndma = {e: 0 for e in self.ENGS}
        self.dma_tok = {e: [] for e in self.ENGS}
        self.out_tokens = []

    def _need(self, eng, tok, raw):
        if tok is None:
            return None
        if tok[0] == "E":
            if tok[1] == eng and not raw:
                return None
            key = ("E", tok[1])
            val = tok[2]
        else:
            key = ("D", tok[1], tok[2])
            val = tok[3]
        if self.seen[eng].get(key, 0) >= val:
            return None
        return key, val

    def op(self, eng, fn, reads=(), writes=(), dma=False, is_out=False):
        needs = {}

        def add(tok, raw):
            n = self._need(eng, tok, raw)
            if n is not None:
                k, v = n
                if needs.get(k, 0) < v:
                    needs[k] = v
        for b in reads:
            add(b.w, True)
        for b in writes:
            add(b.w, False)
            for t in b.r:
                add(t, False)
        if dma:
            n = self.ndma[eng]
            slot = n % self.NDMA
            val = 16 * (n // self.NDMA + 1)
            if n >= self.NDMA:
                add(self.dma_tok[eng][n - self.NDMA], True)
            tok = ("D", eng, slot, val)
            self.ndma[eng] += 1
            self.dma_tok[eng].append(tok)
            inc = ("D", eng, slot)
        else:
            self.cnt[eng] += 1
            tok = ("E", eng, self.cnt[eng])
            inc = ("E", eng)
        for k, v in needs.items():
            self.seen[eng][k] = v
        self.ops[eng].append((list(needs.items()), fn, inc))
        for b in reads:
            b.r.append(tok)
        for b in writes:
            b.w = tok
            b.r = []
        if is_out:
            self.out_tokens.append(tok)
        return tok

    def emit(self, es):
        nc = self.nc
        sems = {}
        for e in self.ENGS:
            sems[("E", e)] = es.enter_context(nc.semaphore("s_" + e))
            for s in range(min(self.NDMA, self.ndma[e])):
                sems[("D", e, s)] = es.enter_context(nc.semaphore("d_%s_%d" % (e, s)))
        fin = {}
        for tok in self.out_tokens:
            key = ("D", tok[1], tok[2])
            fin[key] = max(fin.get(key, 0), tok[3])
        block = es.enter_context(nc.Block())
        engobj = {"pe": "tensor", "act": "scalar", "dve": "vector", "pool": "gpsimd", "sp": "sync"}

        def mk(e):
            def body(eng):
                for waits, fn, inc in self.ops[e]:
                    for k, v in waits:
                        eng.wait_ge(sems[k], v)
                    ins = fn(eng)
                    ins.then_inc(sems[inc], 16 if inc[0] == "D" else 1)
                if e == "sp":
                    for k, v in fin.items():
                        eng.wait_ge(sems[k], v)
            return body
        for e in self.ENGS:
            if self.ops[e] or e == "sp":
                getattr(block, engobj[e])(mk(e))


DTSIZE = {F32: 4, BF16: 2}


class Tile:
    def __init__(self, K, name, ap, off=0, size=0):
        self.K = K
        self.name = name
        self.ap = ap
        self.b = Buf(name)
        self.off = off
        self.size = size

    def __getitem__(self, idx):
        a = self.ap[idx]
        self.K.reg[id(a)] = (a, self.b)
        return a

    def v(self, fn):
        a = fn(self.ap)
        self.K.reg[id(a)] = (a, self.b)
        return a


class KB:
    def __init__(self, nc, es, arena_bytes=200 * 1024):
        self.nc = nc
        self.es = es
        self.P = Prog(nc)
        self.reg = {}
        self.arena = es.enter_context(nc.sbuf_tensor("arena", [128, arena_bytes // 4], F32))
        self.arena_bytes = arena_bytes
        self.live = []
        self.dead = []
        self.banks = []
        for i in range(8):
            t = es.enter_context(nc.psum_tensor("bank%d" % i, [128, 512], F32))
            self.banks.append(Tile(self, "bank%d" % i, t[:, :]))
        self.ndram = 0

    def alloc(self, name, shape, dt=F32, hi=False):
        n = 1
        for s in shape[1:]:
            n *= s
        size = (n * DTSIZE[dt] + 31) // 32 * 32
        self.live.sort(key=lambda t: t.off)
        if hi:
            off = self.arena_bytes - size
            for t in reversed(self.live):
                if t.off + t.size <= off:
                    break
                off = min(off, t.off - size)
            assert off >= 0, "SBUF arena overflow (hi): %s" % name
        else:
            off = 0
            for t in self.live:
                if off + size <= t.off:
                    break
                off = max(off, t.off + t.size)
        if off + size > self.arena_bytes:
            print("ARENA:", [(t.name, t.off, t.size) for t in self.live])
        assert off + size <= self.arena_bytes, "SBUF arena overflow: %s %d" % (name, off + size)
        ap = self.arena[0:shape[0], off // 4:(off + size) // 4]
        if dt != F32:
            ap = ap.bitcast(dt)
        ap = ap[:, 0:n]
        if len(shape) == 3:
            ap = ap.rearrange("p (a b) -> p a b", a=shape[1], b=shape[2])
        elif len(shape) == 4:
            ap = ap.rearrange("p (a b c) -> p a b c", a=shape[1], b=shape[2], c=shape[3])
        tl = Tile(self, name, ap, off, size)
        keep = []
        for d in self.dead:
            if d.off < off + size and off < d.off + d.size:
                if d.b.w is not None:
                    tl.b.r.append(d.b.w)
                tl.b.r.extend(d.b.r)
            keep.append(d)
        self.dead = keep
        self.live.append(tl)
        return tl

    def free(self, *tiles):
        for t in tiles:
            self.live.remove(t)
            self.dead.append(t)

    def dram(self, name, shape, dt=F32, kind="ExternalInput"):
        t = self.nc.dram_tensor(name, list(shape), dt, kind=kind)
        return Tile(self, name, t.ap())

    def _infer(self, args, kw):
        out_ap = kw.get("out", args[0] if args else None)
        acc = kw.get("accum_out", None)
        reads, writes = [], []
        for a in list(args) + list(kw.values()):
            ent = self.reg.get(id(a))
            if ent is None:
                continue
            if a is out_ap or a is acc:
                if ent[1] not in writes:
                    writes.append(ent[1])
            else:
                if ent[1] not in reads:
                    reads.append(ent[1])
        return reads, writes

    def op(self, eng, meth, *args, r=(), w=(), **kw):
        reads, writes = self._infer(args, kw)
        reads = reads + [x.b if isinstance(x, Tile) else x for x in r]
        writes = writes + [x.b if isinstance(x, Tile) else x for x in w]
        return self.P.op(eng, lambda e: getattr(e, meth)(*args, **kw), reads, writes)

    def dve(self, meth, *a, **k):
        return self.op("dve", meth, *a, **k)

    def act(self, meth, *a, **k):
        return self.op("act", meth, *a, **k)

    def pool(self, meth, *a, **k):
        return self.op("pool", meth, *a, **k)

    def pe(self, meth, *a, **k):
        return self.op("pe", meth, *a, **k)

    def dma(self, q, out, in_, is_out=False):
        reads, writes = self._infer((out, in_), {})
        return self.P.op(q, lambda e: e.dma_start(out=out, in_=in_), reads, writes, dma=True, is_out=is_out)

    def finish(self):
        self.P.emit(self.es)


def bc_mid(ap, shape):
    return ap.unsqueeze(2).to_broadcast(list(shape))


def bc_heads(ap, shape):
    return ap.unsqueeze(1).to_broadcast(list(shape))


class Ctx:
    pass


def setup_consts(K):
    C = Ctx()
    C.ident = K.alloc("ident", [128, 128], F32)
    K.pool("memset", C.ident[:, :], 1.0)
    K.pool("affine_select", C.ident[:, :], C.ident[:, :], [[-1, 128]], ALU.is_equal, 0.0, base=0, channel_multiplier=1)
    C.ones = K.alloc("ones", [128, 128], F32)
    K.pool("memset", C.ones[:, :], 1.0)
    C.identb = K.alloc("identb", [128, 128], BF16)
    K.dve("tensor_copy", C.identb[:, :], C.ident[:, :])
    return C


def modulation(K, C, d_c2, d_adaw, d_adab, d_adabT, need_vec, need_gate):
    c2 = K.alloc("c2", [128, 8, 2], F32, hi=True)
    K.dma("sp", c2[:, :, :], d_c2[:, :, :])
    sil = K.alloc("sil", [128, 8, 2], F32, hi=True)
    K.act("activation", sil[:, :, :], c2[:, :, :], AF.Silu)
    rep = [K.alloc("rep%d" % i, [128, 8, 128], F32, hi=True) for i in range(2)]
    for i in range(2):
        K.dve("tensor_copy", rep[i][:, :, :], sil.v(lambda a: a[:, :, i:i + 1].to_broadcast([128, 8, 128])))
    brow = K.alloc("adab_row", [1, 6144], F32, hi=True)
    K.dma("sp", brow[:, :], d_adab[:, :])
    bT = K.alloc("adabT", [128, 48], F32, hi=True)
    K.dma("sp", bT[:, :], d_adabT[:, :])
    modT = K.alloc("modT", [128, 48, 2], F32)
    wblk = [K.alloc("adaw%d" % i, [128, 8, 512], F32, hi=True) for i in range(2)]
    gates = {}
    mps = K.banks[7]
    n = 0
    for blk in range(12):
        if blk not in need_vec and blk not in need_gate:
            continue
        wb = wblk[n % 2]
        n += 1
        K.dma("sp", wb[:, :, :], d_adaw.v(lambda a: a.rearrange("(c p) n -> p c n", p=128)[:, :, blk * 512:(blk + 1) * 512]))
        if blk in need_vec:
            for sub in range(4):
                ec = blk * 4 + sub
                for kc in range(8):
                    K.pe("matmul", mps[:, ec * 2:ec * 2 + 2], wb[:, kc, sub * 128:(sub + 1) * 128], sil[:, kc, :],
                         start=(kc == 0), stop=(kc == 7))
            K.dve("tensor_tensor", modT[:, blk * 4:blk * 4 + 4, :],
                  mps.v(lambda a: a[:, blk * 8:blk * 8 + 8].rearrange("p (a b) -> p a b", b=2)),
                  bT.v(lambda a: a[:, blk * 4:blk * 4 + 4].unsqueeze(2).to_broadcast([128, 4, 2])), ALU.add)
        else:
            for i in range(2):
                gp = K.banks[5 + i]
                for kc in range(8):
                    K.pe("matmul", gp[:, :], rep[i][:, kc, :], wb[:, kc, :], start=(kc == 0), stop=False)
                K.pe("matmul", gp[:, :], C.ones[0:1, :], brow[0:1, blk * 512:(blk + 1) * 512], start=False, stop=True)
                key = (blk // 2, i)
                if key not in gates:
                    gates[key] = K.alloc("gate%d_%d" % key, [128, 1024], F32)
                half = blk % 2
                K.act("activation", gates[key][:, half * 512:(half + 1) * 512], gp[:, :], AF.Copy)
    K.free(c2, rep[0], rep[1], brow, bT, wblk[0], wblk[1])
    return modT, gates, sil


def mod_cols(K, modT, d_g, sh_blk, sc_blk, name):
    gT = K.alloc(name + "_gT", [128, 8], F32, hi=True)
    K.dma("sp", gT[:, :], d_g[:, :])
    A = K.alloc(name + "_A", [128, 8, 2], F32)
    K.dve("tensor_scalar", A[:, :, :], modT[:, sc_blk * 4:sc_blk * 4 + 8, :], 1.0, None, ALU.add)
    K.dve("tensor_tensor", A[:, :, :], A[:, :, :], gT.v(lambda a: a[:, :].unsqueeze(2).to_broadcast([128, 8, 2])), ALU.mult)
    SH = K.alloc(name + "_SH", [128, 8, 2], F32)
    K.dve("tensor_copy", SH[:, :, :], modT[:, sh_blk * 4:sh_blk * 4 + 8, :])
    K.free(gT)
    return A, SH


class Front:
    def __init__(self, K, C, out_dt=BF16, nbuf=2, name="fr", share=None):
        self.K, self.C = K, C
        if share is not None:
            self.xn, self.junk, self.ss = share.xn[:nbuf], share.junk, share.ss[:nbuf]
        else:
            self.xn = [K.alloc(name + "_xn%d" % i, [128, 1024], F32) for i in range(nbuf)]
            self.junk = K.alloc(name + "_junk", [128, 1024], F32)
            self.ss = [K.alloc(name + "_ss%d" % i, [128, 4], F32) for i in range(nbuf)]
        self.hT = [K.alloc(name + "_hT%d" % i, [128, 8, 128], out_dt) for i in range(nbuf)]
        self.n = 0
        self.nbuf = nbuf

    def free(self):
        self.K.free(*(self.xn + self.ss + self.hT + [self.junk]))

    def run(self, xt_ap, A, SH, col, banks):
        K, C = self.K, self.C
        i = self.n % self.nbuf
        self.n += 1
        xn, ss, hT = self.xn[i], self.ss[i], self.hT[i]
        K.act("activation", self.junk[:, :], xt_ap, AF.Square, accum_out=ss[:, 0:1])
        K.act("activation", ss[:, 1:2], ss[:, 0:1], AF.Sqrt, bias=EPS, scale=1.0 / D)
        K.dve("reciprocal", ss[:, 2:3], ss[:, 1:2])
        K.act("activation", xn[:, :], xt_ap, AF.Copy, scale=ss[:, 2:3])
        for dc in range(8):
            bk = banks[dc // 4]
            K.pe("transpose", bk[:, (dc % 4) * 128:(dc % 4 + 1) * 128], xn[:, dc * 128:(dc + 1) * 128], C.ident[:, :])
        for dc in range(8):
            bk = banks[dc // 4]
            K.dve("tensor_scalar", hT[:, dc, :], bk[:, (dc % 4) * 128:(dc % 4 + 1) * 128],
                  A[:, dc, col:col + 1], SH[:, dc, col:col + 1], ALU.mult, ALU.add)
        return hT


def head_norm_rope(K, src_ap, H, G, cos_ap, sin_ap, out, tmp):
    n = H * 64
    if G is not None:
        sq, ssh, qn = tmp["sq"], tmp["ssh"], tmp["qn"]
        K.act("activation", sq[:, 0:n], src_ap, AF.Square)
        K.dve("tensor_reduce", ssh[:, 0:H], sq.v(lambda a: a[:, 0:n].rearrange("p (h d) -> p h d", d=64)), AX.X, ALU.add)
        K.act("activation", ssh[:, 8:8 + H], ssh[:, 0:H], AF.Sqrt, bias=EPS, scale=1.0 / 64)
        K.dve("reciprocal", ssh[:, 16:16 + H], ssh[:, 8:8 + H])
        dst = qn if cos_ap is not None else out
        K.dve("tensor_tensor", dst.v(lambda a: a[:, 0:n].rearrange("p (h d) -> p h d", d=64)),
              _reg_like(K, src_ap, src_ap.rearrange("p (h d) -> p h d", d=64)),
              ssh.v(lambda a: a[:, 16:16 + H].unsqueeze(2).to_broadcast([128, H, 64])), ALU.mult)
        K.pool("tensor_tensor", dst.v(lambda a: a[:, 0:n].rearrange("p (h d) -> p h d", d=64)),
               dst.v(lambda a: a[:, 0:n].rearrange("p (h d) -> p h d", d=64)),
               G.v(lambda a: a[:, :].unsqueeze(1).to_broadcast([128, H, 64])), ALU.mult)
    else:
        qn = tmp["qn"]
        dst = qn if cos_ap is not None else out
        K.act("activation", dst[:, 0:n], src_ap, AF.Copy)
    if cos_ap is None:
        return
    t1, t2 = tmp["t1"], tmp["t2"]

    def v4(t, half):
        return t.v(lambda a: a[:, 0:n].rearrange("p (h t d) -> p h t d", t=2, d=32)[:, :, half, :])

    def v3(t):
        return t.v(lambda a: a[:, 0:H * 32].rearrange("p (h d) -> p h d", d=32))
    cb = _reg_like(K, cos_ap, cos_ap.unsqueeze(1).to_broadcast([128, H, 32]))
    sb = _reg_like(K, sin_ap, sin_ap.unsqueeze(1).to_broadcast([128, H, 32]))
    K.pool("tensor_tensor", v3(t1), v4(qn, 0), cb, ALU.mult)
    K.pool("tensor_tensor", v3(t2), v4(qn, 1), sb, ALU.mult)
    K.dve("tensor_tensor", v4(out, 0), v3(t1), v3(t2), ALU.subtract)
    K.pool("tensor_tensor", v3(t1), v4(qn, 0), sb, ALU.mult)
    K.pool("tensor_tensor", v3(t2), v4(qn, 1), cb, ALU.mult)
    K.dve("tensor_tensor", v4(out, 1), v3(t1), v3(t2), ALU.add)


def _reg_like(K, base_ap, new_ap):
    ent = K.reg.get(id(base_ap))
    assert ent is not None
    K.reg[id(new_ap)] = (new_ap, ent[1])
    return new_ap


def attention(K, C, qT, NQ, QB, key_tiles, kT, Vaug, aT, PT, osb, rden, scale):
    sbanks = [K.banks[0], K.banks[1], K.banks[2]]
    obanks = [K.banks[3], K.banks[4]]
    bcb = K.banks[5]
    steps = []
    for h in range(8):
        for qb in range(NQ // QB):
            for i, kt in enumerate(key_tiles):
                steps.append((h, qb, i, kt))
    nk = len(key_tiles)

    def qk(n):
        h, qb, i, kt = steps[n]
        K.pe("matmul", sbanks[n % 3][:, 0:QB], kT[:, h // 4, kt * 128:(kt + 1) * 128], qT[:, h, qb * QB:(qb + 1) * QB],
             start=True, stop=True)
    LOOK = 2
    for n in range(min(LOOK, len(steps))):
        qk(n)
    for n, (h, qb, i, kt) in enumerate(steps):
        if n + LOOK < len(steps):
            qk(n + LOOK)
        pt = PT[n % 3]
        K.act("activation", pt[:, 0:QB], sbanks[n % 3][:, 0:QB], AF.Exp, scale=scale)
        ob = obanks[(h * (NQ // QB) + qb) % 2]
        K.pe("matmul", ob[0:65, 0:QB], Vaug[:, kt, h // 4, :], pt[:, 0:QB], start=(i == 0), stop=(i == nk - 1))
        if i == nk - 1:
            K.dve("reciprocal", rden[64:65, 0:QB], ob[64:65, 0:QB])
            K.pe("matmul", bcb[0:64, 0:QB], C.ones[64:65, 0:64], rden[64:65, 0:QB], start=True, stop=True)
            K.act("activation", osb[0:64, 0:QB], ob[0:64, 0:QB], AF.Copy)
            K.dve("tensor_tensor", aT[0:64, h, qb * QB:(qb + 1) * QB], osb[0:64, 0:QB], bcb[0:64, 0:QB], ALU.mult)


def softmax16(K, logits_ap, aff_out_ap, tmp):
    K.dve("tensor_reduce", tmp[:, 0:1], logits_ap, AX.X, ALU.max)
    K.dve("tensor_scalar", tmp[:, 1:2], tmp[:, 0:1], -1.0, None, ALU.mult)
    K.act("activation", tmp[:, 8:24], logits_ap, AF.Exp, bias=tmp[:, 1:2], scale=1.0, accum_out=tmp[:, 2:3])
    K.dve("reciprocal", tmp[:, 3:4], tmp[:, 2:3])
    K.dve("tensor_scalar", aff_out_ap, tmp[:, 8:24], tmp[:, 3:4], None, ALU.mult)


def build_stage1():
    nc = bass.Bass("TRN2", target_bir_lowering=False)
    es = contextlib.ExitStack()
    K = KB(nc, es)
    d = {}
    for name, shape in [("xb", [SEQ, D]), ("xo", [NOWN, D]), ("xh", [256, D]), ("ctx", [LC, D]), ("c2", [128, 8, 2]),
                        ("ada_w", [D, 6 * D]), ("ada_b", [1, 6 * D]), ("ada_bT", [128, 48]),
                        ("gmixT", [128, 8]), ("gffnT", [128, 8]), ("w_in", [D, 1280]), ("w_out", [D, D]),
                        ("qg", [128, 64]), ("kg", [128, 64]), ("gw", [4, 128, 128]), ("bsc", [128, 4]),
                        ("cosb", [SEQ, 32]), ("sinb", [SEQ, 32]), ("coso", [NOWN, 32]), ("sino", [NOWN, 32]),
                        ("band", [128, 60, 128]), ("rw", [D, 16])]:
        d[name] = K.dram(name, shape)
    o_x1 = K.dram("x1", [NOWN, D], kind="ExternalOutput")
    o_aff = K.dram("aff", [NOWN, 16], kind="ExternalOutput")
    o_c1 = K.dram("ctx1", [LC, D], kind="ExternalOutput")
    o_affc = K.dram("affc", [LC, 16], kind="ExternalOutput")

    C = setup_consts(K)
    modT, gates, sil = modulation(K, C, d["c2"], d["ada_w"], d["ada_b"], d["ada_bT"],
                                  need_vec=(0, 1, 2, 3, 6, 7, 8, 9), need_gate=(4, 5))
    A1, SH1 = mod_cols(K, modT, d["gmixT"], 0, 2, "m1")
    A2, SH2 = mod_cols(K, modT, d["gffnT"], 6, 8, "m2")
    G1 = [gates[(2, 0)], gates[(2, 1)]]
    K.free(sil)

    Win = K.alloc("Win", [128, 8, 1280], BF16, hi=True)
    K.dma("pool", Win[:, :, :], d["w_in"].v(lambda a: a.rearrange("(c p) n -> p c n", p=128)))
    QG = K.alloc("QG", [128, 64], F32)
    KG = K.alloc("KG", [128, 64], F32)
    K.dma("sp", QG[:, :], d["qg"][:, :])
    K.dma("sp", KG[:, :], d["kg"][:, :])
    band = K.alloc("band", [128, 60, 128], BF16, hi=True)
    K.dma("pool", band[:, :, :], d["band"][:, :, :])
    gw = K.alloc("gw", [128, 4, 128], BF16, hi=True)
    K.dma("pool", gw[:, :, :], d["gw"].v(lambda a: a.rearrange("g p n -> p g n")))
    bsc = K.alloc("bsc", [128, 4], F32)
    K.dma("sp", bsc[:, :], d["bsc"][:, :])
    coso = K.alloc("coso", [128, NT, 32], F32, hi=True)
    sino = K.alloc("sino", [128, NT, 32], F32, hi=True)
    K.dma("sp", coso[:, :, :], d["coso"].v(lambda a: a.rearrange("(j p) n -> p j n", p=128)))
    K.dma("sp", sino[:, :, :], d["sino"].v(lambda a: a.rearrange("(j p) n -> p j n", p=128)))

    qT = K.alloc("qT", [128, 8, NOWN], BF16)
    qTc = K.alloc("qTc", [128, 8, LC], BF16)
    K.pool("memset", qT[64:128, :, :], 0.0)
    K.pool("memset", qTc[64:128, :, :], 0.0)
    utok = K.alloc("utok", [128, 20, 512], BF16, hi=True)
    xt = [K.alloc("xt%d" % i, [128, 1024], F32) for i in range(2)]
    fr = Front(K, C)
    tmp = {"sq": K.alloc("sq", [128, 512], F32, hi=True), "ssh": K.alloc("ssh", [128, 24], F32, hi=True), "qn": K.alloc("qn", [128, 512], F32, hi=True),
           "t1": K.alloc("t1", [128, 256], F32, hi=True), "t2": K.alloc("t2", [128, 256], F32, hi=True)}
    qr = K.alloc("qr", [128, 512], F32, hi=True)
    trb = [K.banks[0], K.banks[1]]
    nx = 0

    passB = [("halo", 0), ("halo", 1)] + [("own", j) for j in range(NT)] + [("ctx", 0), ("ctx", 1)]
    def pB_front(n_, kind, j):
        nonlocal nx
        x_t = xt[nx % 2]
        nx += 1
        src = {"halo": d["xh"], "own": d["xo"], "ctx": d["ctx"]}[kind]
        K.dma("sp", x_t[:, :], src[j * 128:(j + 1) * 128, :])
        hT = fr.run(x_t[:, :], A1, SH1, 1 if kind == "ctx" else 0, trb)
        ub, qb = K.banks[3 + 4 * (n_ % 2)], K.banks[2 + 4 * (n_ % 2)]
        for dc in range(8):
            K.pe("matmul", ub[:, :], hT[:, dc, :], Win[:, dc, 768:1280], start=(dc == 0), stop=(dc == 7))
        if kind != "halo":
            for dc in range(8):
                K.pe("matmul", qb[:, :], hT[:, dc, :], Win[:, dc, 0:512], start=(dc == 0), stop=(dc == 7))

    def pB_back(n_, kind, j):
        ub, qb = K.banks[3 + 4 * (n_ % 2)], K.banks[2 + 4 * (n_ % 2)]
        ui = {"halo": 17 * j, "own": 1 + j, "ctx": 18 + j}[kind]
        K.act("activation", utok[:, ui, :], ub[:, :], AF.Copy)
        if kind == "halo":
            return
        if kind == "own":
            head_norm_rope(K, qb[:, :], 8, QG, coso[:, j, :], sino[:, j, :], qr, tmp)
            dst, off = qT, j * 128
        else:
            head_norm_rope(K, qb[:, :], 8, QG, None, None, qr, tmp)
            dst, off = qTc, j * 128
        for h in range(8):
            bk = K.banks[4 + h // 4]
            K.pe("transpose", bk[0:64, (h % 4) * 128:(h % 4 + 1) * 128], qr[:, h * 64:(h + 1) * 64], C.ident[:, :])
        for half in range(2):
            bk = K.banks[4 + half]
            K.act("activation", dst[0:64, half * 4:half * 4 + 4, off:off + 128],
                  bk.v(lambda a: a[0:64, :].rearrange("p (h t) -> p h t", t=128)), AF.Copy)
    pB_front(0, *passB[0])
    for n_, (kind, j) in enumerate(passB):
        if n_ + 1 < len(passB):
            pB_front(n_ + 1, *passB[n_ + 1])
        pB_back(n_, kind, j)

    bT = K.alloc("bT", [128, 4, NOWN], BF16)
    bTc = K.alloc("bTc", [128, 4, LC], BF16)
    dT = [K.alloc("dT%d" % i, [128, 128], BF16, hi=True) for i in range(2)]
    nd = 0
    jobs = [("own", j) for j in range(NT)] + [("ctx", 0), ("ctx", 1)]
    for kind, j in jobs:
        for g in range(4):
            if kind == "own":
                base = 12 if j == 0 else (24 if j == NT - 1 else 0)
                srcs = [(j + s, base + g * 3 + s) for s in range(3)]
                dst, off = bT, j * 128
            else:
                if j == 0:
                    srcs = [(18, 36 + g * 3 + 1), (19, 36 + g * 3 + 2)]
                else:
                    srcs = [(18, 48 + g * 3 + 0), (19, 48 + g * 3 + 1)]
                dst, off = bTc, j * 128
            pb = K.banks[6]
            for n, (ui, bi) in enumerate(srcs):
                K.pe("matmul", pb[:, 0:128], utok[:, ui, g * 128:(g + 1) * 128], band[:, bi, :],
                     start=(n == 0), stop=(n == len(srcs) - 1))
            dt_ = dT[nd % 2]
            nd += 1
            K.dve("tensor_copy", dt_[:, :], pb[:, 0:128])
            yb = K.banks[7]
            K.pe("matmul", yb[:, 0:128], gw[:, g, :], dt_[:, :], start=True, stop=True)
            K.act("activation", dst[:, g, off:off + 128], yb[:, 0:128], AF.Copy, scale=bsc[:, g:g + 1])
    K.free(utok, band, gw, dT[0], dT[1], qr)

    NKT = 2 + SEQ // 128
    kT = K.alloc("kT", [128, 2, NKT * 128], BF16)
    K.pool("memset", kT[64:128, :, :], 0.0)
    Vaug = K.alloc("Vaug", [128, NKT, 2, 65], BF16)
    K.pool("memset", Vaug.v(lambda a: a[:, :, :, 64:65]), 1.0)
    cosb = K.alloc("cosb", [128, SEQ // 128, 32], F32, hi=True)
    sinb = K.alloc("sinb", [128, SEQ // 128, 32], F32, hi=True)
    K.dma("sp", cosb[:, :, :], d["cosb"].v(lambda a: a.rearrange("(j p) n -> p j n", p=128)))
    K.dma("sp", sinb[:, :, :], d["sinb"].v(lambda a: a.rearrange("(j p) n -> p j n", p=128)))
    kr = K.alloc("kr", [128, 128], F32, hi=True)
    def passA_front(kt):
        nonlocal nx
        x_t = xt[nx % 2]
        nx += 1
        if kt < 2:
            K.dma("sp", x_t[:, :], d["ctx"][kt * 128:(kt + 1) * 128, :])
        else:
            K.dma("sp", x_t[:, :], d["xb"][(kt - 2) * 128:(kt - 1) * 128, :])
        hT = fr.run(x_t[:, :], A1, SH1, 1 if kt < 2 else 0, trb)
        kvb = K.banks[2 + kt % 2]
        for dc in range(8):
            K.pe("matmul", kvb[:, 0:256], hT[:, dc, :], Win[:, dc, 512:768], start=(dc == 0), stop=(dc == 7))

    def passA_back(kt):
        kvb = K.banks[2 + kt % 2]
        if kt < 2:
            head_norm_rope(K, kvb[:, 0:128], 2, KG, None, None, kr, tmp)
        else:
            head_norm_rope(K, kvb[:, 0:128], 2, KG, cosb[:, kt - 2, :], sinb[:, kt - 2, :], kr, tmp)
        K.act("activation", Vaug.v(lambda a: a[:, kt, :, 0:64]),
              kvb.v(lambda a: a[:, 128:256].rearrange("p (h d) -> p h d", d=64)), AF.Copy)
        tb = K.banks[4 + kt % 2]
        for h in range(2):
            K.pe("transpose", tb[0:64, h * 128:(h + 1) * 128], kr[:, h * 64:(h + 1) * 64], C.ident[:, :])
        K.dve("tensor_copy", kT[0:64, :, kt * 128:(kt + 1) * 128],
              tb.v(lambda a: a[0:64, 0:256].rearrange("p (h t) -> p h t", t=128)))
    passA_front(0)
    for kt in range(NKT):
        if kt + 1 < NKT:
            passA_front(kt + 1)
        passA_back(kt)
    K.free(cosb, sinb, kr, Win, coso, sino, *tmp.values())

    aT, aTc = qT, qTc
    PT = [K.alloc("PT%d" % i, [128, 512], BF16) for i in range(3)]
    osb = K.alloc("osb", [64, 512], F32)
    rden = K.alloc("rden", [65, 512], F32)
    attention(K, C, qTc, LC, LC, [0, 1], kT, Vaug, aTc, PT, osb, rden, 0.125)
    attention(K, C, qT, NOWN, 512, list(range(NKT)), kT, Vaug, aT, PT, osb, rden, 0.125)
    K.free(kT, Vaug, PT[0], PT[1], PT[2], osb, rden)

    WoA = K.alloc("WoA", [128, 8, D], BF16)
    K.pool("memset", WoA[64:128, :, :], 0.0)
    WoB = K.alloc("WoB", [128, 4, D], BF16)
    K.dma("pool", WoA[0:64, :, :], d["w_out"].v(lambda a: a[0:512, :].rearrange("(h p) n -> p h n", p=64)))
    K.dma("pool", WoB[:, :, :], d["w_out"].v(lambda a: a[512:1024, :].rearrange("(g p) n -> p g n", p=128)))
    rw = K.alloc("rw", [128, 8, 16], F32)
    K.dma("sp", rw[:, :, :], d["rw"].v(lambda a: a.rearrange("(c p) n -> p c n", p=128)))
    fr32 = Front(K, C, out_dt=F32, name="fr32")
    x1 = [K.alloc("x1_%d" % i, [128, 1024], F32) for i in range(2)]
    tg = K.alloc("tg", [128, 1024], F32)
    affo = K.alloc("affo", [128, NT + 2, 16], F32)
    smt = K.alloc("smt", [128, 24], F32)
    jobs = [("ctx", 0), ("ctx", 1)] + [("own", j) for j in range(NT)]
    tg2 = [tg, K.alloc("tg2", [128, 1024], F32)]

    def op_a(n, kind, j):
        nonlocal nx
        x_t = xt[nx % 2]
        nx += 1
        src, a_, b_, col, outd = (d["xo"], aT, bT, 0, o_x1) if kind == "own" else (d["ctx"], aTc, bTc, 1, o_c1)
        K.dma("sp", x_t[:, :], src[j * 128:(j + 1) * 128, :])
        x1t = x1[n % 2]
        tg_ = tg2[n % 2]
        for half in range(2):
            mb = K.banks[2 + 4 * (n % 2) + half]
            for h in range(8):
                K.pe("matmul", mb[:, :], a_[:, h, j * 128:(j + 1) * 128], WoA[:, h, half * 512:(half + 1) * 512],
                     start=(h == 0), stop=False)
            for g in range(4):
                K.pe("matmul", mb[:, :], b_[:, g, j * 128:(j + 1) * 128], WoB[:, g, half * 512:(half + 1) * 512],
                     start=False, stop=(g == 3))
            K.dve("tensor_tensor", tg_[:, half * 512:(half + 1) * 512], mb[:, :], G1[col][:, half * 512:(half + 1) * 512], ALU.mult)
        K.pool("tensor_tensor", x1t[:, :], tg_[:, :], x_t[:, :], ALU.add)
        K.dma("sp", outd[j * 128:(j + 1) * 128, :], x1t[:, :], is_out=True)

    def op_b(n, kind, j):
        col = 0 if kind == "own" else 1
        x1t = x1[n % 2]
        hT = fr32.run(x1t[:, :], A2, SH2, col, trb)
        lb = K.banks[4 + n % 2]
        for dc in range(8):
            K.pe("matmul", lb[:, 0:16], hT[:, dc, :], rw[:, dc, :], start=(dc == 0), stop=(dc == 7))
        ai = (NT + j) if kind == "ctx" else j
        softmax16(K, lb[:, 0:16], affo[:, ai, :], smt)
    op_a(0, *jobs[0])
    for n, (kind, j) in enumerate(jobs):
        if n + 1 < len(jobs):
            op_a(n + 1, *jobs[n + 1])
        op_b(n, kind, j)
    K.dma("sp", o_aff.v(lambda a: a.rearrange("(j p) e -> p j e", p=128)), affo[:, 0:NT, :], is_out=True)
    K.dma("sp", o_affc.v(lambda a: a.rearrange("(j p) e -> p j e", p=128)), affo[:, NT:NT + 2, :], is_out=True)
    K.finish()
    return nc, es


def rope_tables():
    rows = SEQ // 64
    row = np.repeat(np.arange(rows, dtype=np.float32), 64)
    col = np.tile(np.arange(64, dtype=np.float32), rows)
    freqs = (np.float32(10000.0) ** (-np.arange(16, dtype=np.float32) / np.float32(16))).astype(np.float32)
    ang = np.concatenate([row[:, None] * freqs, col[:, None] * freqs], axis=-1).astype(np.float32)
    return np.cos(ang).astype(np.float32), np.sin(ang).astype(np.float32)


def band_mats(T0, L):
    out = np.zeros((4, 3, 128, 128), np.float32)
    for g, w in enumerate((2, 4, 8, 16)):
        for tl in range(128):
            t = T0 + tl
            lo = min(max(t - w // 2, 0), L)
            hi = min(max(t - w // 2 + w, 0), L)
            for s in range(lo, hi):
                o = (s - T0) // 128 + 1
                out[g, o, (s - T0) % 128, tl] += 1.0 / (hi - lo)
            out[g, 1, tl, tl] -= 1.0
    return out


def fm(v):
    return np.ascontiguousarray(v.reshape(8, 128).T)


def prep_stage1(inp):
    cos, sin = rope_tables()
    mid = band_mats(1280, SEQ)
    first = band_mats(0, SEQ)
    last = band_mats(SEQ - 128, SEQ)
    c0 = band_mats(0, LC)
    c1 = band_mats(128, LC)
    maps = []
    for core in range(NCORES):
        b, r = core // 4, core % 4
        t0 = r * NOWN
        x = inp["x"][b]
        xh = np.zeros((256, D), np.float32)
        if r > 0:
            xh[0:128] = x[t0 - 128:t0]
        if r < 3:
            xh[128:256] = x[t0 + NOWN:t0 + NOWN + 128]
        bands = np.concatenate([mid.reshape(12, 128, 128), (first if r == 0 else mid).reshape(12, 128, 128),
                                (last if r == 3 else mid).reshape(12, 128, 128), c0.reshape(12, 128, 128),
                                c1.reshape(12, 128, 128)], axis=0)
        m = {
            "xb": x, "xo": x[t0:t0 + NOWN], "xh": xh, "ctx": inp["ctx"][b],
            "c2": np.stack([fm(inp["c"][b]), fm(inp["c_ctx"])], axis=-1),
            "ada_w": inp["ada_w"][0], "ada_b": inp["ada_b"][0][None, :],
            "ada_bT": np.ascontiguousarray(inp["ada_b"][0].reshape(48, 128).T),
            "gmixT": fm(inp["norm_mix_g"][0]), "gffnT": fm(inp["norm_ffn_g"][0]),
            "w_in": inp["ab_w_in"][0], "w_out": inp["ab_w_out"][0],
            "qg": np.broadcast_to(inp["a_q_norm_g"][0][None, :], (128, 64)),
            "kg": np.broadcast_to(inp["a_k_norm_g"][0][None, :], (128, 64)),
            "gw": inp["b_group_w"][0], "bsc": np.ascontiguousarray(inp["b_scale"][0].reshape(4, 128).T),
            "cosb": cos, "sinb": sin, "coso": cos[t0:t0 + NOWN], "sino": sin[t0:t0 + NOWN],
            "band": bands.transpose(1, 0, 2), "rw": inp["moe_router"][0],
        }
        maps.append({k: np.ascontiguousarray(v, dtype=np.float32) for k, v in m.items()})
    return maps


GCAP = 96
CCAP = 32
NSLOT = 4 * GCAP + CCAP


def build_moe(has_ctx, final):
    nc = bass.Bass("TRN2", target_bir_lowering=False)
    es = contextlib.ExitStack()
    K = KB(nc, es, arena_bytes=206 * 1024)
    d = {}
    ins = [("x1", [NOWN, D]), ("affb", [SEQ, 16]), ("affo", [NOWN, 16]), ("c2", [128, 8, 2]),
           ("ada_w", [D, 6 * D]), ("ada_b", [1, 6 * D]), ("ada_bT", [128, 48]), ("gffnT", [128, 8]),
           ("wg", [16, D, 2 * D]), ("wu", [16, D, 2 * D]), ("wd", [16, 2 * D, D])]
    if has_ctx:
        ins += [("ctx1", [LC, D]), ("affc", [LC, 16])]
    if final:
        ins += [("fg", [128, D])]
    for name, shape in ins:
        d[name] = K.dram(name, shape)
    o_x2 = K.dram("x2", [NOWN, D], kind="ExternalOutput")
    o_c2 = K.dram("ctx2", [LC, D], kind="ExternalOutput") if has_ctx else None
    NTL = NT + (2 if has_ctx else 0)
    NE2 = 32 if has_ctx else 16

    C = setup_consts(K)
    modT, gates, sil = modulation(K, C, d["c2"], d["ada_w"], d["ada_b"], d["ada_bT"],
                                  need_vec=(6, 7, 8, 9), need_gate=(10, 11))
    A2, SH2 = mod_cols(K, modT, d["gffnT"], 6, 8, "m2")
    G2 = [gates[(5, 0)], gates[(5, 1)]]
    K.free(sil, modT)
    if not has_ctx:
        K.free(G2[1])

    X = [K.alloc("X%d" % j, [128, D], F32) for j in range(NTL)]
    H2 = [K.alloc("H2_%d" % j, [128, D], BF16) for j in range(NTL)]
    junk = K.alloc("junk", [128, D], F32, hi=True)
    ss = K.alloc("ss", [128, NTL, 4], F32)

    def load_tile(j):
        src = d["x1"][j * 128:(j + 1) * 128, :] if j < NT else d["ctx1"][(j - NT) * 128:(j - NT + 1) * 128, :]
        K.dma("sp", X[j][:, :], src)
        K.act("activation", junk[:, :], X[j][:, :], AF.Square, accum_out=ss[:, j, 0:1])
        K.act("activation", ss[:, j, 1:2], ss[:, j, 0:1], AF.Sqrt, bias=EPS, scale=1.0 / D)
        K.dve("reciprocal", ss[:, j, 2:3], ss[:, j, 1:2])
        K.act("activation", H2[j][:, :], X[j][:, :], AF.Copy, scale=ss[:, j, 2:3])

    affb = K.alloc("affb", [128, SEQ // 128, 16], F32, hi=True)
    K.dma("sp", affb[:, :, :], d["affb"].v(lambda a: a.rearrange("(j p) e -> p j e", p=128)))
    affo = K.alloc("affo", [128, NTL, 16], F32)
    K.dma("sp", affo[:, 0:NT, :], d["affo"].v(lambda a: a.rearrange("(j p) e -> p j e", p=128)))
    if has_ctx:
        K.dma("sp", affo[:, NT:NTL, :], d["affc"].v(lambda a: a.rearrange("(j p) e -> p j e", p=128)))
    lo = K.alloc("lo", [128, NE2], F32)
    capt = K.alloc("capt", [128, NE2], F32, hi=True)
    mid = K.alloc("mid", [128, NE2], F32, hi=True)
    cnt = K.alloc("cnt", [128, NE2], F32, hi=True)
    gtmp = K.alloc("gtmp", [128, NE2], F32, hi=True)
    mask = K.alloc("mask", [128, SEQ // 128 + 2, 16], F32, hi=True)
    K.dve("memset", lo[:, :], 0.0)
    K.dve("memset", capt[:, 0:16], float(2 * SEQ // 16))
    if has_ctx:
        K.dve("memset", capt[:, 16:32], float(2 * LC // 16))
    tb = K.banks[0]
    NJ = SEQ // 128
    for it in range(30):
        K.dve("tensor_scalar", mid[:, :], lo[:, :], float(2.0 ** -(it + 1)), None, ALU.add)
        K.dve("tensor_tensor", mask[:, 0:NJ, :], affb[:, :, :],
              mid.v(lambda a: a[:, 0:16].unsqueeze(1).to_broadcast([128, NJ, 16])), ALU.is_ge)
        K.dve("tensor_reduce", cnt[:, 0:16], mask.v(lambda a: a[:, 0:NJ, :].rearrange("p j e -> p e j")), AX.X, ALU.add)
        if has_ctx:
            K.dve("tensor_tensor", mask[:, NJ:NJ + 2, :], affo[:, NT:NTL, :],
                  mid.v(lambda a: a[:, 16:32].unsqueeze(1).to_broadcast([128, 2, 16])), ALU.is_ge)
            K.dve("tensor_reduce", cnt[:, 16:32], mask.v(lambda a: a[:, NJ:NJ + 2, :].rearrange("p j e -> p e j")), AX.X, ALU.add)
        K.pe("matmul", tb[:, 0:NE2], C.ones[:, :], cnt[:, :], start=True, stop=True)
        K.dve("tensor_tensor", gtmp[:, :], tb[:, 0:NE2], capt[:, :], ALU.is_ge)
        K.dve("tensor_tensor", gtmp[:, :], gtmp[:, :], mid[:, :], ALU.mult)
        K.dve("tensor_tensor", lo[:, :], lo[:, :], gtmp[:, :], ALU.max)
        if it < NTL:
            load_tile(it)
    K.free(affb, capt, mid, cnt, gtmp, mask, junk)

    sel = K.alloc("sel", [128, NTL, 16], F32)
    gate = K.alloc("gate", [128, NTL, 16], F32)
    slot = K.alloc("slot", [128, NTL, 16], F32)
    offs = K.alloc("offs", [128, NTL, 16], F32)
    Lm = K.alloc("Lm", [128, 128], F32)
    K.pool("memset", Lm[:, :], 1.0)
    K.pool("affine_select", Lm[:, :], Lm[:, :], [[1, 128]], ALU.is_gt, 0.0, base=0, channel_multiplier=-1)
    iota = K.alloc("iota", [128, GCAP], F32)
    K.pool("iota", iota[:, :], [[1, GCAP]], base=0, channel_multiplier=0, allow_small_or_imprecise_dtypes=True)
    K.dve("tensor_tensor", sel[:, 0:NT, :], affo[:, 0:NT, :],
          lo.v(lambda a: a[:, 0:16].unsqueeze(1).to_broadcast([128, NT, 16])), ALU.is_ge)
    if has_ctx:
        K.dve("tensor_tensor", sel[:, NT:NTL, :], affo[:, NT:NTL, :],
              lo.v(lambda a: a[:, 16:32].unsqueeze(1).to_broadcast([128, 2, 16])), ALU.is_ge)
    K.dve("tensor_tensor", gate[:, :, :], affo[:, :, :], sel[:, :, :], ALU.mult)
    pb, tb2 = K.banks[1], K.banks[2]
    K.pe("matmul", pb[:, 0:NTL * 16], Lm[:, :], sel.v(lambda a: a.rearrange("p j e -> p (j e)")), start=True, stop=True)
    K.pe("matmul", tb2[:, 0:NTL * 16], C.ones[:, :], sel.v(lambda a: a.rearrange("p j e -> p (j e)")), start=True, stop=True)
    K.dve("memset", offs[:, :, :], 0.0)
    for j in range(NTL):
        first = (j % 4 == 0) if j < NT else (j == NT)
        if first:
            continue
        K.dve("tensor_tensor", offs[:, j, :], offs[:, j - 1, :], tb2[:, (j - 1) * 16:j * 16], ALU.add)
    K.dve("tensor_tensor", slot.v(lambda a: a.rearrange("p j e -> p (j e)")), offs.v(lambda a: a.rearrange("p j e -> p (j e)")),
          pb[:, 0:NTL * 16], ALU.add)
    K.dve("tensor_scalar", slot[:, :, :], slot[:, :, :], 1.0, None, ALU.add)
    K.dve("tensor_tensor", slot[:, :, :], slot[:, :, :], sel[:, :, :], ALU.mult)
    K.dve("tensor_scalar", slot[:, :, :], slot[:, :, :], -1.0, None, ALU.add)
    K.free(sel, offs, Lm, affo, lo)

    S = K.alloc("S", [128, NTL, GCAP], BF16)
    Sg = K.alloc("Sg", [128, NTL, GCAP], BF16)
    ST = K.alloc("ST", [GCAP, NTL, 128], BF16)
    xsT = K.alloc("xsT", [128, 8, NSLOT], BF16)
    actT = K.alloc("actT", [128, 16, NSLOT], BF16)
    silt = [K.alloc("silt%d" % i, [128, NSLOT], F32) for i in range(2)]
    ysb = [K.alloc("ysb%d" % i, [GCAP, D], BF16) for i in range(5 if has_ctx else 4)]
    Wg = [K.alloc("Wg%d" % i, [128, 8, 256], BF16) for i in range(2)]
    Wu = [K.alloc("Wu%d" % i, [128, 8, 256], BF16) for i in range(2)]
    Wd = [K.alloc("Wd%d" % i, [128, 16, 256], BF16) for i in range(2)]
    groups = [(g, list(range(4 * g, 4 * g + 4)), g * GCAP, GCAP, 0) for g in range(4)]
    if has_ctx:
        groups.append((4, [NT, NT + 1], 4 * GCAP, CCAP, 1))
    NS = 4 * GCAP + (CCAP if has_ctx else 0)
    nw = 0
    nd = 0
    st_ = {"nw": 0, "nd": 0}

    def build_S(e):
        K.dve("tensor_tensor", S[:, :, :], iota.v(lambda a: a[:, :].unsqueeze(1).to_broadcast([128, NTL, GCAP])),
              slot.v(lambda a: a[:, :, e:e + 1].to_broadcast([128, NTL, GCAP])), ALU.is_equal)
        K.dve("tensor_tensor", Sg[:, :, :], S[:, :, :],
              gate.v(lambda a: a[:, :, e:e + 1].to_broadcast([128, NTL, GCAP])), ALU.mult)

    def gather(e):
        for dc in range(8):
            xb_ = K.banks[dc % 2]
            for (g, tiles, c0, ncol, col) in groups:
                for n, j in enumerate(tiles):
                    K.pe("matmul", xb_[:, c0:c0 + ncol], H2[j][:, dc * 128:(dc + 1) * 128], S[:, j, 0:ncol],
                         start=(n == 0), stop=(n == len(tiles) - 1))
            K.dve("tensor_scalar", xsT[:, dc, 0:4 * GCAP], xb_[:, 0:4 * GCAP], A2[:, dc, 0:1], SH2[:, dc, 0:1], ALU.mult, ALU.add)
            if has_ctx:
                K.dve("tensor_scalar", xsT[:, dc, 4 * GCAP:NS], xb_[:, 4 * GCAP:NS], A2[:, dc, 1:2], SH2[:, dc, 1:2], ALU.mult, ALU.add)

    def st_transposes(e):
        stb_t = K.banks[6]
        for j0 in range(0, NTL, 8):
            js = list(range(j0, min(j0 + 8, NTL)))
            for n, j in enumerate(js):
                o_ap = stb_t.v(lambda a: a.bitcast(BF16)[0:GCAP, n * 128:(n + 1) * 128])
                K.pe("transpose", o_ap, Sg[:, j, :], C.identb[:, :])
            K.act("activation", ST[:, j0:j0 + len(js), :],
                  stb_t.v(lambda a: a.bitcast(BF16)[0:GCAP, 0:len(js) * 128].rearrange("p (j t) -> p j t", t=128)), AF.Copy)

    def gate_up(e, hook=None):
        for q in range(8):
            wg_, wu_ = Wg[st_["nw"] % 2], Wu[st_["nw"] % 2]
            st_["nw"] += 1
            K.dma("pool", wg_[:, :, :], d["wg"].v(lambda a: a[e].rearrange("(c p) n -> p c n", p=128)[:, :, q * 256:(q + 1) * 256]))
            K.dma("pool", wu_[:, :, :], d["wu"].v(lambda a: a[e].rearrange("(c p) n -> p c n", p=128)[:, :, q * 256:(q + 1) * 256]))
            for f2 in range(2):
                fc = q * 2 + f2
                ab, ub = K.banks[2 + fc % 2], K.banks[4 + fc % 2]
                for dc in range(8):
                    K.pe("matmul", ab[:, 0:NS], wg_[:, dc, f2 * 128:(f2 + 1) * 128], xsT[:, dc, 0:NS], start=(dc == 0), stop=(dc == 7))
                for dc in range(8):
                    K.pe("matmul", ub[:, 0:NS], wu_[:, dc, f2 * 128:(f2 + 1) * 128], xsT[:, dc, 0:NS], start=(dc == 0), stop=(dc == 7))
                sl_ = silt[fc % 2]
                K.act("activation", sl_[:, 0:NS], ab[:, 0:NS], AF.Silu)
                K.dve("tensor_tensor", actT[:, fc, 0:NS], sl_[:, 0:NS], ub[:, 0:NS], ALU.mult)
            if q == 0 and hook is not None:
                hook()

    def down(e):
        for dq in range(4):
            wd_ = Wd[st_["nd"] % 2]
            st_["nd"] += 1
            K.dma("pool", wd_[:, :, :], d["wd"].v(lambda a: a[e].rearrange("(c p) n -> p c n", p=128)[:, :, dq * 256:(dq + 1) * 256]))
            for (g, tiles, c0, ncol, col) in groups:
                yb = K.banks[6 + g % 2]
                for fc in range(16):
                    K.pe("matmul", yb[0:ncol, 0:256], actT[:, fc, c0:c0 + ncol], wd_[:, fc, :], start=(fc == 0), stop=(fc == 15))
                K.dve("tensor_tensor", ysb[g][0:ncol, dq * 256:(dq + 1) * 256], yb[0:ncol, 0:256],
                      G2[col][0:ncol, dq * 256:(dq + 1) * 256], ALU.mult)

    def scatter(e):
        for (g, tiles, c0, ncol, col) in groups:
            for j in tiles:
                for half in range(2):
                    ob = K.banks[2 + (j % 2) * 2 + half]
                    K.pe("matmul", ob[:, :], ST[0:ncol, j, :], ysb[g][0:ncol, half * 512:(half + 1) * 512], start=True, stop=True)
                    K.dve("tensor_tensor", X[j][:, half * 512:(half + 1) * 512], X[j][:, half * 512:(half + 1) * 512], ob[:, :], ALU.add)

    build_S(0)
    gather(0)
    st_transposes(0)
    for e in range(16):
        gate_up(e, hook=(lambda e=e: build_S(e + 1)) if e + 1 < 16 else None)
        if e + 1 < 16:
            gather(e + 1)
        down(e)
        scatter(e)
        if e + 1 < 16:
            st_transposes(e + 1)

    if final:
        fg = K.alloc("fg", [128, D], F32, hi=True)
        K.dma("sp", fg[:, :], d["fg"][:, :])
        junk = K.alloc("junk2", [128, D], F32, hi=True)
    for j in range(NTL):
        dst = o_x2[j * 128:(j + 1) * 128, :] if j < NT else o_c2[(j - NT) * 128:(j - NT + 1) * 128, :]
        if final:
            K.act("activation", junk[:, :], X[j][:, :], AF.Square, accum_out=ss[:, j, 0:1])
            K.act("activation", ss[:, j, 1:2], ss[:, j, 0:1], AF.Sqrt, bias=EPS, scale=1.0 / D)
            K.dve("reciprocal", ss[:, j, 2:3], ss[:, j, 1:2])
            K.act("activation", X[j][:, :], X[j][:, :], AF.Copy, scale=ss[:, j, 2:3])
            K.dve("tensor_tensor", X[j][:, :], X[j][:, :], fg[:, :], ALU.mult)
        K.dma("sp", dst, X[j][:, :], is_out=True)
    K.finish()
    return nc, es


def prep_moe(inp, layer, x1, aff, ctx1=None, affc=None, final=False):
    maps = []
    for core in range(NCORES):
        b, r = core // 4, core % 4
        t0 = r * NOWN
        m = {
            "x1": x1[b, t0:t0 + NOWN], "affb": aff[b], "affo": aff[b, t0:t0 + NOWN],
            "c2": np.stack([fm(inp["c"][b]), fm(inp["c_ctx"])], axis=-1),
            "ada_w": inp["ada_w"][layer], "ada_b": inp["ada_b"][layer][None, :],
            "ada_bT": np.ascontiguousarray(inp["ada_b"][layer].reshape(48, 128).T),
            "gffnT": fm(inp["norm_ffn_g"][layer]),
            "wg": inp["moe_w_gate"][layer], "wu": inp["moe_w_up"][layer], "wd": inp["moe_w_down"][layer],
        }
        if ctx1 is not None:
            m["ctx1"] = ctx1[b]
            m["affc"] = affc[b]
        if final:
            m["fg"] = np.broadcast_to(inp["final_norm_g"][None, :], (128, D))
        maps.append({k: np.ascontiguousarray(v, dtype=np.float32) for k, v in m.items()})
    return maps


LN2 = 0.6931471805599453
STAGE3_DEBUG = False
NKB = 2 + SEQ // 128


def build_stage3():
    nc = bass.Bass("TRN2", target_bir_lowering=False)
    es = contextlib.ExitStack()
    K = KB(nc, es, arena_bytes=206 * 1024)
    d = {}
    for name, shape in [("xb", [SEQ, D]), ("xo", [NOWN, D]), ("xh", [256, D]), ("ctx", [LC, D]), ("c2", [128, 8, 2]),
                        ("ada_w", [D, 6 * D]), ("ada_b", [1, 6 * D]), ("ada_bT", [128, 48]),
                        ("gmixT", [128, 8]), ("gffnT", [128, 8]), ("w_in", [D, 2304]), ("w_out", [D, D]),
                        ("dec", [128, 8]), ("gng", [128, 512]), ("sink", [128, 8]),
                        ("cosb", [SEQ, 32]), ("sinb", [SEQ, 32]), ("coso", [NOWN, 32]), ("sino", [NOWN, 32]),
                        ("cosh", [256, 32]), ("sinh", [256, 32]),
                        ("EF", [128, NKB]), ("EB", [128, NKB]), ("MF", [128, NKB]), ("MB", [128, NKB]),
                        ("wm", [128, 3, 384]), ("rw", [D, 16])]:
        d[name] = K.dram(name, shape)
    o_x3 = K.dram("x3", [NOWN, D], kind="ExternalOutput")
    o_aff = K.dram("aff", [NOWN, 16], kind="ExternalOutput")
    DBG = STAGE3_DEBUG
    if DBG:
        o_cat = K.dram("dbg_cat", [NOWN, D], kind="ExternalOutput")
        o_rf = K.dram("dbg_rf", [64, 1024], kind="ExternalOutput")
        o_z = K.dram("dbg_z", [128, 2 * NKB * 4], kind="ExternalOutput")

    C = setup_consts(K)
    modT, gates, sil = modulation(K, C, d["c2"], d["ada_w"], d["ada_b"], d["ada_bT"],
                                  need_vec=(0, 1, 2, 3, 6, 7, 8, 9), need_gate=(4, 5))
    A1, SH1 = mod_cols(K, modT, d["gmixT"], 0, 2, "m1")
    A2, SH2 = mod_cols(K, modT, d["gffnT"], 6, 8, "m2")
    G1 = gates[(2, 0)]
    K.free(sil, modT, gates[(2, 1)])

    dec = K.alloc("dec", [128, 8], F32)
    K.dma("sp", dec[:, :], d["dec"][:, :])
    lgt = K.alloc("lgt", [128, 8], F32)
    K.act("activation", dec[:, :], dec[:, :], AF.Exp, scale=-LN2)
    K.act("activation", lgt[:, :], dec[:, :], AF.Ln, scale=-1.0, bias=1.0)
    gT = K.alloc("gT", [128, 8], F32)
    K.act("activation", gT[:, :], lgt[:, :], AF.Exp, scale=128.0)
    pcol = K.alloc("pcol", [128, 2], F32)
    K.pool("iota", pcol[:, 0:1], [[1, 1]], base=127, channel_multiplier=-1, allow_small_or_imprecise_dtypes=True)
    K.pool("iota", pcol[:, 1:2], [[1, 1]], base=0, channel_multiplier=1, allow_small_or_imprecise_dtypes=True)
    zloc = K.alloc("zloc", [128, 8], F32)
    for h in range(4):
        K.act("activation", zloc[:, h:h + 1], pcol[:, 0:1], AF.Exp, scale=lgt[:, h:h + 1])
        K.act("activation", zloc[:, 4 + h:5 + h], pcol[:, 1:2], AF.Exp, scale=lgt[:, 4 + h:5 + h])
    K.dve("tensor_scalar", zloc[:, :], zloc[:, :], 0.125, None, ALU.mult)
    diff = K.alloc("diff", [128, 128], F32, hi=True)
    K.pool("iota", diff[:, :], [[1, 128]], base=0, channel_multiplier=-1, allow_small_or_imprecise_dtypes=True)
    dpos = K.alloc("dpos", [128, 128], F32, hi=True)
    dneg = K.alloc("dneg", [128, 128], F32, hi=True)
    K.dve("tensor_scalar", dpos[:, :], diff[:, :], 0.0, None, ALU.max)
    K.dve("tensor_scalar", dneg[:, :], diff[:, :], -1.0, 0.0, ALU.mult, ALU.max)
    DmT = K.alloc("DmT", [128, 4, 128], F32)
    tmpd = K.alloc("tmpd", [128, 128], F32, hi=True)
    for h in range(4):
        K.dve("tensor_scalar", tmpd[:, :], dpos[:, :], lgt[:, h:h + 1], None, ALU.mult)
        K.dve("scalar_tensor_tensor", tmpd[:, :], dneg[:, :], lgt[:, 4 + h:5 + h], tmpd[:, :], ALU.mult, ALU.add)
        K.act("activation", DmT[:, h, :], tmpd[:, :], AF.Exp)
    trow = K.alloc("trow", [128, 2, 128], F32, hi=True)
    K.pool("iota", trow[:, 0, :], [[1, 128]], base=1, channel_multiplier=0, allow_small_or_imprecise_dtypes=True)
    K.pool("iota", trow[:, 1, :], [[-1, 128]], base=128, channel_multiplier=0, allow_small_or_imprecise_dtypes=True)
    Xi = K.alloc("Xi", [64, 2, 4, 128], F32)
    for h in range(4):
        K.act("activation", Xi[:, 0, h, :], trow[0:64, 0, :], AF.Exp, scale=lgt[0:64, h:h + 1])
        K.act("activation", Xi[:, 1, h, :], trow[0:64, 1, :], AF.Exp, scale=lgt[0:64, 4 + h:5 + h])
    K.free(diff, dpos, dneg, tmpd, trow)
    EF = K.alloc("EF", [128, 2, NKB], F32, hi=True)
    MF = K.alloc("MF", [128, 2, NKB], F32, hi=True)
    K.dma("sp", EF[:, 0, :], d["EF"][:, :])
    K.dma("sp", EF[:, 1, :], d["EB"][:, :])
    K.dma("sp", MF[:, 0, :], d["MF"][:, :])
    K.dma("sp", MF[:, 1, :], d["MB"][:, :])
    Z = K.alloc("Z", [128, 2, NKB, 4], F32, hi=True)
    for dr in range(2):
        for h in range(4):
            K.act("activation", Z[:, dr, :, h], EF[:, dr, :], AF.Exp, scale=lgt[:, dr * 4 + h:dr * 4 + h + 1])
        K.dve("scalar_tensor_tensor", Z[:, dr, :, :], Z[:, dr, :, :], 0.125,
              MF.v(lambda a: a[:, dr, :].unsqueeze(2).to_broadcast([128, NKB, 4])), ALU.mult, ALU.mult)
    K.free(EF, MF)
    if DBG:
        K.dma("sp", o_z[:, :], Z.v(lambda a: a.rearrange("p a b c -> p (a b c)")), is_out=True)

    Wkv = K.alloc("Wkv", [128, 8, 1024], BF16, hi=True)
    K.dma("pool", Wkv[:, :, 0:768], d["w_in"].v(lambda a: a.rearrange("(c p) n -> p c n", p=128)[:, :, 256:1024]))
    K.dma("pool", Wkv[:, :, 768:1024], d["w_in"].v(lambda a: a.rearrange("(c p) n -> p c n", p=128)[:, :, 2048:2304]))
    cosb = K.alloc("cosb", [128, SEQ // 128, 32], F32, hi=True)
    sinb = K.alloc("sinb", [128, SEQ // 128, 32], F32, hi=True)
    K.dma("sp", cosb[:, :, :], d["cosb"].v(lambda a: a.rearrange("(j p) n -> p j n", p=128)))
    K.dma("sp", sinb[:, :, :], d["sinb"].v(lambda a: a.rearrange("(j p) n -> p j n", p=128)))
    xt = [K.alloc("xt%d" % i, [128, 1024], F32) for i in range(2)]
    fr = Front(K, C)
    tmp = {"qn": K.alloc("qn", [128, 512], F32, hi=True), "t1": K.alloc("t1", [128, 256], F32, hi=True),
           "t2": K.alloc("t2", [128, 256], F32, hi=True)}
    kr = K.alloc("kr", [128, 256], F32, hi=True)
    kz = [K.alloc("kz%d" % i, [128, 2, 4, 64], BF16, hi=True) for i in range(2)]
    Vt = [K.alloc("Vt%d" % i, [128, 512], BF16, hi=True) for i in range(2)]
    trb = [K.banks[0], K.banks[1]]
    RFp, RBp = K.banks[6], K.banks[7]
    nx = 0
    def p1_front(i):
        nonlocal nx
        x_t = xt[nx % 2]
        nx += 1
        if i < 2:
            K.dma("sp", x_t[:, :], d["ctx"][i * 128:(i + 1) * 128, :])
        else:
            K.dma("sp", x_t[:, :], d["xb"][(i - 2) * 128:(i - 1) * 128, :])
        hT = fr.run(x_t[:, :], A1, SH1, 1 if i < 2 else 0, trb)
        kb, vb = K.banks[2 + i % 2], K.banks[4 + i % 2]
        for dc in range(8):
            K.pe("matmul", kb[:, 0:256], hT[:, dc, :], Wkv[:, dc, 0:256], start=(dc == 0), stop=(dc == 7))
        for dc in range(8):
            K.pe("matmul", vb[:, :], hT[:, dc, :], Wkv[:, dc, 256:768], start=(dc == 0), stop=(dc == 7))

    def p1_back(i):
        kb, vb = K.banks[2 + i % 2], K.banks[4 + i % 2]
        if i < 2:
            head_norm_rope(K, kb[:, 0:256], 4, None, None, None, kr, tmp)
        else:
            head_norm_rope(K, kb[:, 0:256], 4, None, cosb[:, i - 2, :], sinb[:, i - 2, :], kr, tmp)
        kz_, vt_ = kz[i % 2], Vt[i % 2]
        for dr in range(2):
            K.dve("tensor_tensor", kz_[:, dr, :, :], kr.v(lambda a: a[:, :].rearrange("p (h d) -> p h d", d=64)),
                  Z.v(lambda a: a[:, dr, i, :].unsqueeze(2).to_broadcast([128, 4, 64])), ALU.mult)
        K.act("activation", vt_[:, :], vb[:, :], AF.Copy)
        for dr, bank in ((0, RFp), (1, RBp)):
            for h in range(4):
                K.pe("matmul", bank[0:64, h * 128:(h + 1) * 128], kz_[:, dr, h, :], vt_[:, h * 128:(h + 1) * 128],
                     start=(i == 0 and h == 0), stop=(i == NKB - 1 and h == 3))
    p1_front(0)
    for i in range(NKB):
        if i + 1 < NKB:
            p1_front(i + 1)
        p1_back(i)
    Rf = K.alloc("Rf", [64, 512], F32)
    Rb = K.alloc("Rb", [64, 512], F32)
    K.act("activation", Rf[:, :], RFp[0:64, :], AF.Copy)
    K.act("activation", Rb[:, :], RBp[0:64, :], AF.Copy)
    if DBG:
        K.dma("sp", o_rf[:, 0:512], Rf[:, :], is_out=True)
        K.dma("sp", o_rf[:, 512:1024], Rb[:, :], is_out=True)
    K.free(cosb, sinb, Z)

    coso = K.alloc("coso", [128, NT + 2, 32], F32)
    sino = K.alloc("sino", [128, NT + 2, 32], F32)
    K.dma("sp", coso[:, 0:NT, :], d["coso"].v(lambda a: a.rearrange("(j p) n -> p j n", p=128)))
    K.dma("sp", sino[:, 0:NT, :], d["sino"].v(lambda a: a.rearrange("(j p) n -> p j n", p=128)))
    K.dma("sp", coso[:, NT:NT + 2, :], d["cosh"].v(lambda a: a.rearrange("(j p) n -> p j n", p=128)))
    K.dma("sp", sino[:, NT:NT + 2, :], d["sinh"].v(lambda a: a.rearrange("(j p) n -> p j n", p=128)))
    kTr = K.alloc("kTr", [128, NT, 4, 128], BF16)
    K.pool("memset", kTr[64:128, :, :, :], 0.0)
    Vr = K.alloc("Vr", [128, NT, 512], BF16)
    kzb = K.alloc("kzb", [128, NT, 4, 64], BF16)
    Rfb = K.alloc("Rfb", [128, NT, 512], BF16)
    Rbb = K.alloc("Rbb", [128, NT, 512], BF16)
    K.pool("memset", Rfb[64:128, :, :], 0.0)
    K.pool("memset", Rbb[64:128, :, :], 0.0)
    kTd = K.alloc("kTd", [128, 2, 20 * 128], BF16)
    K.pool("memset", kTd[64:128, :, :], 0.0)
    Vd = K.alloc("Vd", [128, 20, 2, 64], BF16)
    kzf = [K.alloc("kzf%d" % i, [128, 4, 64], BF16, hi=True) for i in range(2)]
    krd = K.alloc("krd", [128, 128], F32, hi=True)
    sweep = [("halo", 0), ("halo", 1), ("ctx", 0), ("ctx", 1)] + [("own", n) for n in range(NT)]
    def p2_front(n_, kind, n):
        nonlocal nx
        x_t = xt[nx % 2]
        nx += 1
        src = {"halo": d["xh"], "own": d["xo"], "ctx": d["ctx"]}[kind]
        K.dma("sp", x_t[:, :], src[n * 128:(n + 1) * 128, :])
        hT = fr.run(x_t[:, :], A1, SH1, 1 if kind == "ctx" else 0, trb)
        ab = K.banks[2 if n_ % 2 == 0 else 7]
        if kind == "own":
            for dc in range(8):
                K.pe("matmul", ab[:, 0:256], hT[:, dc, :], Wkv[:, dc, 0:256], start=(dc == 0), stop=(dc == 7))
        for dc in range(8):
            K.pe("matmul", ab[:, 256:512], hT[:, dc, :], Wkv[:, dc, 768:1024], start=(dc == 0), stop=(dc == 7))
        if kind == "own":
            vb = K.banks[4 + n_ % 2]
            for dc in range(8):
                K.pe("matmul", vb[:, :], hT[:, dc, :], Wkv[:, dc, 256:768], start=(dc == 0), stop=(dc == 7))

    def p2_back(n_, kind, n):
        idx = {"halo": 17 * n, "own": 1 + n, "ctx": 18 + n}[kind]
        ci = {"halo": NT + n, "own": n, "ctx": None}[kind]
        ab = K.banks[2 if n_ % 2 == 0 else 7]
        if ci is None:
            head_norm_rope(K, ab[:, 256:384], 2, None, None, None, krd, tmp)
        else:
            head_norm_rope(K, ab[:, 256:384], 2, None, coso[:, ci, :], sino[:, ci, :], krd, tmp)
        K.act("activation", Vd[:, idx, :, :], ab.v(lambda a: a[:, 384:512].rearrange("p (h d) -> p h d", d=64)), AF.Copy)
        tb = K.banks[3]
        for h in range(2):
            K.pe("transpose", tb[0:64, h * 128:(h + 1) * 128], krd[:, h * 64:(h + 1) * 64], C.ident[:, :])
        K.dve("tensor_copy", kTd[0:64, :, idx * 128:(idx + 1) * 128],
              tb.v(lambda a: a[0:64, 0:256].rearrange("p (h t) -> p h t", t=128)))
        if kind != "own":
            return
        vb = K.banks[4 + n_ % 2]
        head_norm_rope(K, ab[:, 0:256], 4, None, coso[:, n, :], sino[:, n, :], kr, tmp)
        K.act("activation", Vr[:, n, :], vb[:, :], AF.Copy)
        kzf_ = kzf[n % 2]
        K.dve("tensor_tensor", kzf_[:, :, :], kr.v(lambda a: a[:, :].rearrange("p (h d) -> p h d", d=64)),
              zloc.v(lambda a: a[:, 0:4].unsqueeze(2).to_broadcast([128, 4, 64])), ALU.mult)
        K.dve("tensor_tensor", kzb[:, n, :, :], kr.v(lambda a: a[:, :].rearrange("p (h d) -> p h d", d=64)),
              zloc.v(lambda a: a[:, 4:8].unsqueeze(2).to_broadcast([128, 4, 64])), ALU.mult)
        tk = K.banks[3]
        for h in range(4):
            K.pe("transpose", tk[0:64, h * 128:(h + 1) * 128], kr[:, h * 64:(h + 1) * 64], C.ident[:, :])
        K.act("activation", kTr[0:64, n, :, :], tk.v(lambda a: a[0:64, :].rearrange("p (h t) -> p h t", t=128)), AF.Copy, scale=0.125)
        K.act("activation", Rfb[0:64, n, :], Rf[:, :], AF.Copy)
        sb_ = K.banks[6]
        for h in range(4):
            K.pe("matmul", sb_[0:64, h * 128:(h + 1) * 128], kzf_[:, h, :], Vr[:, n, h * 128:(h + 1) * 128], start=True, stop=True)
        for h in range(4):
            K.dve("scalar_tensor_tensor", Rf[:, h * 128:(h + 1) * 128], Rf[:, h * 128:(h + 1) * 128], gT[0:64, h:h + 1],
                  sb_[0:64, h * 128:(h + 1) * 128], ALU.mult, ALU.add)
    p2_front(0, *sweep[0])
    for n_, (kind, n) in enumerate(sweep):
        if n_ + 1 < len(sweep):
            p2_front(n_ + 1, *sweep[n_ + 1])
        p2_back(n_, kind, n)
    for n in range(NT - 1, -1, -1):
        K.act("activation", Rbb[0:64, n, :], Rb[:, :], AF.Copy)
        sb_ = K.banks[6 + n % 2]
        for h in range(4):
            K.pe("matmul", sb_[0:64, h * 128:(h + 1) * 128], kzb[:, n, h, :], Vr[:, n, h * 128:(h + 1) * 128], start=True, stop=True)
        for h in range(4):
            K.dve("scalar_tensor_tensor", Rb[:, h * 128:(h + 1) * 128], Rb[:, h * 128:(h + 1) * 128], gT[0:64, 4 + h:5 + h],
                  sb_[0:64, h * 128:(h + 1) * 128], ALU.mult, ALU.add)
    K.free(Wkv, kr, kz[0], kz[1], Vt[0], Vt[1], kzf[0], kzf[1], krd, kzb, Rf, Rb)

    Wq = K.alloc("Wq", [128, 8, 1280], BF16)
    K.dma("pool", Wq[:, :, 0:256], d["w_in"].v(lambda a: a.rearrange("(c p) n -> p c n", p=128)[:, :, 0:256]))
    K.dma("pool", Wq[:, :, 256:1280], d["w_in"].v(lambda a: a.rearrange("(c p) n -> p c n", p=128)[:, :, 1024:2048]))
    Wo = K.alloc("Wo", [128, 8, D], BF16)
    K.dma("pool", Wo[:, :, :], d["w_out"].v(lambda a: a.rearrange("(c p) n -> p c n", p=128)))
    gng = K.alloc("gng", [128, 512], F32)
    K.dma("sp", gng[:, :], d["gng"][:, :])
    sink = K.alloc("sink", [128, 8], F32)
    K.dma("sp", sink[:, :], d["sink"][:, :])
    wm = K.alloc("wm", [128, 3, 384], F32)
    K.dma("sp", wm[:, :, :], d["wm"][:, :, :])
    rw = K.alloc("rw", [128, 8, 16], F32)
    K.dma("sp", rw[:, :, :], d["rw"].v(lambda a: a.rearrange("(c p) n -> p c n", p=128)))
    fr32 = Front(K, C, out_dt=F32, name="fr32", nbuf=1, share=fr)
    qr = K.alloc("qr", [128, 512], F32)
    qT = K.alloc("qT", [128, 3, 4, 128], BF16)
    K.pool("memset", qT[64:128, :, :, :], 0.0)
    PTr = K.alloc("PTr", [128, 512], BF16)
    cat = K.alloc("cat", [128, D], F32)
    catT = K.alloc("catT", [128, 8, 128], BF16)
    osb = K.alloc("osb", [128, 512], F32)
    sq = K.alloc("sq", [128, 512], F32)
    sg = sq
    st = K.alloc("st", [128, 32], F32)
    qTd = K.alloc("qTd", [128, 8, 128], BF16)
    K.pool("memset", qTd[64:128, :, :], 0.0)
    Pld = [K.alloc("Pl%d" % i, [128, 384], BF16) for i in range(2)]
    Pmd = [K.alloc("Pm%d" % i, [128, 640], BF16) for i in range(2)]
    PTdd = [K.alloc("PTd%d" % i, [128, 5, 128], BF16) for i in range(2)]
    smd = [K.alloc("sm%d" % i, [128, 16], F32) for i in range(2)]
    tg = cat
    affo = K.alloc("affo", [128, NT, 16], F32)
    smt = K.alloc("smt", [128, 24], F32)
    cat2 = [cat, K.alloc("cat_b", [128, D], F32)]
    qTd2 = [qTd, K.alloc("qTd_b", [128, 8, 128], BF16)]
    K.pool("memset", qTd2[1][64:128, :, :], 0.0)
    xsave = {}

    def h1(n):
        nonlocal nx
        cat_, qTd_ = cat2[n % 2], qTd2[n % 2]
        x_t = xt[nx % 2]
        nx += 1
        K.dma("sp", x_t[:, :], d["xo"][n * 128:(n + 1) * 128, :])
        hT = fr.run(x_t[:, :], A1, SH1, 0, trb)
        cqb, cgb, dqb = K.banks[2], K.banks[3], K.banks[4]
        for dc in range(8):
            K.pe("matmul", cqb[:, 0:256], hT[:, dc, :], Wq[:, dc, 0:256], start=(dc == 0), stop=(dc == 7))
        for dc in range(8):
            K.pe("matmul", cgb[:, :], hT[:, dc, :], Wq[:, dc, 256:768], start=(dc == 0), stop=(dc == 7))
        for dc in range(8):
            K.pe("matmul", dqb[:, :], hT[:, dc, :], Wq[:, dc, 768:1280], start=(dc == 0), stop=(dc == 7))
        head_norm_rope(K, cqb[:, 0:256], 4, None, coso[:, n, :], sino[:, n, :], qr, tmp)
        tq = K.banks[5]
        for h in range(4):
            K.pe("transpose", tq[0:64, h * 128:(h + 1) * 128], qr[:, h * 64:(h + 1) * 64], C.ident[:, :])
        K.act("activation", qT[0:64, 0, :, :], tq.v(lambda a: a[0:64, :].rearrange("p (h t) -> p h t", t=128)), AF.Copy)
        for v_ in range(2):
            K.dve("tensor_tensor", qT[0:64, 1 + v_, :, :], tq.v(lambda a: a[0:64, :].rearrange("p (h t) -> p h t", t=128)),
                  Xi[:, v_, :, :], ALU.mult)
        atb = K.banks[6]
        for h in range(4):
            K.pe("matmul", atb[:, h * 128:(h + 1) * 128], kTr[:, n, h, :], qT[:, 0, h, :], start=True, stop=True)
        K.dve("tensor_tensor", PTr[:, :], atb[:, :], DmT.v(lambda a: a.rearrange("p h t -> p (h t)")), ALU.mult)
        ob = K.banks[7]
        for h in range(4):
            hs = slice(h * 128, (h + 1) * 128)
            K.pe("matmul", ob[:, hs], PTr[:, hs], Vr[:, n, hs], start=True, stop=False)
            K.pe("matmul", ob[:, hs], qT[:, 1, h, :], Rfb[:, n, hs], start=False, stop=False)
            K.pe("matmul", ob[:, hs], qT[:, 2, h, :], Rbb[:, n, hs], start=False, stop=True)
        K.act("activation", osb[:, :], ob[:, :], AF.Copy)
        K.act("activation", sq[:, :], ob[:, :], AF.Square)
        K.dve("tensor_reduce", st[:, 0:4], osb.v(lambda a: a.rearrange("p (h v) -> p h v", v=128)), AX.X, ALU.add)
        K.dve("tensor_reduce", st[:, 4:8], sq.v(lambda a: a.rearrange("p (h v) -> p h v", v=128)), AX.X, ALU.add)
        K.dve("tensor_scalar", st[:, 8:12], st[:, 0:4], 1.0 / 128, None, ALU.mult)
        K.dve("tensor_tensor", st[:, 12:16], st[:, 8:12], st[:, 8:12], ALU.mult)
        K.dve("scalar_tensor_tensor", st[:, 16:20], st[:, 4:8], 1.0 / 128, st[:, 12:16], ALU.mult, ALU.subtract)
        K.act("activation", st[:, 20:24], st[:, 16:20], AF.Sqrt, bias=1e-5, scale=1.0)
        K.dve("reciprocal", st[:, 24:28], st[:, 20:24])
        for h in range(4):
            hs = slice(h * 128, (h + 1) * 128)
            K.dve("tensor_scalar", osb[:, hs], osb[:, hs], st[:, 8 + h:9 + h], st[:, 24 + h:25 + h], ALU.subtract, ALU.mult)
        K.act("activation", sg[:, :], cgb[:, :], AF.Silu)
        K.pool("tensor_tensor", osb[:, :], osb[:, :], gng[:, :], ALU.mult)
        K.pool("tensor_tensor", cat_[:, 0:512], osb[:, :], sg[:, :], ALU.mult)
        head_norm_rope(K, dqb[:, :], 8, None, coso[:, n, :], sino[:, n, :], qr, tmp)
        for h in range(8):
            bk = K.banks[5 + h // 4]
            K.pe("transpose", bk[0:64, (h % 4) * 128:(h % 4 + 1) * 128], qr[:, h * 64:(h + 1) * 64], C.ident[:, :])
        for half in range(2):
            bk = K.banks[5 + half]
            K.act("activation", qTd_[0:64, half * 4:half * 4 + 4, :], bk.v(lambda a: a[0:64, :].rearrange("p (h t) -> p h t", t=128)), AF.Copy)
        xsave[n] = x_t

    def h2(n):
        cat_, qTd_ = cat2[n % 2], qTd2[n % 2]
        x_t = xsave[n]
        mi = 0 if n == 0 else (2 if n == NT - 1 else 1)
        odb = K.banks[7]

        def win_a(h):
            kv = h // 4
            sm_, Pl_, Pm_ = smd[h % 2], Pld[h % 2], Pmd[h % 2]
            s1, s2 = K.banks[2 + h % 2], K.banks[4 + h % 2]
            K.pe("matmul", s1[:, 0:384], qTd_[:, h, :], kTd[:, kv, n * 128:(n + 3) * 128], start=True, stop=True)
            K.pe("matmul", s2[:, 0:256], qTd_[:, h, :], kTd[:, kv, 18 * 128:20 * 128], start=True, stop=True)
            K.dve("tensor_reduce", sm_[:, 0:1], s1[:, 0:384], AX.X, ALU.max)
            K.dve("tensor_reduce", sm_[:, 1:2], s2[:, 0:256], AX.X, ALU.max)
            K.dve("tensor_tensor", sm_[:, 2:3], sm_[:, 0:1], sm_[:, 1:2], ALU.max)
            K.dve("scalar_tensor_tensor", sm_[:, 3:4], sm_[:, 2:3], 0.125, sink[:, h:h + 1], ALU.mult, ALU.max)
            K.dve("tensor_scalar", sm_[:, 4:5], sm_[:, 3:4], -1.0, None, ALU.mult)
            K.act("activation", Pl_[:, 0:384], s1[:, 0:384], AF.Exp, bias=sm_[:, 4:5], scale=0.125)
            K.act("activation", Pm_[:, 384:640], s2[:, 0:256], AF.Exp, bias=sm_[:, 4:5], scale=0.125, accum_out=sm_[:, 5:6])
            K.act("activation", sm_[:, 6:7], sink[:, h:h + 1], AF.Exp, bias=sm_[:, 4:5], scale=1.0)
            K.dve("scalar_tensor_tensor", Pm_[:, 0:384], Pl_[:, 0:384], 1.0, wm[:, mi, :], ALU.mult, ALU.mult, accum_out=sm_[:, 7:8])
            K.dve("tensor_tensor", sm_[:, 8:9], sm_[:, 5:6], sm_[:, 6:7], ALU.add)
            K.dve("tensor_tensor", sm_[:, 8:9], sm_[:, 8:9], sm_[:, 7:8], ALU.add)
            K.dve("reciprocal", sm_[:, 9:10], sm_[:, 8:9])

        def win_b(h):
            kv = h // 4
            sm_, Pm_, PTd_ = smd[h % 2], Pmd[h % 2], PTdd[h % 2]
            ptb = K.banks[6]
            for i in range(5):
                K.pe("transpose", ptb.v(lambda a: a.bitcast(BF16)[:, i * 128:(i + 1) * 128]), Pm_[:, i * 128:(i + 1) * 128], C.identb[:, :])
            K.act("activation", PTd_[:, :, :], ptb.v(lambda a: a.bitcast(BF16)[:, 0:640].rearrange("p (i t) -> p i t", t=128)), AF.Copy)
            for i in range(5):
                vidx = (n + i) if i < 3 else (18 + i - 3)
                K.pe("matmul", odb[:, h * 64:(h + 1) * 64], PTd_[:, i, :], Vd[:, vidx, kv, :], start=(i == 0), stop=(i == 4))
            K.dve("tensor_scalar", cat_[:, 512 + h * 64:512 + (h + 1) * 64], odb[:, h * 64:(h + 1) * 64], sm_[:, 9:10], None, ALU.mult)
        win_a(0)
        for h in range(8):
            if h + 1 < 8:
                win_a(h + 1)
            win_b(h)
        if DBG:
            K.dma("sp", o_cat[n * 128:(n + 1) * 128, :], cat_[:, :], is_out=True)
        for dc in range(8):
            bk = trb[dc // 4]
            K.pe("transpose", bk[:, (dc % 4) * 128:(dc % 4 + 1) * 128], cat_[:, dc * 128:(dc + 1) * 128], C.ident[:, :])
        for half in range(2):
            K.act("activation", catT[:, half * 4:half * 4 + 4, :], trb[half].v(lambda a: a[:, :].rearrange("p (c t) -> p c t", t=128)), AF.Copy)
        x3t = cat_
        for half in range(2):
            mb = K.banks[2 + half]
            for dc in range(8):
                K.pe("matmul", mb[:, :], catT[:, dc, :], Wo[:, dc, half * 512:(half + 1) * 512], start=(dc == 0), stop=(dc == 7))
            K.dve("tensor_tensor", cat_[:, half * 512:(half + 1) * 512], mb[:, :], G1[:, half * 512:(half + 1) * 512], ALU.mult)
        K.pool("tensor_tensor", x3t[:, :], cat_[:, :], x_t[:, :], ALU.add)
        K.dma("sp", o_x3[n * 128:(n + 1) * 128, :], x3t[:, :], is_out=True)
        hT32 = fr32.run(x3t[:, :], A2, SH2, 0, trb)
        lb = K.banks[4]
        for dc in range(8):
            K.pe("matmul", lb[:, 0:16], hT32[:, dc, :], rw[:, dc, :], start=(dc == 0), stop=(dc == 7))
        softmax16(K, lb[:, 0:16], affo[:, n, :], smt)
    h1(0)
    for n in range(NT):
        if n + 1 < NT:
            h1(n + 1)
        h2(n)
    K.dma("sp", o_aff.v(lambda a: a.rearrange("(j p) e -> p j e", p=128)), affo[:, :, :], is_out=True)
    K.finish()
    return nc, es


def prep_stage3(inp, x2, ctx2):
    cos, sin = rope_tables()
    p = np.arange(128)
    tq, tk = p[:, None], p[None, :]
    prev_std = (tq <= tk).astype(np.float32)
    next_std = (tk <= tq).astype(np.float32)
    ones = np.ones((128, 128), np.float32)
    zeros = np.zeros((128, 128), np.float32)
    maps = []
    for core in range(NCORES):
        b, r = core // 4, core % 4
        t0, t1 = r * NOWN, (r + 1) * NOWN
        x = x2[b]
        xh = np.zeros((256, D), np.float32)
        ch = np.zeros((256, 32), np.float32)
        sh = np.zeros((256, 32), np.float32)
        if r > 0:
            xh[0:128] = x[t0 - 128:t0]
            ch[0:128], sh[0:128] = cos[t0 - 128:t0], sin[t0 - 128:t0]
        if r < 3:
            xh[128:256] = x[t1:t1 + 128]
            ch[128:256], sh[128:256] = cos[t1:t1 + 128], sin[t1:t1 + 128]
        EF = np.zeros((128, NKB), np.float32)
        EB = np.zeros((128, NKB), np.float32)
        MF = np.zeros((128, NKB), np.float32)
        MB = np.zeros((128, NKB), np.float32)
        for i in range(NKB):
            if i < 2:
                m = i * 128 + p
                EF[:, i] = t0 - 1 + LC - m
                MF[:, i] = 1.0
                EB[:, i] = SEQ + m - t1
                MB[:, i] = 1.0
            else:
                pos = (i - 2) * 128 + p
                if pos[0] < t0:
                    EF[:, i] = t0 - 1 - pos
                    MF[:, i] = 1.0
                elif pos[0] >= t1:
                    EB[:, i] = pos - t1
                    MB[:, i] = 1.0
        wm = np.stack([np.concatenate([prev_std if r > 0 else zeros, ones, next_std], axis=1),
                       np.concatenate([prev_std, ones, next_std], axis=1),
                       np.concatenate([prev_std, ones, next_std if r < 3 else zeros], axis=1)], axis=1)
        m = {
            "xb": x, "xo": x[t0:t1], "xh": xh, "ctx": ctx2[b],
            "c2": np.stack([fm(inp["c"][b]), fm(inp["c_ctx"])], axis=-1),
            "ada_w": inp["ada_w"][1], "ada_b": inp["ada_b"][1][None, :],
            "ada_bT": np.ascontiguousarray(inp["ada_b"][1].reshape(48, 128).T),
            "gmixT": fm(inp["norm_mix_g"][1]), "gffnT": fm(inp["norm_ffn_g"][1]),
            "w_in": inp["cd_w_in"][0], "w_out": inp["cd_w_out"][0],
            "dec": np.broadcast_to(np.concatenate([inp["c_decay_fwd"][0], inp["c_decay_bwd"][0]])[None, :], (128, 8)),
            "gng": np.broadcast_to(inp["c_norm_g"][0][None, :], (128, 512)),
            "sink": np.broadcast_to(inp["d_sink"][0][None, :], (128, 8)),
            "cosb": cos, "sinb": sin, "coso": cos[t0:t1], "sino": sin[t0:t1], "cosh": ch, "sinh": sh,
            "EF": EF, "EB": EB, "MF": MF, "MB": MB, "wm": wm, "rw": inp["moe_router"][1],
        }
        maps.append({k: np.ascontiguousarray(v, dtype=np.float32) for k, v in m.items()})
    return maps


def _run(builder, maps):
    nc, es = builder()
    es.close()
    res = run_bass_kernel_spmd(nc, maps, core_ids=list(range(NCORES)))
    return res.results


def _gather(results, key):
    return np.stack([np.concatenate([np.asarray(results[b * 4 + r][key]) for r in range(4)], axis=0) for b in range(2)])


def kernel(**inputs):
    inp = {k: np.asarray(v) for k, v in inputs.items()}
    r1 = _run(build_stage1, prep_stage1(inp))
    x1, aff0 = _gather(r1, "x1"), _gather(r1, "aff")
    ctx1 = np.stack([np.asarray(r1[0]["ctx1"]), np.asarray(r1[4]["ctx1"])])
    affc = np.stack([np.asarray(r1[0]["affc"]), np.asarray(r1[4]["affc"])])
    r2 = _run(lambda: build_moe(True, False), prep_moe(inp, 0, x1, aff0, ctx1, affc))
    x2 = _gather(r2, "x2")
    ctx2 = np.stack([np.asarray(r2[0]["ctx2"]), np.asarray(r2[4]["ctx2"])])
    r3 = _run(build_stage3, prep_stage3(inp, x2, ctx2))
    x3, aff1 = _gather(r3, "x3"), _gather(r3, "aff")
    r4 = _run(lambda: build_moe(False, True), prep_moe(inp, 1, x3, aff1, final=True))
    return _gather(r4, "x2").astype(np.float32)
```

```python
import contextlib
import numpy as np
import concourse.bass as bass
import concourse.mybir as mybir
from concourse.bass_utils import run_bass_kernel_spmd

F32 = mybir.dt.float32
BF16 = mybir.dt.bfloat16
ALU = mybir.AluOpType
AF = mybir.ActivationFunctionType
AX = mybir.AxisListType

NCORES = 8
D = 1024
SEQ = 8192
LC = 256
NOWN = 2048
NT = 16
EPS = 1e-6


class Buf:
    __slots__ = ("name", "w", "r")

    def __init__(self, name=""):
        self.name = name
        self.w = None
        self.r = []


class Prog:
    ENGS = ("pe", "act", "dve", "pool", "sp")
    NDMA = 12

    def __init__(self, nc):
        self.nc = nc
        self.ops = {e: [] for e in self.ENGS}
        self.cnt = {e: 0 for e in self.ENGS}
        self.seen = {e: {} for e in self.ENGS}
        self.ndma = {e: 0 for e in self.ENGS}
        self.dma_tok = {e: [] for e in self.ENGS}
        self.out_tokens = []

    def _need(self, eng, tok, raw):
        if tok is None:
            return None
        if tok[0] == "E":
            if tok[1] == eng and not raw:
                return None
            key = ("E", tok[1])
            val = tok[2]
        else:
            key = ("D", tok[1], tok[2])
            val = tok[3]
        if self.seen[eng].get(key, 0) >= val:
            return None
        return key, val

    def op(self, eng, fn, reads=(), writes=(), dma=False, is_out=False):
        needs = {}

        def add(tok, raw):
            n = self._need(eng, tok, raw)
            if n is not None:
                k, v = n
                if needs.get(k, 0) < v:
                    needs[k] = v
        for b in reads:
            add(b.w, True)
        for b in writes:
            add(b.w, False)
            for t in b.r:
                add(t, False)
        if dma:
            n = self.ndma[eng]
            slot = n % self.NDMA
            val = 16 * (n // self.NDMA + 1)
            if n >= self.NDMA:
                add(self.dma_tok[eng][n - self.NDMA], True)
            tok = ("D", eng, slot, val)
            self.ndma[eng] += 1
            self.dma_tok[eng].append(tok)
            inc = ("D", eng, slot)
        else:
            self.cnt[eng] += 1
            tok = ("E", eng, self.cnt[eng])
            inc = ("E", eng)
        for k, v in needs.items():
            self.seen[eng][k] = v
        self.ops[eng].append((list(needs.items()), fn, inc))
        for b in reads:
            b.r.append(tok)
        for b in writes:
            b.w = tok
            b.r = []
        if is_out:
            self.out_tokens.append(tok)
        return tok

    def emit(self, es):
        nc = self.nc
        sems = {}
        for e in self.ENGS:
            sems[("E", e)] = es.enter_context(nc.semaphore("s_" + e))
            for s in range(min(self.NDMA, self.ndma[e])):
                sems[("D", e, s)] = es.enter_context(nc.semaphore("d_%s_%d" % (e, s)))
        fin = {}
        for tok in self.out_tokens:
            key = ("D", tok[1], tok[2])
            fin[key] = max(fin.get(key, 0), tok[3])
        block = es.enter_context(nc.Block())
        engobj = {"pe": "tensor", "act": "scalar", "dve": "vector", "pool": "gpsimd", "sp": "sync"}

        def mk(e):
            def body(eng):
                for waits, fn, inc in self.ops[e]:
                    for k, v in waits:
                        eng.wait_ge(sems[k], v)
                    ins = fn(eng)
                    ins.then_inc(sems[inc], 16 if inc[0] == "D" else 1)
                if e == "sp":
                    for k, v in fin.items():
                        eng.wait_ge(sems[k], v)
            return body
        for e in self.ENGS:
            if self.ops[e] or e == "sp":
                getattr(block, engobj[e])(mk(e))


DTSIZE = {F32: 4, BF16: 2}


class Tile:
    def __init__(self, K, name, ap, off=0, size=0):
        self.K = K
        self.name = name
        self.ap = ap
        self.b = Buf(name)
        self.off = off
        self.size = size

    def __getitem__(self, idx):
        a = self.ap[idx]
        self.K.reg[id(a)] = (a, self.b)
        return a

    def v(self, fn):
        a = fn(self.ap)
        self.K.reg[id(a)] = (a, self.b)
        return a


class KB:
    def __init__(self, nc, es, arena_bytes=200 * 1024):
        self.nc = nc
        self.es = es
        self.P = Prog(nc)
        self.reg = {}
        self.arena = es.enter_context(nc.sbuf_tensor("arena", [128, arena_bytes // 4], F32))
        self.arena_bytes = arena_bytes
        self.live = []
        self.dead = []
        self.banks = []
        for i in range(8):
            t = es.enter_context(nc.psum_tensor("bank%d" % i, [128, 512], F32))
            self.banks.append(Tile(self, "bank%d" % i, t[:, :]))
        self.ndram = 0

    def alloc(self, name, shape, dt=F32, hi=False):
        n = 1
        for s in shape[1:]:
            n *= s
        size = (n * DTSIZE[dt] + 31) // 32 * 32
        self.live.sort(key=lambda t: t.off)
        if hi:
            off = self.arena_bytes - size
            for t in reversed(self.live):
                if t.off + t.size <= off:
                    break
                off = min(off, t.off - size)
            assert off >= 0, "SBUF arena overflow (hi): %s" % name
        else:
            off = 0
            for t in self.live:
                if off + size <= t.off:
                    break
                off = max(off, t.off + t.size)
        if off + size > self.arena_bytes:
            print("ARENA:", [(t.name, t.off, t.size) for t in self.live])
        assert off + size <= self.arena_bytes, "SBUF arena overflow: %s %d" % (name, off + size)
        ap = self.arena[0:shape[0], off // 4:(off + size) // 4]
        if dt != F32:
            ap = ap.bitcast(dt)
        ap = ap[:, 0:n]
        if len(shape) == 3:
            ap = ap.rearrange("p (a b) -> p a b", a=shape[1], b=shape[2])
        elif len(shape) == 4:
            ap = ap.rearrange("p (a b c) -> p a b c", a=shape[1], b=shape[2], c=shape[3])
        tl = Tile(self, name, ap, off, size)
        keep = []
        for d in self.dead:
            if d.off < off + size and off < d.off + d.size:
                if d.b.w is not None:
                    tl.b.r.append(d.b.w)
                tl.b.r.extend(d.b.r)
            keep.append(d)
        self.dead = keep
        self.live.append(tl)
        return tl

    def free(self, *tiles):
        for t in tiles:
            self.live.remove(t)
            self.dead.append(t)

    def dram(self, name, shape, dt=F32, kind="ExternalInput"):
        t = self.nc.dram_tensor(name, list(shape), dt, kind=kind)
        return Tile(self, name, t.ap())

    def _infer(self, args, kw):
        out_ap = kw.get("out", args[0] if args else None)
        acc = kw.get("accum_out", None)
        reads, writes = [], []
        for a in list(args) + list(kw.values()):
            ent = self.reg.get(id(a))
            if ent is None:
                continue
            if a is out_ap or a is acc:
                if ent[1] not in writes:
                    writes.append(ent[1])
            else:
                if ent[1] not in reads:
                    reads.append(ent[1])
        return reads, writes

    def op(self, eng, meth, *args, r=(), w=(), **kw):
        reads, writes = self._infer(args, kw)
        reads = reads + [x.b if isinstance(x, Tile) else x for x in r]
        writes = writes + [x.b if isinstance(x, Tile) else x for x in w]
        return self.P.op(eng, lambda e: getattr(e, meth)(*args, **kw), reads, writes)

    def dve(self, meth, *a, **k):
        return self.op("dve", meth, *a, **k)

    def act(self, meth, *a, **k):
        return self.op("act", meth, *a, **k)

    def pool(self, meth, *a, **k):
        return self.op("pool", meth, *a, **k)

    def pe(self, meth, *a, **k):
        return self.op("pe", meth, *a, **k)

    def dma(self, q, out, in_, is_out=False):
        reads, writes = self._infer((out, in_), {})
        return self.P.op(q, lambda e: e.dma_start(out=out, in_=in_), reads, writes, dma=True, is_out=is_out)

    def finish(self):
        self.P.emit(self.es)


def bc_mid(ap, shape):
    return ap.unsqueeze(2).to_broadcast(list(shape))


def bc_heads(ap, shape):
    return ap.unsqueeze(1).to_broadcast(list(shape))


class Ctx:
    pass


def setup_consts(K):
    C = Ctx()
    C.ident = K.alloc("ident", [128, 128], F32)
    K.pool("memset", C.ident[:, :], 1.0)
    K.pool("affine_select", C.ident[:, :], C.ident[:, :], [[-1, 128]], ALU.is_equal, 0.0, base=0, channel_multiplier=1)
    C.ones = K.alloc("ones", [128, 128], F32)
    K.pool("memset", C.ones[:, :], 1.0)
    C.identb = K.alloc("identb", [128, 128], BF16)
    K.dve("tensor_copy", C.identb[:, :], C.ident[:, :])
    return C


def modulation_gen(K, C, d_c2, d_adaw, d_adab, d_adabT, need_vec, need_gate, res):
    c2 = K.alloc("c2", [128, 8, 2], F32, hi=True)
    K.dma("sp", c2[:, :, :], d_c2[:, :, :])
    sil = K.alloc("sil", [128, 8, 2], F32, hi=True)
    K.act("activation", sil[:, :, :], c2[:, :, :], AF.Silu)
    rep = [K.alloc("rep%d" % i, [128, 8, 128], F32, hi=True) for i in range(2)]
    for i in range(2):
        K.dve("tensor_copy", rep[i][:, :, :], sil.v(lambda a: a[:, :, i:i + 1].to_broadcast([128, 8, 128])))
    brow = K.alloc("adab_row", [1, 6144], F32, hi=True)
    K.dma("sp", brow[:, :], d_adab[:, :])
    bT = K.alloc("adabT", [128, 48], F32, hi=True)
    K.dma("sp", bT[:, :], d_adabT[:, :])
    modT = K.alloc("modT", [128, 48, 2], F32)
    wblk = [K.alloc("adaw%d" % i, [128, 8, 512], F32, hi=True) for i in range(2)]
    gates = {}
    mps = K.banks[7]
    n = 0
    for blk in range(12):
        if blk not in need_vec and blk not in need_gate:
            continue
        wb = wblk[n % 2]
        n += 1
        K.dma("sp", wb[:, :, :], d_adaw.v(lambda a: a.rearrange("(c p) n -> p c n", p=128)[:, :, blk * 512:(blk + 1) * 512]))
        if blk in need_vec:
            for sub in range(4):
                ec = blk * 4 + sub
                for kc in range(8):
                    K.pe("matmul", mps[:, ec * 2:ec * 2 + 2], wb[:, kc, sub * 128:(sub + 1) * 128], sil[:, kc, :],
                         start=(kc == 0), stop=(kc == 7))
            for sub in range(4):
                ec = blk * 4 + sub
                K.act("activation", modT[:, ec, :], mps[:, ec * 2:ec * 2 + 2], AF.Identity, bias=bT[:, ec:ec + 1], scale=1.0)
        else:
            for i in range(2):
                gp = K.banks[5 + i]
                for kc in range(8):
                    K.pe("matmul", gp[:, :], rep[i][:, kc, :], wb[:, kc, :], start=(kc == 0), stop=False)
                K.pe("matmul", gp[:, :], C.ones[0:1, :], brow[0:1, blk * 512:(blk + 1) * 512], start=False, stop=True)
                key = (blk // 2, i)
                if key not in gates:
                    gates[key] = K.alloc("gate%d_%d" % key, [128, 1024], F32)
                half = blk % 2
                K.act("activation", gates[key][:, half * 512:(half + 1) * 512], gp[:, :], AF.Copy)
        yield blk
    K.free(c2, rep[0], rep[1], brow, bT, wblk[0], wblk[1])
    res.extend([modT, gates, sil])


def modulation(K, C, d_c2, d_adaw, d_adab, d_adabT, need_vec, need_gate):
    res = []
    for _ in modulation_gen(K, C, d_c2, d_adaw, d_adab, d_adabT, need_vec, need_gate, res):
        pass
    return res[0], res[1], res[2]


def mod_cols(K, modT, d_g, sh_blk, sc_blk, name):
    gT = K.alloc(name + "_gT", [128, 8], F32, hi=True)
    K.dma("sp", gT[:, :], d_g[:, :])
    A = K.alloc(name + "_A", [128, 8, 2], F32)
    K.dve("tensor_scalar", A[:, :, :], modT[:, sc_blk * 4:sc_blk * 4 + 8, :], 1.0, None, ALU.add)
    K.dve("tensor_tensor", A[:, :, :], A[:, :, :], gT.v(lambda a: a[:, :].unsqueeze(2).to_broadcast([128, 8, 2])), ALU.mult)
    SH = K.alloc(name + "_SH", [128, 8, 2], F32)
    K.dve("tensor_copy", SH[:, :, :], modT[:, sh_blk * 4:sh_blk * 4 + 8, :])
    K.free(gT)
    return A, SH


class Front:
    def __init__(self, K, C, out_dt=BF16, nbuf=2, name="fr", share=None):
        self.K, self.C = K, C
        if share is not None:
            self.xn, self.junk, self.ss = share.xn[:nbuf], share.junk, share.ss[:nbuf]
        else:
            self.xn = [K.alloc(name + "_xn%d" % i, [128, 1024], F32) for i in range(nbuf)]
            self.junk = K.alloc(name + "_junk", [128, 1024], F32)
            self.ss = [K.alloc(name + "_ss%d" % i, [128, 4], F32) for i in range(nbuf)]
        self.hT = [K.alloc(name + "_hT%d" % i, [128, 8, 128], out_dt) for i in range(nbuf)]
        self.n = 0
        self.nbuf = nbuf

    def free(self):
        self.K.free(*(self.xn + self.ss + self.hT + [self.junk]))

    def run(self, xt_ap, A, SH, col, banks):
        K, C = self.K, self.C
        i = self.n % self.nbuf
        self.n += 1
        xn, ss, hT = self.xn[i], self.ss[i], self.hT[i]
        K.act("activation", self.junk[:, :], xt_ap, AF.Square, accum_out=ss[:, 0:1])
        K.act("activation", ss[:, 1:2], ss[:, 0:1], AF.Sqrt, bias=EPS, scale=1.0 / D)
        K.dve("reciprocal", ss[:, 2:3], ss[:, 1:2])
        K.act("activation", xn[:, :], xt_ap, AF.Copy, scale=ss[:, 2:3])
        for dc in range(8):
            bk = banks[dc // 4]
            K.pe("transpose", bk[:, (dc % 4) * 128:(dc % 4 + 1) * 128], xn[:, dc * 128:(dc + 1) * 128], C.ident[:, :])
        for dc in range(8):
            bk = banks[dc // 4]
            K.dve("tensor_scalar", hT[:, dc, :], bk[:, (dc % 4) * 128:(dc % 4 + 1) * 128],
                  A[:, dc, col:col + 1], SH[:, dc, col:col + 1], ALU.mult, ALU.add)
        return hT


def head_norm_rope(K, src_ap, H, G, cos_ap, sin_ap, out, tmp):
    n = H * 64
    if G is not None:
        sq, ssh, qn = tmp["sq"], tmp["ssh"], tmp["qn"]
        K.act("activation", sq[:, 0:n], src_ap, AF.Square)
        K.dve("tensor_reduce", ssh[:, 0:H], sq.v(lambda a: a[:, 0:n].rearrange("p (h d) -> p h d", d=64)), AX.X, ALU.add)
        K.act("activation", ssh[:, 8:8 + H], ssh[:, 0:H], AF.Sqrt, bias=EPS, scale=1.0 / 64)
        K.dve("reciprocal", ssh[:, 16:16 + H], ssh[:, 8:8 + H])
        dst = qn if cos_ap is not None else out
        K.dve("tensor_tensor", dst.v(lambda a: a[:, 0:n].rearrange("p (h d) -> p h d", d=64)),
              _reg_like(K, src_ap, src_ap.rearrange("p (h d) -> p h d", d=64)),
              ssh.v(lambda a: a[:, 16:16 + H].unsqueeze(2).to_broadcast([128, H, 64])), ALU.mult)
        K.pool("tensor_tensor", dst.v(lambda a: a[:, 0:n].rearrange("p (h d) -> p h d", d=64)),
               dst.v(lambda a: a[:, 0:n].rearrange("p (h d) -> p h d", d=64)),
               G.v(lambda a: a[:, :].unsqueeze(1).to_broadcast([128, H, 64])), ALU.mult)
    else:
        qn = tmp["qn"]
        dst = qn if cos_ap is not None else out
        K.act("activation", dst[:, 0:n], src_ap, AF.Copy)
    if cos_ap is None:
        return
    t1, t2 = tmp["t1"], tmp["t2"]

    def v4(t, half):
        return t.v(lambda a: a[:, 0:n].rearrange("p (h t d) -> p h t d", t=2, d=32)[:, :, half, :])

    def v3(t):
        return t.v(lambda a: a[:, 0:H * 32].rearrange("p (h d) -> p h d", d=32))
    cb = _reg_like(K, cos_ap, cos_ap.unsqueeze(1).to_broadcast([128, H, 32]))
    sb = _reg_like(K, sin_ap, sin_ap.unsqueeze(1).to_broadcast([128, H, 32]))
    K.pool("tensor_tensor", v3(t1), v4(qn, 0), cb, ALU.mult)
    K.pool("tensor_tensor", v3(t2), v4(qn, 1), sb, ALU.mult)
    K.dve("tensor_tensor", v4(out, 0), v3(t1), v3(t2), ALU.subtract)
    K.pool("tensor_tensor", v3(t1), v4(qn, 0), sb, ALU.mult)
    K.pool("tensor_tensor", v3(t2), v4(qn, 1), cb, ALU.mult)
    K.dve("tensor_tensor", v4(out, 1), v3(t1), v3(t2), ALU.add)


def _reg_like(K, base_ap, new_ap):
    ent = K.reg.get(id(base_ap))
    assert ent is not None
    K.reg[id(new_ap)] = (new_ap, ent[1])
    return new_ap


def attention(K, C, qT, NQ, QB, key_tiles, kT, Vaug, aT, PT, osb, rden, scale):
    sbanks = [K.banks[0], K.banks[1], K.banks[2]]
    obanks = [K.banks[3], K.banks[4]]
    bcb = K.banks[5]
    steps = []
    for h in range(8):
        for qb in range(NQ // QB):
            for i, kt in enumerate(key_tiles):
                steps.append((h, qb, i, kt))
    nk = len(key_tiles)

    def qk(n):
        h, qb, i, kt = steps[n]
        K.pe("matmul", sbanks[n % 3][:, 0:QB], kT[:, h // 4, kt * 128:(kt + 1) * 128], qT[:, h, qb * QB:(qb + 1) * QB],
             start=True, stop=True)
    LOOK = 2
    for n in range(min(LOOK, len(steps))):
        qk(n)
    for n, (h, qb, i, kt) in enumerate(steps):
        if n + LOOK < len(steps):
            qk(n + LOOK)
        pt = PT[n % 3]
        K.act("activation", pt[:, 0:QB], sbanks[n % 3][:, 0:QB], AF.Exp, scale=scale)
        ob = obanks[(h * (NQ // QB) + qb) % 2]
        K.pe("matmul", ob[0:65, 0:QB], Vaug[:, kt, h // 4, :], pt[:, 0:QB], start=(i == 0), stop=(i == nk - 1))
        if i == nk - 1:
            K.dve("reciprocal", rden[64:65, 0:QB], ob[64:65, 0:QB])
            K.pe("matmul", bcb[0:64, 0:QB], C.ones[64:65, 0:64], rden[64:65, 0:QB], start=True, stop=True)
            K.act("activation", osb[0:64, 0:QB], ob[0:64, 0:QB], AF.Copy)
            K.dve("tensor_tensor", aT[0:64, h, qb * QB:(qb + 1) * QB], osb[0:64, 0:QB], bcb[0:64, 0:QB], ALU.mult)


def softmax16(K, logits_ap, aff_out_ap, tmp):
    K.dve("tensor_reduce", tmp[:, 0:1], logits_ap, AX.X, ALU.max)
    K.dve("tensor_scalar", tmp[:, 1:2], tmp[:, 0:1], -1.0, None, ALU.mult)
    K.act("activation", tmp[:, 8:24], logits_ap, AF.Exp, bias=tmp[:, 1:2], scale=1.0, accum_out=tmp[:, 2:3])
    K.dve("reciprocal", tmp[:, 3:4], tmp[:, 2:3])
    K.dve("tensor_scalar", aff_out_ap, tmp[:, 8:24], tmp[:, 3:4], None, ALU.mult)


def build_stage1():
    nc = bass.Bass("TRN2", target_bir_lowering=False)
    es = contextlib.ExitStack()
    K = KB(nc, es)
    d = {}
    for name, shape in [("xb", [SEQ, D]), ("xo", [NOWN, D]), ("xh", [256, D]), ("ctx", [LC, D]), ("c2", [128, 8, 2]),
                        ("ada_w", [D, 6 * D]), ("ada_b", [1, 6 * D]), ("ada_bT", [128, 48]),
                        ("gmixT", [128, 8]), ("gffnT", [128, 8]), ("w_in", [D, 1280]), ("w_out", [D, D]),
                        ("qg", [128, 64]), ("kg", [128, 64]), ("gw", [4, 128, 128]), ("bsc", [128, 4]),
                        ("cosb", [SEQ, 32]), ("sinb", [SEQ, 32]), ("coso", [NOWN, 32]), ("sino", [NOWN, 32]),
                        ("band", [128, 60, 128]), ("rw", [D, 16])]:
        d[name] = K.dram(name, shape)
    o_x1 = K.dram("x1", [NOWN, D], kind="ExternalOutput")
    o_aff = K.dram("aff", [NOWN, 16], kind="ExternalOutput")
    o_c1 = K.dram("ctx1", [LC, D], kind="ExternalOutput")
    o_affc = K.dram("affc", [LC, 16], kind="ExternalOutput")

    C = setup_consts(K)
    modT, gates, sil = modulation(K, C, d["c2"], d["ada_w"], d["ada_b"], d["ada_bT"],
                                  need_vec=(0, 1, 2, 3, 6, 7, 8, 9), need_gate=(4, 5))
    A1, SH1 = mod_cols(K, modT, d["gmixT"], 0, 2, "m1")
    A2, SH2 = mod_cols(K, modT, d["gffnT"], 6, 8, "m2")
    G1 = [gates[(2, 0)], gates[(2, 1)]]
    K.free(sil)

    Win = K.alloc("Win", [128, 8, 1280], BF16, hi=True)
    K.dma("pool", Win[:, :, :], d["w_in"].v(lambda a: a.rearrange("(c p) n -> p c n", p=128)))
    QG = K.alloc("QG", [128, 64], F32)
    KG = K.alloc("KG", [128, 64], F32)
    K.dma("sp", QG[:, :], d["qg"][:, :])
    K.dma("sp", KG[:, :], d["kg"][:, :])
    band = K.alloc("band", [128, 60, 128], BF16, hi=True)
    K.dma("pool", band[:, :, :], d["band"][:, :, :])
    gw = K.alloc("gw", [128, 4, 128], BF16, hi=True)
    K.dma("pool", gw[:, :, :], d["gw"].v(lambda a: a.rearrange("g p n -> p g n")))
    bsc = K.alloc("bsc", [128, 4], F32)
    K.dma("sp", bsc[:, :], d["bsc"][:, :])
    coso = K.alloc("coso", [128, NT, 32], F32, hi=True)
    sino = K.alloc("sino", [128, NT, 32], F32, hi=True)
    K.dma("sp", coso[:, :, :], d["coso"].v(lambda a: a.rearrange("(j p) n -> p j n", p=128)))
    K.dma("sp", sino[:, :, :], d["sino"].v(lambda a: a.rearrange("(j p) n -> p j n", p=128)))

    qT = K.alloc("qT", [128, 8, NOWN], BF16)
    qTc = K.alloc("qTc", [128, 8, LC], BF16)
    K.pool("memset", qT[64:128, :, :], 0.0)
    K.pool("memset", qTc[64:128, :, :], 0.0)
    utok = K.alloc("utok", [128, 20, 512], BF16, hi=True)
    xt = [K.alloc("xt%d" % i, [128, 1024], F32) for i in range(2)]
    fr = Front(K, C)
    tmp = {"sq": K.alloc("sq", [128, 512], F32, hi=True), "ssh": K.alloc("ssh", [128, 24], F32, hi=True), "qn": K.alloc("qn", [128, 512], F32, hi=True),
           "t1": K.alloc("t1", [128, 256], F32, hi=True), "t2": K.alloc("t2", [128, 256], F32, hi=True)}
    qr = K.alloc("qr", [128, 512], F32, hi=True)
    trb = [K.banks[0], K.banks[1]]
    nx = 0

    passB = [("halo", 0), ("halo", 1)] + [("own", j) for j in range(NT)] + [("ctx", 0), ("ctx", 1)]
    def pB_front(n_, kind, j):
        nonlocal nx
        x_t = xt[nx % 2]
        nx += 1
        src = {"halo": d["xh"], "own": d["xo"], "ctx": d["ctx"]}[kind]
        K.dma("sp", x_t[:, :], src[j * 128:(j + 1) * 128, :])
        hT = fr.run(x_t[:, :], A1, SH1, 1 if kind == "ctx" else 0, trb)
        ub, qb = K.banks[3 + 4 * (n_ % 2)], K.banks[2 + 4 * (n_ % 2)]
        for dc in range(8):
            K.pe("matmul", ub[:, :], hT[:, dc, :], Win[:, dc, 768:1280], start=(dc == 0), stop=(dc == 7))
        if kind != "halo":
            for dc in range(8):
                K.pe("matmul", qb[:, :], hT[:, dc, :], Win[:, dc, 0:512], start=(dc == 0), stop=(dc == 7))

    def pB_back(n_, kind, j):
        ub, qb = K.banks[3 + 4 * (n_ % 2)], K.banks[2 + 4 * (n_ % 2)]
        ui = {"halo": 17 * j, "own": 1 + j, "ctx": 18 + j}[kind]
        K.act("activation", utok[:, ui, :], ub[:, :], AF.Copy)
        if kind == "halo":
            return
        if kind == "own":
            head_norm_rope(K, qb[:, :], 8, QG, coso[:, j, :], sino[:, j, :], qr, tmp)
            dst, off = qT, j * 128
        else:
            head_norm_rope(K, qb[:, :], 8, QG, None, None, qr, tmp)
            dst, off = qTc, j * 128
        for h in range(8):
            bk = K.banks[4 + h // 4]
            K.pe("transpose", bk[0:64, (h % 4) * 128:(h % 4 + 1) * 128], qr[:, h * 64:(h + 1) * 64], C.ident[:, :])
        for half in range(2):
            bk = K.banks[4 + half]
            K.act("activation", dst[0:64, half * 4:half * 4 + 4, off:off + 128],
                  bk.v(lambda a: a[0:64, :].rearrange("p (h t) -> p h t", t=128)), AF.Copy)
    pB_front(0, *passB[0])
    for n_, (kind, j) in enumerate(passB):
        if n_ + 1 < len(passB):
            pB_front(n_ + 1, *passB[n_ + 1])
        pB_back(n_, kind, j)

    bT = K.alloc("bT", [128, 4, NOWN], BF16)
    bTc = K.alloc("bTc", [128, 4, LC], BF16)
    dT = [K.alloc("dT%d" % i, [128, 128], BF16, hi=True) for i in range(2)]
    nd = 0
    jobs = [("own", j) for j in range(NT)] + [("ctx", 0), ("ctx", 1)]
    for kind, j in jobs:
        for g in range(4):
            if kind == "own":
                base = 12 if j == 0 else (24 if j == NT - 1 else 0)
                srcs = [(j + s, base + g * 3 + s) for s in range(3)]
                dst, off = bT, j * 128
            else:
                if j == 0:
                    srcs = [(18, 36 + g * 3 + 1), (19, 36 + g * 3 + 2)]
                else:
                    srcs = [(18, 48 + g * 3 + 0), (19, 48 + g * 3 + 1)]
                dst, off = bTc, j * 128
            pb = K.banks[6]
            for n, (ui, bi) in enumerate(srcs):
                K.pe("matmul", pb[:, 0:128], utok[:, ui, g * 128:(g + 1) * 128], band[:, bi, :],
                     start=(n == 0), stop=(n == len(srcs) - 1))
            dt_ = dT[nd % 2]
            nd += 1
            K.dve("tensor_copy", dt_[:, :], pb[:, 0:128])
            yb = K.banks[7]
            K.pe("matmul", yb[:, 0:128], gw[:, g, :], dt_[:, :], start=True, stop=True)
            K.act("activation", dst[:, g, off:off + 128], yb[:, 0:128], AF.Copy, scale=bsc[:, g:g + 1])
    K.free(utok, band, gw, dT[0], dT[1], qr)

    NKT = 2 + SEQ // 128
    kT = K.alloc("kT", [128, 2, NKT * 128], BF16)
    K.pool("memset", kT[64:128, :, :], 0.0)
    Vaug = K.alloc("Vaug", [128, NKT, 2, 65], BF16)
    K.pool("memset", Vaug.v(lambda a: a[:, :, :, 64:65]), 1.0)
    cosb = K.alloc("cosb", [128, SEQ // 128, 32], F32, hi=True)
    sinb = K.alloc("sinb", [128, SEQ // 128, 32], F32, hi=True)
    K.dma("sp", cosb[:, :, :], d["cosb"].v(lambda a: a.rearrange("(j p) n -> p j n", p=128)))
    K.dma("sp", sinb[:, :, :], d["sinb"].v(lambda a: a.rearrange("(j p) n -> p j n", p=128)))
    kr = K.alloc("kr", [128, 128], F32, hi=True)
    def passA_front(kt):
        nonlocal nx
        x_t = xt[nx % 2]
        nx += 1
        if kt < 2:
            K.dma("sp", x_t[:, :], d["ctx"][kt * 128:(kt + 1) * 128, :])
        else:
            K.dma("sp", x_t[:, :], d["xb"][(kt - 2) * 128:(kt - 1) * 128, :])
        hT = fr.run(x_t[:, :], A1, SH1, 1 if kt < 2 else 0, trb)
        kvb = K.banks[2 + kt % 2]
        for dc in range(8):
            K.pe("matmul", kvb[:, 0:256], hT[:, dc, :], Win[:, dc, 512:768], start=(dc == 0), stop=(dc == 7))

    def passA_back(kt):
        kvb = K.banks[2 + kt % 2]
        if kt < 2:
            head_norm_rope(K, kvb[:, 0:128], 2, KG, None, None, kr, tmp)
        else:
            head_norm_rope(K, kvb[:, 0:128], 2, KG, cosb[:, kt - 2, :], sinb[:, kt - 2, :], kr, tmp)
        K.act("activation", Vaug.v(lambda a: a[:, kt, :, 0:64]),
              kvb.v(lambda a: a[:, 128:256].rearrange("p (h d) -> p h d", d=64)), AF.Copy)
        tb = K.banks[4 + kt % 2]
        for h in range(2):
            K.pe("transpose", tb[0:64, h * 128:(h + 1) * 128], kr[:, h * 64:(h + 1) * 64], C.ident[:, :])
        K.dve("tensor_copy", kT[0:64, :, kt * 128:(kt + 1) * 128],
              tb.v(lambda a: a[0:64, 0:256].rearrange("p (h t) -> p h t", t=128)))
    passA_front(0)
    for kt in range(NKT):
        if kt + 1 < NKT:
            passA_front(kt + 1)
        passA_back(kt)
    K.free(cosb, sinb, kr, Win, coso, sino, *tmp.values())

    aT, aTc = qT, qTc
    PT = [K.alloc("PT%d" % i, [128, 512], BF16) for i in range(3)]
    osb = K.alloc("osb", [64, 512], F32)
    rden = K.alloc("rden", [65, 512], F32)
    attention(K, C, qTc, LC, LC, [0, 1], kT, Vaug, aTc, PT, osb, rden, 0.125)
    attention(K, C, qT, NOWN, 512, list(range(NKT)), kT, Vaug, aT, PT, osb, rden, 0.125)
    K.free(kT, Vaug, PT[0], PT[1], PT[2], osb, rden)

    WoA = K.alloc("WoA", [128, 8, D], BF16)
    K.pool("memset", WoA[64:128, :, :], 0.0)
    WoB = K.alloc("WoB", [128, 4, D], BF16)
    K.dma("pool", WoA[0:64, :, :], d["w_out"].v(lambda a: a[0:512, :].rearrange("(h p) n -> p h n", p=64)))
    K.dma("pool", WoB[:, :, :], d["w_out"].v(lambda a: a[512:1024, :].rearrange("(g p) n -> p g n", p=128)))
    rw = K.alloc("rw", [128, 8, 16], F32)
    K.dma("sp", rw[:, :, :], d["rw"].v(lambda a: a.rearrange("(c p) n -> p c n", p=128)))
    fr32 = Front(K, C, out_dt=F32, name="fr32")
    x1 = [K.alloc("x1_%d" % i, [128, 1024], F32) for i in range(2)]
    tg = K.alloc("tg", [128, 1024], F32)
    affo = K.alloc("affo", [128, NT + 2, 16], F32)
    smt = K.alloc("smt", [128, 24], F32)
    jobs = [("ctx", 0), ("ctx", 1)] + [("own", j) for j in range(NT)]
    tg2 = [tg, K.alloc("tg2", [128, 1024], F32)]

    def op_a(n, kind, j):
        nonlocal nx
        x_t = xt[nx % 2]
        nx += 1
        src, a_, b_, col, outd = (d["xo"], aT, bT, 0, o_x1) if kind == "own" else (d["ctx"], aTc, bTc, 1, o_c1)
        K.dma("sp", x_t[:, :], src[j * 128:(j + 1) * 128, :])
        x1t = x1[n % 2]
        tg_ = tg2[n % 2]
        for half in range(2):
            mb = K.banks[2 + 4 * (n % 2) + half]
            for h in range(8):
                K.pe("matmul", mb[:, :], a_[:, h, j * 128:(j + 1) * 128], WoA[:, h, half * 512:(half + 1) * 512],
                     start=(h == 0), stop=False)
            for g in range(4):
                K.pe("matmul", mb[:, :], b_[:, g, j * 128:(j + 1) * 128], WoB[:, g, half * 512:(half + 1) * 512],
                     start=False, stop=(g == 3))
            K.dve("tensor_tensor", tg_[:, half * 512:(half + 1) * 512], mb[:, :], G1[col][:, half * 512:(half + 1) * 512], ALU.mult)
        K.pool("tensor_tensor", x1t[:, :], tg_[:, :], x_t[:, :], ALU.add)
        K.dma("sp", outd[j * 128:(j + 1) * 128, :], x1t[:, :], is_out=True)

    def op_b(n, kind, j):
        col = 0 if kind == "own" else 1
        x1t = x1[n % 2]
        hT = fr32.run(x1t[:, :], A2, SH2, col, trb)
        lb = K.banks[4 + n % 2]
        for dc in range(8):
            K.pe("matmul", lb[:, 0:16], hT[:, dc, :], rw[:, dc, :], start=(dc == 0), stop=(dc == 7))
        ai = (NT + j) if kind == "ctx" else j
        softmax16(K, lb[:, 0:16], affo[:, ai, :], smt)
    op_a(0, *jobs[0])
    for n, (kind, j) in enumerate(jobs):
        if n + 1 < len(jobs):
            op_a(n + 1, *jobs[n + 1])
        op_b(n, kind, j)
    K.dma("sp", o_aff.v(lambda a: a.rearrange("(j p) e -> p j e", p=128)), affo[:, 0:NT, :], is_out=True)
    K.dma("sp", o_affc.v(lambda a: a.rearrange("(j p) e -> p j e", p=128)), affo[:, NT:NT + 2, :], is_out=True)
    K.finish()
    return nc, es


def rope_tables():
    rows = SEQ // 64
    row = np.repeat(np.arange(rows, dtype=np.float32), 64)
    col = np.tile(np.arange(64, dtype=np.float32), rows)
    freqs = (np.float32(10000.0) ** (-np.arange(16, dtype=np.float32) / np.float32(16))).astype(np.float32)
    ang = np.concatenate([row[:, None] * freqs, col[:, None] * freqs], axis=-1).astype(np.float32)
    return np.cos(ang).astype(np.float32), np.sin(ang).astype(np.float32)


def band_mats(T0, L):
    out = np.zeros((4, 3, 128, 128), np.float32)
    for g, w in enumerate((2, 4, 8, 16)):
        for tl in range(128):
            t = T0 + tl
            lo = min(max(t - w // 2, 0), L)
            hi = min(max(t - w // 2 + w, 0), L)
            for s in range(lo, hi):
                o = (s - T0) // 128 + 1
                out[g, o, (s - T0) % 128, tl] += 1.0 / (hi - lo)
            out[g, 1, tl, tl] -= 1.0
    return out


def fm(v):
    return np.ascontiguousarray(v.reshape(8, 128).T)


def prep_stage1(inp):
    cos, sin = rope_tables()
    mid = band_mats(1280, SEQ)
    first = band_mats(0, SEQ)
    last = band_mats(SEQ - 128, SEQ)
    c0 = band_mats(0, LC)
    c1 = band_mats(128, LC)
    maps = []
    for core in range(NCORES):
        b, r = core // 4, core % 4
        t0 = r * NOWN
        x = inp["x"][b]
        xh = np.zeros((256, D), np.float32)
        if r > 0:
            xh[0:128] = x[t0 - 128:t0]
        if r < 3:
            xh[128:256] = x[t0 + NOWN:t0 + NOWN + 128]
        bands = np.concatenate([mid.reshape(12, 128, 128), (first if r == 0 else mid).reshape(12, 128, 128),
                                (last if r == 3 else mid).reshape(12, 128, 128), c0.reshape(12, 128, 128),
                                c1.reshape(12, 128, 128)], axis=0)
        m = {
            "xb": x, "xo": x[t0:t0 + NOWN], "xh": xh, "ctx": inp["ctx"][b],
            "c2": np.stack([fm(inp["c"][b]), fm(inp["c_ctx"])], axis=-1),
            "ada_w": inp["ada_w"][0], "ada_b": inp["ada_b"][0][None, :],
            "ada_bT": np.ascontiguousarray(inp["ada_b"][0].reshape(48, 128).T),
            "gmixT": fm(inp["norm_mix_g"][0]), "gffnT": fm(inp["norm_ffn_g"][0]),
            "w_in": inp["ab_w_in"][0], "w_out": inp["ab_w_out"][0],
            "qg": np.broadcast_to(inp["a_q_norm_g"][0][None, :], (128, 64)),
            "kg": np.broadcast_to(inp["a_k_norm_g"][0][None, :], (128, 64)),
            "gw": inp["b_group_w"][0], "bsc": np.ascontiguousarray(inp["b_scale"][0].reshape(4, 128).T),
            "cosb": cos, "sinb": sin, "coso": cos[t0:t0 + NOWN], "sino": sin[t0:t0 + NOWN],
            "band": bands.transpose(1, 0, 2), "rw": inp["moe_router"][0],
        }
        maps.append({k: np.ascontiguousarray(v, dtype=np.float32) for k, v in m.items()})
    return maps


GCAP = 96
CCAP = 32
NSLOT = 4 * GCAP + CCAP


def build_moe(has_ctx, final):
    nc = bass.Bass("TRN2", target_bir_lowering=False)
    es = contextlib.ExitStack()
    K = KB(nc, es, arena_bytes=206 * 1024)
    d = {}
    ins = [("x1", [NOWN, D]), ("affb", [SEQ, 16]), ("affo", [NOWN, 16]), ("c2", [128, 8, 2]),
           ("ada_w", [D, 6 * D]), ("ada_b", [1, 6 * D]), ("ada_bT", [128, 48]), ("gffnT", [128, 8]),
           ("wg", [16, D, 2 * D]), ("wu", [16, D, 2 * D]), ("wd", [16, 2 * D, D])]
    if has_ctx:
        ins += [("ctx1", [LC, D]), ("affc", [LC, 16])]
    if final:
        ins += [("fg", [128, D])]
    for name, shape in ins:
        d[name] = K.dram(name, shape)
    o_x2 = K.dram("x2", [NOWN, D], kind="ExternalOutput")
    o_c2 = K.dram("ctx2", [LC, D], kind="ExternalOutput") if has_ctx else None
    NTL = NT + (2 if has_ctx else 0)
    NE2 = 32 if has_ctx else 16

    C = setup_consts(K)
    mres = []
    mgen = modulation_gen(K, C, d["c2"], d["ada_w"], d["ada_b"], d["ada_bT"], (6, 7, 8, 9), (10, 11), mres)
    next(mgen, None)

    X = [K.alloc("X%d" % j, [128, D], F32) for j in range(NTL)]
    H2 = [K.alloc("H2_%d" % j, [128, D], BF16) for j in range(NTL)]
    junk = K.alloc("junk", [128, D], F32, hi=True)
    ss = K.alloc("ss", [128, NTL, 4], F32)

    def load_tile(j):
        src = d["x1"][j * 128:(j + 1) * 128, :] if j < NT else d["ctx1"][(j - NT) * 128:(j - NT + 1) * 128, :]
        K.dma("sp", X[j][:, :], src)
        K.act("activation", junk[:, :], X[j][:, :], AF.Square, accum_out=ss[:, j, 0:1])
        K.act("activation", ss[:, j, 1:2], ss[:, j, 0:1], AF.Sqrt, bias=EPS, scale=1.0 / D)
        K.dve("reciprocal", ss[:, j, 2:3], ss[:, j, 1:2])
        K.act("activation", H2[j][:, :], X[j][:, :], AF.Copy, scale=ss[:, j, 2:3])

    affb = K.alloc("affb", [128, SEQ // 128, 16], F32, hi=True)
    K.dma("sp", affb[:, :, :], d["affb"].v(lambda a: a.rearrange("(j p) e -> p j e", p=128)))
    affo = K.alloc("affo", [128, NTL, 16], F32)
    K.dma("sp", affo[:, 0:NT, :], d["affo"].v(lambda a: a.rearrange("(j p) e -> p j e", p=128)))
    if has_ctx:
        K.dma("sp", affo[:, NT:NTL, :], d["affc"].v(lambda a: a.rearrange("(j p) e -> p j e", p=128)))
    lo = K.alloc("lo", [128, NE2], F32)
    capt = K.alloc("capt", [128, NE2], F32, hi=True)
    mid = K.alloc("mid", [128, NE2], F32, hi=True)
    cnt = K.alloc("cnt", [128, NE2], F32, hi=True)
    gtmp = K.alloc("gtmp", [128, NE2], F32, hi=True)
    mask = K.alloc("mask", [128, SEQ // 128 + 2, 16], F32, hi=True)
    K.dve("memset", lo[:, :], 0.0)
    K.dve("memset", capt[:, 0:16], float(2 * SEQ // 16))
    if has_ctx:
        K.dve("memset", capt[:, 16:32], float(2 * LC // 16))
    tb = K.banks[0]
    NJ = SEQ // 128
    for it in range(30):
        K.dve("tensor_scalar", mid[:, :], lo[:, :], float(2.0 ** -(it + 1)), None, ALU.add)
        K.dve("tensor_tensor", mask[:, 0:NJ, :], affb[:, :, :],
              mid.v(lambda a: a[:, 0:16].unsqueeze(1).to_broadcast([128, NJ, 16])), ALU.is_ge)
        K.dve("tensor_reduce", cnt[:, 0:16], mask.v(lambda a: a[:, 0:NJ, :].rearrange("p j e -> p e j")), AX.X, ALU.add)
        if has_ctx:
            K.dve("tensor_tensor", mask[:, NJ:NJ + 2, :], affo[:, NT:NTL, :],
                  mid.v(lambda a: a[:, 16:32].unsqueeze(1).to_broadcast([128, 2, 16])), ALU.is_ge)
            K.dve("tensor_reduce", cnt[:, 16:32], mask.v(lambda a: a[:, NJ:NJ + 2, :].rearrange("p j e -> p e j")), AX.X, ALU.add)
        K.pe("matmul", tb[:, 0:NE2], C.ones[:, :], cnt[:, :], start=True, stop=True)
        K.dve("tensor_tensor", gtmp[:, :], tb[:, 0:NE2], capt[:, :], ALU.is_ge)
        K.dve("tensor_tensor", gtmp[:, :], gtmp[:, :], mid[:, :], ALU.mult)
        K.dve("tensor_tensor", lo[:, :], lo[:, :], gtmp[:, :], ALU.max)
        if it < NTL:
            load_tile(it)
        if it % 3 == 1:
            next(mgen, None)
    for _ in mgen:
        pass
    modT, gates, sil = mres
    A2, SH2 = mod_cols(K, modT, d["gffnT"], 6, 8, "m2")
    G2 = [gates[(5, 0)], gates[(5, 1)]]
    K.free(sil, modT)
    if not has_ctx:
        K.free(G2[1])
    K.free(affb, capt, mid, cnt, gtmp, mask, junk)

    sel = K.alloc("sel", [128, NTL, 16], F32)
    gate = K.alloc("gate", [128, NTL, 16], F32)
    slot = K.alloc("slot", [128, NTL, 16], F32)
    offs = K.alloc("offs", [128, NTL, 16], F32)
    Lm = K.alloc("Lm", [128, 128], F32)
    K.pool("memset", Lm[:, :], 1.0)
    K.pool("affine_select", Lm[:, :], Lm[:, :], [[1, 128]], ALU.is_gt, 0.0, base=0, channel_multiplier=-1)
    iota = K.alloc("iota", [128, GCAP], F32)
    K.pool("iota", iota[:, :], [[1, GCAP]], base=0, channel_multiplier=0, allow_small_or_imprecise_dtypes=True)
    K.dve("tensor_tensor", sel[:, 0:NT, :], affo[:, 0:NT, :],
          lo.v(lambda a: a[:, 0:16].unsqueeze(1).to_broadcast([128, NT, 16])), ALU.is_ge)
    if has_ctx:
        K.dve("tensor_tensor", sel[:, NT:NTL, :], affo[:, NT:NTL, :],
              lo.v(lambda a: a[:, 16:32].unsqueeze(1).to_broadcast([128, 2, 16])), ALU.is_ge)
    K.dve("tensor_tensor", gate[:, :, :], affo[:, :, :], sel[:, :, :], ALU.mult)
    pb, tb2 = K.banks[1], K.banks[2]
    K.pe("matmul", pb[:, 0:NTL * 16], Lm[:, :], sel.v(lambda a: a.rearrange("p j e -> p (j e)")), start=True, stop=True)
    K.pe("matmul", tb2[:, 0:NTL * 16], C.ones[:, :], sel.v(lambda a: a.rearrange("p j e -> p (j e)")), start=True, stop=True)
    K.dve("memset", offs[:, :, :], 0.0)
    for j in range(NTL):
        first = (j % 4 == 0) if j < NT else (j == NT)
        if first:
            continue
        K.dve("tensor_tensor", offs[:, j, :], offs[:, j - 1, :], tb2[:, (j - 1) * 16:j * 16], ALU.add)
    K.dve("tensor_tensor", slot.v(lambda a: a.rearrange("p j e -> p (j e)")), offs.v(lambda a: a.rearrange("p j e -> p (j e)")),
          pb[:, 0:NTL * 16], ALU.add)
    K.dve("tensor_scalar", slot[:, :, :], slot[:, :, :], 1.0, None, ALU.add)
    K.dve("tensor_tensor", slot[:, :, :], slot[:, :, :], sel[:, :, :], ALU.mult)
    K.dve("tensor_scalar", slot[:, :, :], slot[:, :, :], -1.0, None, ALU.add)
    K.free(sel, offs, Lm, affo, lo)

    S = K.alloc("S", [128, NTL, GCAP], BF16)
    Sg = K.alloc("Sg", [128, NTL, GCAP], BF16)
    ST = K.alloc("ST", [GCAP, NTL, 128], BF16)
    xsT = K.alloc("xsT", [128, 8, NSLOT], BF16)
    actT = K.alloc("actT", [128, 16, NSLOT], BF16)
    silt = [K.alloc("silt%d" % i, [128, NSLOT], F32) for i in range(2)]
    ysb = [K.alloc("ysb%d" % i, [GCAP, D], BF16) for i in range(5 if has_ctx else 4)]
    Wg = [K.alloc("Wg%d" % i, [128, 8, 256], BF16) for i in range(2)]
    Wu = [K.alloc("Wu%d" % i, [128, 8, 256], BF16) for i in range(2)]
    Wd = [K.alloc("Wd%d" % i, [128, 16, 256], BF16) for i in range(2)]
    groups = [(g, list(range(4 * g, 4 * g + 4)), g * GCAP, GCAP, 0) for g in range(4)]
    if has_ctx:
        groups.append((4, [NT, NT + 1], 4 * GCAP, CCAP, 1))
    NS = 4 * GCAP + (CCAP if has_ctx else 0)
    nw = 0
    nd = 0
    st_ = {"nw": 0, "nd": 0}

    def build_S(e):
        K.dve("tensor_tensor", S[:, :, :], iota.v(lambda a: a[:, :].unsqueeze(1).to_broadcast([128, NTL, GCAP])),
              slot.v(lambda a: a[:, :, e:e + 1].to_broadcast([128, NTL, GCAP])), ALU.is_equal)
        K.dve("tensor_tensor", Sg[:, :, :], S[:, :, :],
              gate.v(lambda a: a[:, :, e:e + 1].to_broadcast([128, NTL, GCAP])), ALU.mult)

    def gather(e):
        for dc in range(8):
            xb_ = K.banks[dc % 2]
            for (g, tiles, c0, ncol, col) in groups:
                for n, j in enumerate(tiles):
                    K.pe("matmul", xb_[:, c0:c0 + ncol], H2[j][:, dc * 128:(dc + 1) * 128], S[:, j, 0:ncol],
                         start=(n == 0), stop=(n == len(tiles) - 1))
            K.dve("tensor_scalar", xsT[:, dc, 0:4 * GCAP], xb_[:, 0:4 * GCAP], A2[:, dc, 0:1], SH2[:, dc, 0:1], ALU.mult, ALU.add)
            if has_ctx:
                K.dve("tensor_scalar", xsT[:, dc, 4 * GCAP:NS], xb_[:, 4 * GCAP:NS], A2[:, dc, 1:2], SH2[:, dc, 1:2], ALU.mult, ALU.add)

    def st_transposes(e):
        stb_t = K.banks[6]
        for j0 in range(0, NTL, 8):
            js = list(range(j0, min(j0 + 8, NTL)))
            for n, j in enumerate(js):
                o_ap = stb_t.v(lambda a: a.bitcast(BF16)[0:GCAP, n * 128:(n + 1) * 128])
                K.pe("transpose", o_ap, Sg[:, j, :], C.identb[:, :])
            K.act("activation", ST[:, j0:j0 + len(js), :],
                  stb_t.v(lambda a: a.bitcast(BF16)[0:GCAP, 0:len(js) * 128].rearrange("p (j t) -> p j t", t=128)), AF.Copy)

    def gate_up(e, hook=None):
        for q in range(8):
            wg_, wu_ = Wg[st_["nw"] % 2], Wu[st_["nw"] % 2]
            st_["nw"] += 1
            K.dma("pool", wg_[:, :, :], d["wg"].v(lambda a: a[e].rearrange("(c p) n -> p c n", p=128)[:, :, q * 256:(q + 1) * 256]))
            K.dma("pool", wu_[:, :, :], d["wu"].v(lambda a: a[e].rearrange("(c p) n -> p c n", p=128)[:, :, q * 256:(q + 1) * 256]))
            for f2 in range(2):
                fc = q * 2 + f2
                ab, ub = K.banks[2 + fc % 2], K.banks[4 + fc % 2]
                for dc in range(8):
                    K.pe("matmul", ab[:, 0:NS], wg_[:, dc, f2 * 128:(f2 + 1) * 128], xsT[:, dc, 0:NS], start=(dc == 0), stop=(dc == 7))
                for dc in range(8):
                    K.pe("matmul", ub[:, 0:NS], wu_[:, dc, f2 * 128:(f2 + 1) * 128], xsT[:, dc, 0:NS], start=(dc == 0), stop=(dc == 7))
                sl_ = silt[fc % 2]
                K.act("activation", sl_[:, 0:NS], ab[:, 0:NS], AF.Silu)
                K.dve("tensor_tensor", actT[:, fc, 0:NS], sl_[:, 0:NS], ub[:, 0:NS], ALU.mult)
            if q == 0 and hook is not None:
                hook()

    def down(e):
        for dq in range(4):
            wd_ = Wd[st_["nd"] % 2]
            st_["nd"] += 1
            K.dma("pool", wd_[:, :, :], d["wd"].v(lambda a: a[e].rearrange("(c p) n -> p c n", p=128)[:, :, dq * 256:(dq + 1) * 256]))
            for (g, tiles, c0, ncol, col) in groups:
                yb = K.banks[6 + g % 2]
                for fc in range(16):
                    K.pe("matmul", yb[0:ncol, 0:256], actT[:, fc, c0:c0 + ncol], wd_[:, fc, :], start=(fc == 0), stop=(fc == 15))
                K.dve("tensor_tensor", ysb[g][0:ncol, dq * 256:(dq + 1) * 256], yb[0:ncol, 0:256],
                      G2[col][0:ncol, dq * 256:(dq + 1) * 256], ALU.mult)

    def scatter(e):
        for (g, tiles, c0, ncol, col) in groups:
            for j in tiles:
                for half in range(2):
                    ob = K.banks[2 + (j % 2) * 2 + half]
                    K.pe("matmul", ob[:, :], ST[0:ncol, j, :], ysb[g][0:ncol, half * 512:(half + 1) * 512], start=True, stop=True)
                    K.dve("tensor_tensor", X[j][:, half * 512:(half + 1) * 512], X[j][:, half * 512:(half + 1) * 512], ob[:, :], ALU.add)

    build_S(0)
    gather(0)
    st_transposes(0)
    for e in range(16):
        gate_up(e, hook=(lambda e=e: build_S(e + 1)) if e + 1 < 16 else None)
        if e + 1 < 16:
            gather(e + 1)
        down(e)
        scatter(e)
        if e + 1 < 16:
            st_transposes(e + 1)

    if final:
        fg = K.alloc("fg", [128, D], F32, hi=True)
        K.dma("sp", fg[:, :], d["fg"][:, :])
        junk = K.alloc("junk2", [128, D], F32, hi=True)
    for j in range(NTL):
        dst = o_x2[j * 128:(j + 1) * 128, :] if j < NT else o_c2[(j - NT) * 128:(j - NT + 1) * 128, :]
        if final:
            K.act("activation", junk[:, :], X[j][:, :], AF.Square, accum_out=ss[:, j, 0:1])
            K.act("activation", ss[:, j, 1:2], ss[:, j, 0:1], AF.Sqrt, bias=EPS, scale=1.0 / D)
            K.dve("reciprocal", ss[:, j, 2:3], ss[:, j, 1:2])
            K.act("activation", X[j][:, :], X[j][:, :], AF.Copy, scale=ss[:, j, 2:3])
            K.dve("tensor_tensor", X[j][:, :], X[j][:, :], fg[:, :], ALU.mult)
        K.dma("sp", dst, X[j][:, :], is_out=True)
    K.finish()
    return nc, es


def prep_moe(inp, layer, x1, aff, ctx1=None, affc=None, final=False):
    maps = []
    for core in range(NCORES):
        b, r = core // 4, core % 4
        t0 = r * NOWN
        m = {
            "x1": x1[b, t0:t0 + NOWN], "affb": aff[b], "affo": aff[b, t0:t0 + NOWN],
            "c2": np.stack([fm(inp["c"][b]), fm(inp["c_ctx"])], axis=-1),
            "ada_w": inp["ada_w"][layer], "ada_b": inp["ada_b"][layer][None, :],
            "ada_bT": np.ascontiguousarray(inp["ada_b"][layer].reshape(48, 128).T),
            "gffnT": fm(inp["norm_ffn_g"][layer]),
            "wg": inp["moe_w_gate"][layer], "wu": inp["moe_w_up"][layer], "wd": inp["moe_w_down"][layer],
        }
        if ctx1 is not None:
            m["ctx1"] = ctx1[b]
            m["affc"] = affc[b]
        if final:
            m["fg"] = np.broadcast_to(inp["final_norm_g"][None, :], (128, D))
        maps.append({k: np.ascontiguousarray(v, dtype=np.float32) for k, v in m.items()})
    return maps


LN2 = 0.6931471805599453
STAGE3_DEBUG = False
NKB = 2 + SEQ // 128


def build_stage3():
    nc = bass.Bass("TRN2", target_bir_lowering=False)
    es = contextlib.ExitStack()
    K = KB(nc, es, arena_bytes=206 * 1024)
    d = {}
    for name, shape in [("xb", [SEQ, D]), ("xo", [NOWN, D]), ("xh", [256, D]), ("ctx", [LC, D]), ("c2", [128, 8, 2]),
                        ("ada_w", [D, 6 * D]), ("ada_b", [1, 6 * D]), ("ada_bT", [128, 48]),
                        ("gmixT", [128, 8]), ("gffnT", [128, 8]), ("w_in", [D, 2304]), ("w_out", [D, D]),
                        ("dec", [128, 8]), ("gng", [128, 512]), ("sink", [128, 8]),
                        ("cosb", [SEQ, 32]), ("sinb", [SEQ, 32]), ("coso", [NOWN, 32]), ("sino", [NOWN, 32]),
                        ("cosh", [256, 32]), ("sinh", [256, 32]),
                        ("EF", [128, NKB]), ("EB", [128, NKB]), ("MF", [128, NKB]), ("MB", [128, NKB]),
                        ("wm", [128, 3, 384]), ("rw", [D, 16])]:
        d[name] = K.dram(name, shape)
    o_x3 = K.dram("x3", [NOWN, D], kind="ExternalOutput")
    o_aff = K.dram("aff", [NOWN, 16], kind="ExternalOutput")
    DBG = STAGE3_DEBUG
    if DBG:
        o_cat = K.dram("dbg_cat", [NOWN, D], kind="ExternalOutput")
        o_rf = K.dram("dbg_rf", [64, 1024], kind="ExternalOutput")
        o_z = K.dram("dbg_z", [128, 2 * NKB * 4], kind="ExternalOutput")

    C = setup_consts(K)
    modT, gates, sil = modulation(K, C, d["c2"], d["ada_w"], d["ada_b"], d["ada_bT"],
                                  need_vec=(0, 1, 2, 3, 6, 7, 8, 9), need_gate=(4, 5))
    A1, SH1 = mod_cols(K, modT, d["gmixT"], 0, 2, "m1")
    A2, SH2 = mod_cols(K, modT, d["gffnT"], 6, 8, "m2")
    G1 = gates[(2, 0)]
    K.free(sil, modT, gates[(2, 1)])

    dec = K.alloc("dec", [128, 8], F32)
    K.dma("sp", dec[:, :], d["dec"][:, :])
    lgt = K.alloc("lgt", [128, 8], F32)
    K.act("activation", dec[:, :], dec[:, :], AF.Exp, scale=-LN2)
    K.act("activation", lgt[:, :], dec[:, :], AF.Ln, scale=-1.0, bias=1.0)
    gT = K.alloc("gT", [128, 8], F32)
    K.act("activation", gT[:, :], lgt[:, :], AF.Exp, scale=128.0)
    pcol = K.alloc("pcol", [128, 2], F32)
    K.pool("iota", pcol[:, 0:1], [[1, 1]], base=127, channel_multiplier=-1, allow_small_or_imprecise_dtypes=True)
    K.pool("iota", pcol[:, 1:2], [[1, 1]], base=0, channel_multiplier=1, allow_small_or_imprecise_dtypes=True)
    zloc = K.alloc("zloc", [128, 8], F32)
    for h in range(4):
        K.act("activation", zloc[:, h:h + 1], pcol[:, 0:1], AF.Exp, scale=lgt[:, h:h + 1])
        K.act("activation", zloc[:, 4 + h:5 + h], pcol[:, 1:2], AF.Exp, scale=lgt[:, 4 + h:5 + h])
    K.dve("tensor_scalar", zloc[:, :], zloc[:, :], 0.125, None, ALU.mult)
    diff = K.alloc("diff", [128, 128], F32, hi=True)
    K.pool("iota", diff[:, :], [[1, 128]], base=0, channel_multiplier=-1, allow_small_or_imprecise_dtypes=True)
    dpos = K.alloc("dpos", [128, 128], F32, hi=True)
    dneg = K.alloc("dneg", [128, 128], F32, hi=True)
    K.dve("tensor_scalar", dpos[:, :], diff[:, :], 0.0, None, ALU.max)
    K.dve("tensor_scalar", dneg[:, :], diff[:, :], -1.0, 0.0, ALU.mult, ALU.max)
    DmT = K.alloc("DmT", [128, 4, 128], F32)
    tmpd = K.alloc("tmpd", [128, 128], F32, hi=True)
    for h in range(4):
        K.dve("tensor_scalar", tmpd[:, :], dpos[:, :], lgt[:, h:h + 1], None, ALU.mult)
        K.dve("scalar_tensor_tensor", tmpd[:, :], dneg[:, :], lgt[:, 4 + h:5 + h], tmpd[:, :], ALU.mult, ALU.add)
        K.act("activation", DmT[:, h, :], tmpd[:, :], AF.Exp)
    trow = K.alloc("trow", [128, 2, 128], F32, hi=True)
    K.pool("iota", trow[:, 0, :], [[1, 128]], base=1, channel_multiplier=0, allow_small_or_imprecise_dtypes=True)
    K.pool("iota", trow[:, 1, :], [[-1, 128]], base=128, channel_multiplier=0, allow_small_or_imprecise_dtypes=True)
    Xi = K.alloc("Xi", [64, 2, 4, 128], F32)
    for h in range(4):
        K.act("activation", Xi[:, 0, h, :], trow[0:64, 0, :], AF.Exp, scale=lgt[0:64, h:h + 1])
        K.act("activation", Xi[:, 1, h, :], trow[0:64, 1, :], AF.Exp, scale=lgt[0:64, 4 + h:5 + h])
    K.free(diff, dpos, dneg, tmpd, trow)
    EF = K.alloc("EF", [128, 2, NKB], F32, hi=True)
    MF = K.alloc("MF", [128, 2, NKB], F32, hi=True)
    K.dma("sp", EF[:, 0, :], d["EF"][:, :])
    K.dma("sp", EF[:, 1, :], d["EB"][:, :])
    K.dma("sp", MF[:, 0, :], d["MF"][:, :])
    K.dma("sp", MF[:, 1, :], d["MB"][:, :])
    Z = K.alloc("Z", [128, 2, NKB, 4], F32, hi=True)
    for dr in range(2):
        for h in range(4):
            K.act("activation", Z[:, dr, :, h], EF[:, dr, :], AF.Exp, scale=lgt[:, dr * 4 + h:dr * 4 + h + 1])
        K.dve("scalar_tensor_tensor", Z[:, dr, :, :], Z[:, dr, :, :], 0.125,
              MF.v(lambda a: a[:, dr, :].unsqueeze(2).to_broadcast([128, NKB, 4])), ALU.mult, ALU.mult)
    K.free(EF, MF)
    if DBG:
        K.dma("sp", o_z[:, :], Z.v(lambda a: a.rearrange("p a b c -> p (a b c)")), is_out=True)

    Wkv = K.alloc("Wkv", [128, 8, 1024], BF16, hi=True)
    K.dma("pool", Wkv[:, :, 0:768], d["w_in"].v(lambda a: a.rearrange("(c p) n -> p c n", p=128)[:, :, 256:1024]))
    K.dma("pool", Wkv[:, :, 768:1024], d["w_in"].v(lambda a: a.rearrange("(c p) n -> p c n", p=128)[:, :, 2048:2304]))
    cosb = K.alloc("cosb", [128, SEQ // 128, 32], F32, hi=True)
    sinb = K.alloc("sinb", [128, SEQ // 128, 32], F32, hi=True)
    K.dma("sp", cosb[:, :, :], d["cosb"].v(lambda a: a.rearrange("(j p) n -> p j n", p=128)))
    K.dma("sp", sinb[:, :, :], d["sinb"].v(lambda a: a.rearrange("(j p) n -> p j n", p=128)))
    xt = [K.alloc("xt%d" % i, [128, 1024], F32) for i in range(2)]
    fr = Front(K, C)
    tmp = {"qn": K.alloc("qn", [128, 512], F32, hi=True), "t1": K.alloc("t1", [128, 256], F32, hi=True),
           "t2": K.alloc("t2", [128, 256], F32, hi=True)}
    kr = K.alloc("kr", [128, 256], F32, hi=True)
    kz = [K.alloc("kz%d" % i, [128, 2, 4, 64], BF16, hi=True) for i in range(2)]
    Vt = [K.alloc("Vt%d" % i, [128, 512], BF16, hi=True) for i in range(2)]
    trb = [K.banks[0], K.banks[1]]
    RFp, RBp = K.banks[6], K.banks[7]
    nx = 0
    def p1_front(i):
        nonlocal nx
        x_t = xt[nx % 2]
        nx += 1
        if i < 2:
            K.dma("sp", x_t[:, :], d["ctx"][i * 128:(i + 1) * 128, :])
        else:
            K.dma("sp", x_t[:, :], d["xb"][(i - 2) * 128:(i - 1) * 128, :])
        hT = fr.run(x_t[:, :], A1, SH1, 1 if i < 2 else 0, trb)
        kb, vb = K.banks[2 + i % 2], K.banks[4 + i % 2]
        for dc in range(8):
            K.pe("matmul", kb[:, 0:256], hT[:, dc, :], Wkv[:, dc, 0:256], start=(dc == 0), stop=(dc == 7))
        for dc in range(8):
            K.pe("matmul", vb[:, :], hT[:, dc, :], Wkv[:, dc, 256:768], start=(dc == 0), stop=(dc == 7))

    def p1_back(i):
        kb, vb = K.banks[2 + i % 2], K.banks[4 + i % 2]
        if i < 2:
            head_norm_rope(K, kb[:, 0:256], 4, None, None, None, kr, tmp)
        else:
            head_norm_rope(K, kb[:, 0:256], 4, None, cosb[:, i - 2, :], sinb[:, i - 2, :], kr, tmp)
        kz_, vt_ = kz[i % 2], Vt[i % 2]
        for dr in range(2):
            K.dve("tensor_tensor", kz_[:, dr, :, :], kr.v(lambda a: a[:, :].rearrange("p (h d) -> p h d", d=64)),
                  Z.v(lambda a: a[:, dr, i, :].unsqueeze(2).to_broadcast([128, 4, 64])), ALU.mult)
        K.act("activation", vt_[:, :], vb[:, :], AF.Copy)
        for dr, bank in ((0, RFp), (1, RBp)):
            for h in range(4):
                K.pe("matmul", bank[0:64, h * 128:(h + 1) * 128], kz_[:, dr, h, :], vt_[:, h * 128:(h + 1) * 128],
                     start=(i == 0 and h == 0), stop=(i == NKB - 1 and h == 3))
    p1_front(0)
    for i in range(NKB):
        if i + 1 < NKB:
            p1_front(i + 1)
        p1_back(i)
    Rf = K.alloc("Rf", [64, 512], F32)
    Rb = K.alloc("Rb", [64, 512], F32)
    K.act("activation", Rf[:, :], RFp[0:64, :], AF.Copy)
    K.act("activation", Rb[:, :], RBp[0:64, :], AF.Copy)
    if DBG:
        K.dma("sp", o_rf[:, 0:512], Rf[:, :], is_out=True)
        K.dma("sp", o_rf[:, 512:1024], Rb[:, :], is_out=True)
    K.free(cosb, sinb, Z)

    coso = K.alloc("coso", [128, NT + 2, 32], F32)
    sino = K.alloc("sino", [128, NT + 2, 32], F32)
    K.dma("sp", coso[:, 0:NT, :], d["coso"].v(lambda a: a.rearrange("(j p) n -> p j n", p=128)))
    K.dma("sp", sino[:, 0:NT, :], d["sino"].v(lambda a: a.rearrange("(j p) n -> p j n", p=128)))
    K.dma("sp", coso[:, NT:NT + 2, :], d["cosh"].v(lambda a: a.rearrange("(j p) n -> p j n", p=128)))
    K.dma("sp", sino[:, NT:NT + 2, :], d["sinh"].v(lambda a: a.rearrange("(j p) n -> p j n", p=128)))
    kTr = K.alloc("kTr", [128, NT, 4, 128], BF16)
    K.pool("memset", kTr[64:128, :, :, :], 0.0)
    Vr = K.alloc("Vr", [128, NT, 512], BF16)
    kzb = K.alloc("kzb", [128, NT, 4, 64], BF16)
    Rfb = K.alloc("Rfb", [128, NT, 512], BF16)
    Rbb = K.alloc("Rbb", [128, NT, 512], BF16)
    K.pool("memset", Rfb[64:128, :, :], 0.0)
    K.pool("memset", Rbb[64:128, :, :], 0.0)
    kTd = K.alloc("kTd", [128, 2, 20 * 128], BF16)
    K.pool("memset", kTd[64:128, :, :], 0.0)
    Vd = K.alloc("Vd", [128, 20, 2, 64], BF16)
    kzf = [K.alloc("kzf%d" % i, [128, 4, 64], BF16, hi=True) for i in range(2)]
    krd = K.alloc("krd", [128, 128], F32, hi=True)
    sweep = [("halo", 0), ("halo", 1), ("ctx", 0), ("ctx", 1)] + [("own", n) for n in range(NT)]
    def p2_front(n_, kind, n):
        nonlocal nx
        x_t = xt[nx % 2]
        nx += 1
        src = {"halo": d["xh"], "own": d["xo"], "ctx": d["ctx"]}[kind]
        K.dma("sp", x_t[:, :], src[n * 128:(n + 1) * 128, :])
        hT = fr.run(x_t[:, :], A1, SH1, 1 if kind == "ctx" else 0, trb)
        ab = K.banks[2 if n_ % 2 == 0 else 7]
        if kind == "own":
            for dc in range(8):
                K.pe("matmul", ab[:, 0:256], hT[:, dc, :], Wkv[:, dc, 0:256], start=(dc == 0), stop=(dc == 7))
        for dc in range(8):
            K.pe("matmul", ab[:, 256:512], hT[:, dc, :], Wkv[:, dc, 768:1024], start=(dc == 0), stop=(dc == 7))
        if kind == "own":
            vb = K.banks[4 + n_ % 2]
            for dc in range(8):
                K.pe("matmul", vb[:, :], hT[:, dc, :], Wkv[:, dc, 256:768], start=(dc == 0), stop=(dc == 7))

    def p2_back(n_, kind, n):
        idx = {"halo": 17 * n, "own": 1 + n, "ctx": 18 + n}[kind]
        ci = {"halo": NT + n, "own": n, "ctx": None}[kind]
        ab = K.banks[2 if n_ % 2 == 0 else 7]
        if ci is None:
            head_norm_rope(K, ab[:, 256:384], 2, None, None, None, krd, tmp)
        else:
            head_norm_rope(K, ab[:, 256:384], 2, None, coso[:, ci, :], sino[:, ci, :], krd, tmp)
        K.act("activation", Vd[:, idx, :, :], ab.v(lambda a: a[:, 384:512].rearrange("p (h d) -> p h d", d=64)), AF.Copy)
        tb = K.banks[3]
        for h in range(2):
            K.pe("transpose", tb[0:64, h * 128:(h + 1) * 128], krd[:, h * 64:(h + 1) * 64], C.ident[:, :])
        K.dve("tensor_copy", kTd[0:64, :, idx * 128:(idx + 1) * 128],
              tb.v(lambda a: a[0:64, 0:256].rearrange("p (h t) -> p h t", t=128)))
        if kind != "own":
            return
        vb = K.banks[4 + n_ % 2]
        head_norm_rope(K, ab[:, 0:256], 4, None, coso[:, n, :], sino[:, n, :], kr, tmp)
        K.act("activation", Vr[:, n, :], vb[:, :], AF.Copy)
        kzf_ = kzf[n % 2]
        K.dve("tensor_tensor", kzf_[:, :, :], kr.v(lambda a: a[:, :].rearrange("p (h d) -> p h d", d=64)),
              zloc.v(lambda a: a[:, 0:4].unsqueeze(2).to_broadcast([128, 4, 64])), ALU.mult)
        K.dve("tensor_tensor", kzb[:, n, :, :], kr.v(lambda a: a[:, :].rearrange("p (h d) -> p h d", d=64)),
              zloc.v(lambda a: a[:, 4:8].unsqueeze(2).to_broadcast([128, 4, 64])), ALU.mult)
        tk = K.banks[3]
        for h in range(4):
            K.pe("transpose", tk[0:64, h * 128:(h + 1) * 128], kr[:, h * 64:(h + 1) * 64], C.ident[:, :])
        K.act("activation", kTr[0:64, n, :, :], tk.v(lambda a: a[0:64, :].rearrange("p (h t) -> p h t", t=128)), AF.Copy, scale=0.125)
        K.act("activation", Rfb[0:64, n, :], Rf[:, :], AF.Copy)
        sb_ = K.banks[6]
        for h in range(4):
            K.pe("matmul", sb_[0:64, h * 128:(h + 1) * 128], kzf_[:, h, :], Vr[:, n, h * 128:(h + 1) * 128], start=True, stop=True)
        for h in range(4):
            K.dve("scalar_tensor_tensor", Rf[:, h * 128:(h + 1) * 128], Rf[:, h * 128:(h + 1) * 128], gT[0:64, h:h + 1],
                  sb_[0:64, h * 128:(h + 1) * 128], ALU.mult, ALU.add)
    p2_front(0, *sweep[0])
    for n_, (kind, n) in enumerate(sweep):
        if n_ + 1 < len(sweep):
            p2_front(n_ + 1, *sweep[n_ + 1])
        p2_back(n_, kind, n)
    for n in range(NT - 1, -1, -1):
        K.act("activation", Rbb[0:64, n, :], Rb[:, :], AF.Copy)
        sb_ = K.banks[6 + n % 2]
        for h in range(4):
            K.pe("matmul", sb_[0:64, h * 128:(h + 1) * 128], kzb[:, n, h, :], Vr[:, n, h * 128:(h + 1) * 128], start=True, stop=True)
        for h in range(4):
            K.dve("scalar_tensor_tensor", Rb[:, h * 128:(h + 1) * 128], Rb[:, h * 128:(h + 1) * 128], gT[0:64, 4 + h:5 + h],
                  sb_[0:64, h * 128:(h + 1) * 128], ALU.mult, ALU.add)
    K.free(Wkv, kr, kz[0], kz[1], Vt[0], Vt[1], kzf[0], kzf[1], krd, kzb, Rf, Rb)

    Wq = K.alloc("Wq", [128, 8, 1280], BF16)
    K.dma("pool", Wq[:, :, 0:256], d["w_in"].v(lambda a: a.rearrange("(c p) n -> p c n", p=128)[:, :, 0:256]))
    K.dma("pool", Wq[:, :, 256:1280], d["w_in"].v(lambda a: a.rearrange("(c p) n -> p c n", p=128)[:, :, 1024:2048]))
    Wo = K.alloc("Wo", [128, 8, D], BF16)
    K.dma("pool", Wo[:, :, :], d["w_out"].v(lambda a: a.rearrange("(c p) n -> p c n", p=128)))
    gng = K.alloc("gng", [128, 512], F32)
    K.dma("sp", gng[:, :], d["gng"][:, :])
    sink = K.alloc("sink", [128, 8], F32)
    K.dma("sp", sink[:, :], d["sink"][:, :])
    wm = K.alloc("wm", [128, 3, 384], F32)
    K.dma("sp", wm[:, :, :], d["wm"][:, :, :])
    rw = K.alloc("rw", [128, 8, 16], F32)
    K.dma("sp", rw[:, :, :], d["rw"].v(lambda a: a.rearrange("(c p) n -> p c n", p=128)))
    fr32 = Front(K, C, out_dt=F32, name="fr32", nbuf=1, share=fr)
    qr = K.alloc("qr", [128, 512], F32)
    qT = K.alloc("qT", [128, 3, 4, 128], BF16)
    K.pool("memset", qT[64:128, :, :, :], 0.0)
    PTr = K.alloc("PTr", [128, 512], BF16)
    cat = K.alloc("cat", [128, D], F32)
    catT = K.alloc("catT", [128, 8, 128], BF16)
    osb = K.alloc("osb", [128, 512], F32)
    sq = K.alloc("sq", [128, 512], F32)
    sg = sq
    st = K.alloc("st", [128, 32], F32)
    qTd = K.alloc("qTd", [128, 8, 128], BF16)
    K.pool("memset", qTd[64:128, :, :], 0.0)
    Pld = [K.alloc("Pl%d" % i, [128, 384], BF16) for i in range(2)]
    Pmd = [K.alloc("Pm%d" % i, [128, 640], BF16) for i in range(2)]
    PTdd = [K.alloc("PTd%d" % i, [128, 5, 128], BF16) for i in range(2)]
    smd = [K.alloc("sm%d" % i, [128, 16], F32) for i in range(2)]
    tg = cat
    affo = K.alloc("affo", [128, NT, 16], F32)
    smt = K.alloc("smt", [128, 24], F32)
    cat2 = [cat, K.alloc("cat_b", [128, D], F32)]
    qTd2 = [qTd, K.alloc("qTd_b", [128, 8, 128], BF16)]
    K.pool("memset", qTd2[1][64:128, :, :], 0.0)
    xsave = {}

    def h1(n):
        nonlocal nx
        cat_, qTd_ = cat2[n % 2], qTd2[n % 2]
        x_t = xt[nx % 2]
        nx += 1
        K.dma("sp", x_t[:, :], d["xo"][n * 128:(n + 1) * 128, :])
        hT = fr.run(x_t[:, :], A1, SH1, 0, trb)
        cqb, cgb, dqb = K.banks[2], K.banks[3], K.banks[4]
        for dc in range(8):
            K.pe("matmul", cqb[:, 0:256], hT[:, dc, :], Wq[:, dc, 0:256], start=(dc == 0), stop=(dc == 7))
        for dc in range(8):
            K.pe("matmul", cgb[:, :], hT[:, dc, :], Wq[:, dc, 256:768], start=(dc == 0), stop=(dc == 7))
        for dc in range(8):
            K.pe("matmul", dqb[:, :], hT[:, dc, :], Wq[:, dc, 768:1280], start=(dc == 0), stop=(dc == 7))
        head_norm_rope(K, cqb[:, 0:256], 4, None, coso[:, n, :], sino[:, n, :], qr, tmp)
        tq = K.banks[5]
        for h in range(4):
            K.pe("transpose", tq[0:64, h * 128:(h + 1) * 128], qr[:, h * 64:(h + 1) * 64], C.ident[:, :])
        K.act("activation", qT[0:64, 0, :, :], tq.v(lambda a: a[0:64, :].rearrange("p (h t) -> p h t", t=128)), AF.Copy)
        for v_ in range(2):
            K.dve("tensor_tensor", qT[0:64, 1 + v_, :, :], tq.v(lambda a: a[0:64, :].rearrange("p (h t) -> p h t", t=128)),
                  Xi[:, v_, :, :], ALU.mult)
        atb = K.banks[6]
        for h in range(4):
            K.pe("matmul", atb[:, h * 128:(h + 1) * 128], kTr[:, n, h, :], qT[:, 0, h, :], start=True, stop=True)
        K.dve("tensor_tensor", PTr[:, :], atb[:, :], DmT.v(lambda a: a.rearrange("p h t -> p (h t)")), ALU.mult)
        ob = K.banks[7]
        for h in range(4):
            hs = slice(h * 128, (h + 1) * 128)
            K.pe("matmul", ob[:, hs], PTr[:, hs], Vr[:, n, hs], start=True, stop=False)
            K.pe("matmul", ob[:, hs], qT[:, 1, h, :], Rfb[:, n, hs], start=False, stop=False)
            K.pe("matmul", ob[:, hs], qT[:, 2, h, :], Rbb[:, n, hs], start=False, stop=True)
        K.act("activation", osb[:, :], ob[:, :], AF.Copy)
        K.act("activation", sq[:, :], ob[:, :], AF.Square)
        K.dve("tensor_reduce", st[:, 0:4], osb.v(lambda a: a.rearrange("p (h v) -> p h v", v=128)), AX.X, ALU.add)
        K.dve("tensor_reduce", st[:, 4:8], sq.v(lambda a: a.rearrange("p (h v) -> p h v", v=128)), AX.X, ALU.add)
        K.dve("tensor_scalar", st[:, 8:12], st[:, 0:4], 1.0 / 128, None, ALU.mult)
        K.dve("tensor_tensor", st[:, 12:16], st[:, 8:12], st[:, 8:12], ALU.mult)
        K.dve("scalar_tensor_tensor", st[:, 16:20], st[:, 4:8], 1.0 / 128, st[:, 12:16], ALU.mult, ALU.subtract)
        K.act("activation", st[:, 20:24], st[:, 16:20], AF.Sqrt, bias=1e-5, scale=1.0)
        K.dve("reciprocal", st[:, 24:28], st[:, 20:24])
        for h in range(4):
            hs = slice(h * 128, (h + 1) * 128)
            K.dve("tensor_scalar", osb[:, hs], osb[:, hs], st[:, 8 + h:9 + h], st[:, 24 + h:25 + h], ALU.subtract, ALU.mult)
        K.act("activation", sg[:, :], cgb[:, :], AF.Silu)
        K.pool("tensor_tensor", osb[:, :], osb[:, :], gng[:, :], ALU.mult)
        K.pool("tensor_tensor", cat_[:, 0:512], osb[:, :], sg[:, :], ALU.mult)
        head_norm_rope(K, dqb[:, :], 8, None, coso[:, n, :], sino[:, n, :], qr, tmp)
        for h in range(8):
            bk = K.banks[5 + h // 4]
            K.pe("transpose", bk[0:64, (h % 4) * 128:(h % 4 + 1) * 128], qr[:, h * 64:(h + 1) * 64], C.ident[:, :])
        for half in range(2):
            bk = K.banks[5 + half]
            K.act("activation", qTd_[0:64, half * 4:half * 4 + 4, :], bk.v(lambda a: a[0:64, :].rearrange("p (h t) -> p h t", t=128)), AF.Copy)
        xsave[n] = x_t

    def h2(n):
        cat_, qTd_ = cat2[n % 2], qTd2[n % 2]
        x_t = xsave[n]
        mi = 0 if n == 0 else (2 if n == NT - 1 else 1)
        odb = K.banks[7]

        def win_a(h):
            kv = h // 4
            sm_, Pl_, Pm_ = smd[h % 2], Pld[h % 2], Pmd[h % 2]
            s1, s2 = K.banks[2 + h % 2], K.banks[4 + h % 2]
            K.pe("matmul", s1[:, 0:384], qTd_[:, h, :], kTd[:, kv, n * 128:(n + 3) * 128], start=True, stop=True)
            K.pe("matmul", s2[:, 0:256], qTd_[:, h, :], kTd[:, kv, 18 * 128:20 * 128], start=True, stop=True)
            K.dve("tensor_reduce", sm_[:, 0:1], s1[:, 0:384], AX.X, ALU.max)
            K.dve("tensor_reduce", sm_[:, 1:2], s2[:, 0:256], AX.X, ALU.max)
            K.dve("tensor_tensor", sm_[:, 2:3], sm_[:, 0:1], sm_[:, 1:2], ALU.max)
            K.dve("scalar_tensor_tensor", sm_[:, 3:4], sm_[:, 2:3], 0.125, sink[:, h:h + 1], ALU.mult, ALU.max)
            K.dve("tensor_scalar", sm_[:, 4:5], sm_[:, 3:4], -1.0, None, ALU.mult)
            K.act("activation", Pl_[:, 0:384], s1[:, 0:384], AF.Exp, bias=sm_[:, 4:5], scale=0.125)
            K.act("activation", Pm_[:, 384:640], s2[:, 0:256], AF.Exp, bias=sm_[:, 4:5], scale=0.125, accum_out=sm_[:, 5:6])
            K.act("activation", sm_[:, 6:7], sink[:, h:h + 1], AF.Exp, bias=sm_[:, 4:5], scale=1.0)
            K.dve("scalar_tensor_tensor", Pm_[:, 0:384], Pl_[:, 0:384], 1.0, wm[:, mi, :], ALU.mult, ALU.mult, accum_out=sm_[:, 7:8])
            K.dve("tensor_tensor", sm_[:, 8:9], sm_[:, 5:6], sm_[:, 6:7], ALU.add)
            K.dve("tensor_tensor", sm_[:, 8:9], sm_[:, 8:9], sm_[:, 7:8], ALU.add)
            K.dve("reciprocal", sm_[:, 9:10], sm_[:, 8:9])

        def win_b(h):
            kv = h // 4
            sm_, Pm_, PTd_ = smd[h % 2], Pmd[h % 2], PTdd[h % 2]
            ptb = K.banks[6]
            for i in range(5):
                K.pe("transpose", ptb.v(lambda a: a.bitcast(BF16)[:, i * 128:(i + 1) * 128]), Pm_[:, i * 128:(i + 1) * 128], C.identb[:, :])
            K.act("activation", PTd_[:, :, :], ptb.v(lambda a: a.bitcast(BF16)[:, 0:640].rearrange("p (i t) -> p i t", t=128)), AF.Copy)
            for i in range(5):
                vidx = (n + i) if i < 3 else (18 + i - 3)
                K.pe("matmul", odb[:, h * 64:(h + 1) * 64], PTd_[:, i, :], Vd[:, vidx, kv, :], start=(i == 0), stop=(i == 4))
            K.dve("tensor_scalar", cat_[:, 512 + h * 64:512 + (h + 1) * 64], odb[:, h * 64:(h + 1) * 64], sm_[:, 9:10], None, ALU.mult)
        win_a(0)
        for h in range(8):
            if h + 1 < 8:
                win_a(h + 1)
            win_b(h)
        if DBG:
            K.dma("sp", o_cat[n * 128:(n + 1) * 128, :], cat_[:, :], is_out=True)
        for dc in range(8):
            bk = trb[dc // 4]
            K.pe("transpose", bk[:, (dc % 4) * 128:(dc % 4 + 1) * 128], cat_[:, dc * 128:(dc + 1) * 128], C.ident[:, :])
        for half in range(2):
            K.act("activation", catT[:, half * 4:half * 4 + 4, :], trb[half].v(lambda a: a[:, :].rearrange("p (c t) -> p c t", t=128)), AF.Copy)
        x3t = cat_
        for half in range(2):
            mb = K.banks[2 + half]
            for dc in range(8):
                K.pe("matmul", mb[:, :], catT[:, dc, :], Wo[:, dc, half * 512:(half + 1) * 512], start=(dc == 0), stop=(dc == 7))
            K.dve("tensor_tensor", cat_[:, half * 512:(half + 1) * 512], mb[:, :], G1[:, half * 512:(half + 1) * 512], ALU.mult)
        K.pool("tensor_tensor", x3t[:, :], cat_[:, :], x_t[:, :], ALU.add)
        K.dma("sp", o_x3[n * 128:(n + 1) * 128, :], x3t[:, :], is_out=True)
        hT32 = fr32.run(x3t[:, :], A2, SH2, 0, trb)
        lb = K.banks[4]
        for dc in range(8):
            K.pe("matmul", lb[:, 0:16], hT32[:, dc, :], rw[:, dc, :], start=(dc == 0), stop=(dc == 7))
        softmax16(K, lb[:, 0:16], affo[:, n, :], smt)
    h1(0)
    for n in range(NT):
        if n + 1 < NT:
            h1(n + 1)
        h2(n)
    K.dma("sp", o_aff.v(lambda a: a.rearrange("(j p) e -> p j e", p=128)), affo[:, :, :], is_out=True)
    K.finish()
    return nc, es


def prep_stage3(inp, x2, ctx2):
    cos, sin = rope_tables()
    p = np.arange(128)
    tq, tk = p[:, None], p[None, :]
    prev_std = (tq <= tk).astype(np.float32)
    next_std = (tk <= tq).astype(np.float32)
    ones = np.ones((128, 128), np.float32)
    zeros = np.zeros((128, 128), np.float32)
    maps = []
    for core in range(NCORES):
        b, r = core // 4, core % 4
        t0, t1 = r * NOWN, (r + 1) * NOWN
        x = x2[b]
        xh = np.zeros((256, D), np.float32)
        ch = np.zeros((256, 32), np.float32)
        sh = np.zeros((256, 32), np.float32)
        if r > 0:
            xh[0:128] = x[t0 - 128:t0]
            ch[0:128], sh[0:128] = cos[t0 - 128:t0], sin[t0 - 128:t0]
        if r < 3:
            xh[128:256] = x[t1:t1 + 128]
            ch[128:256], sh[128:256] = cos[t1:t1 + 128], sin[t1:t1 + 128]
        EF = np.zeros((128, NKB), np.float32)
        EB = np.zeros((128, NKB), np.float32)
        MF = np.zeros((128, NKB), np.float32)
        MB = np.zeros((128, NKB), np.float32)
        for i in range(NKB):
            if i < 2:
                m = i * 128 + p
                EF[:, i] = t0 - 1 + LC - m
                MF[:, i] = 1.0
                EB[:, i] = SEQ + m - t1
                MB[:, i] = 1.0
            else:
                pos = (i - 2) * 128 + p
                if pos[0] < t0:
                    EF[:, i] = t0 - 1 - pos
                    MF[:, i] = 1.0
                elif pos[0] >= t1:
                    EB[:, i] = pos - t1
                    MB[:, i] = 1.0
        wm = np.stack([np.concatenate([prev_std if r > 0 else zeros, ones, next_std], axis=1),
                       np.concatenate([prev_std, ones, next_std], axis=1),
                       np.concatenate([prev_std, ones, next_std if r < 3 else zeros], axis=1)], axis=1)
        m = {
            "xb": x, "xo": x[t0:t1], "xh": xh, "ctx": ctx2[b],
            "c2": np.stack([fm(inp["c"][b]), fm(inp["c_ctx"])], axis=-1),
            "ada_w": inp["ada_w"][1], "ada_b": inp["ada_b"][1][None, :],
            "ada_bT": np.ascontiguousarray(inp["ada_b"][1].reshape(48, 128).T),
            "gmixT": fm(inp["norm_mix_g"][1]), "gffnT": fm(inp["norm_ffn_g"][1]),
            "w_in": inp["cd_w_in"][0], "w_out": inp["cd_w_out"][0],
            "dec": np.broadcast_to(np.concatenate([inp["c_decay_fwd"][0], inp["c_decay_bwd"][0]])[None, :], (128, 8)),
            "gng": np.broadcast_to(inp["c_norm_g"][0][None, :], (128, 512)),
            "sink": np.broadcast_to(inp["d_sink"][0][None, :], (128, 8)),
            "cosb": cos, "sinb": sin, "coso": cos[t0:t1], "sino": sin[t0:t1], "cosh": ch, "sinh": sh,
            "EF": EF, "EB": EB, "MF": MF, "MB": MB, "wm": wm, "rw": inp["moe_router"][1],
        }
        maps.append({k: np.ascontiguousarray(v, dtype=np.float32) for k, v in m.items()})
    return maps


def _run(builder, maps):
    nc, es = builder()
    es.close()
    res = run_bass_kernel_spmd(nc, maps, core_ids=list(range(NCORES)))
    return res.results


def _gather(results, key):
    return np.stack([np.concatenate([np.asarray(results[b * 4 + r][key]) for r in range(4)], axis=0) for b in range(2)])


def kernel(**inputs):
    inp = {k: np.asarray(v) for k, v in inputs.items()}
    r1 = _run(build_stage1, prep_stage1(inp))
    x1, aff0 = _gather(r1, "x1"), _gather(r1, "aff")
    ctx1 = np.stack([np.asarray(r1[0]["ctx1"]), np.asarray(r1[4]["ctx1"])])
    affc = np.stack([np.asarray(r1[0]["affc"]), np.asarray(r1[4]["affc"])])
    r2 = _run(lambda: build_moe(True, False), prep_moe(inp, 0, x1, aff0, ctx1, affc))
    x2 = _gather(r2, "x2")
    ctx2 = np.stack([np.asarray(r2[0]["ctx2"]), np.asarray(r2[4]["ctx2"])])
    r3 = _run(build_stage3, prep_stage3(inp, x2, ctx2))
    x3, aff1 = _gather(r3, "x3"), _gather(r3, "aff")
    r4 = _run(lambda: build_moe(False, True), prep_moe(inp, 1, x3, aff1, final=True))
    return _gather(r4, "x2").astype(np.float32)
```

```python
import contextlib
import numpy as np
import concourse.bass as bass
import concourse.mybir as mybir
from concourse.bass_utils import run_bass_kernel_spmd

F32 = mybir.dt.float32
BF16 = mybir.dt.bfloat16
ALU = mybir.AluOpType
AF = mybir.ActivationFunctionType
AX = mybir.AxisListType

NCORES = 8
D = 1024
SEQ = 8192
LC = 256
NOWN = 2048
NT = 16
EPS = 1e-6


class Buf:
    __slots__ = ("name", "w", "r")

    def __init__(self, name=""):
        self.name = name
        self.w = None
        self.r = []


class Prog:
    ENGS = ("pe", "act", "dve", "pool", "sp")
    NDMA = 12

    def __init__(self, nc):
        self.nc = nc
        self.ops = {e: [] for e in self.ENGS}
        self.cnt = {e: 0 for e in self.ENGS}
        self.seen = {e: {} for e in self.ENGS}
        self.ndma = {e: 0 for e in self.ENGS}
        self.dma_tok = {e: [] for e in self.ENGS}
        self.out_tokens = []

    def _need(self, eng, tok, raw):
        if tok is None:
            return None
        if tok[0] == "E":
            if tok[1] == eng and not raw:
                return None
            key = ("E", tok[1])
            val = tok[2]
        else:
            key = ("D", tok[1], tok[2])
            val = tok[3]
        if self.seen[eng].get(key, 0) >= val:
            return None
        return key, val

    def op(self, eng, fn, reads=(), writes=(), dma=False, is_out=False):
        needs = {}

        def add(tok, raw):
            n = self._need(eng, tok, raw)
            if n is not None:
                k, v = n
                if needs.get(k, 0) < v:
                    needs[k] = v
        for b in reads:
            add(b.w, True)
        for b in writes:
            add(b.w, False)
            for t in b.r:
                add(t, False)
        if dma:
            n = self.ndma[eng]
            slot = n % self.NDMA
            val = 16 * (n // self.NDMA + 1)
            if n >= self.NDMA:
                add(self.dma_tok[eng][n - self.NDMA], True)
            tok = ("D", eng, slot, val)
            self.ndma[eng] += 1
            self.dma_tok[eng].append(tok)
            inc = ("D", eng, slot)
        else:
            self.cnt[eng] += 1
            tok = ("E", eng, self.cnt[eng])
            inc = ("E", eng)
        for k, v in needs.items():
            self.seen[eng][k] = v
        self.ops[eng].append((list(needs.items()), fn, inc))
        for b in reads:
            b.r.append(tok)
        for b in writes:
            b.w = tok
            b.r = []
        if is_out:
            self.out_tokens.append(tok)
        return tok

    def emit(self, es):
        nc = self.nc
        sems = {}
        for e in self.ENGS:
            sems[("E", e)] = es.enter_context(nc.semaphore("s_" + e))
            for s in range(min(self.NDMA, self.ndma[e])):
                sems[("D", e, s)] = es.enter_context(nc.semaphore("d_%s_%d" % (e, s)))
        fin = {}
        for tok in self.out_tokens:
            key = ("D", tok[1], tok[2])
            fin[key] = max(fin.get(key, 0), tok[3])
        block = es.enter_context(nc.Block())
        engobj = {"pe": "tensor", "act": "scalar", "dve": "vector", "pool": "gpsimd", "sp": "sync"}

        def mk(e):
            def body(eng):
                for waits, fn, inc in self.ops[e]:
                    for k, v in waits:
                        eng.wait_ge(sems[k], v)
                    ins = fn(eng)
                    ins.then_inc(sems[inc], 16 if inc[0] == "D" else 1)
                if e == "sp":
                    for k, v in fin.items():
                        eng.wait_ge(sems[k], v)
            return body
        for e in self.ENGS:
            if self.ops[e] or e == "sp":
                getattr(block, engobj[e])(mk(e))


DTSIZE = {F32: 4, BF16: 2}


class Tile:
    def __init__(self, K, name, ap, off=0, size=0):
        self.K = K
        self.name = name
        self.ap = ap
        self.b = Buf(name)
        self.off = off
        self.size = size

    def __getitem__(self, idx):
        a = self.ap[idx]
        self.K.reg[id(a)] = (a, self.b)
        return a

    def v(self, fn):
        a = fn(self.ap)
        self.K.reg[id(a)] = (a, self.b)
        return a


class KB:
    def __init__(self, nc, es, arena_bytes=200 * 1024):
        self.nc = nc
        self.es = es
        self.P = Prog(nc)
        self.reg = {}
        self.arena = es.enter_context(nc.sbuf_tensor("arena", [128, arena_bytes // 4], F32))
        self.arena_bytes = arena_bytes
        self.live = []
        self.dead = []
        self.banks = []
        for i in range(8):
            t = es.enter_context(nc.psum_tensor("bank%d" % i, [128, 512], F32))
            self.banks.append(Tile(self, "bank%d" % i, t[:, :]))
        self.ndram = 0

    def alloc(self, name, shape, dt=F32, hi=False):
        n = 1
        for s in shape[1:]:
            n *= s
        size = (n * DTSIZE[dt] + 31) // 32 * 32
        self.live.sort(key=lambda t: t.off)
        if hi:
            off = self.arena_bytes - size
            for t in reversed(self.live):
                if t.off + t.size <= off:
                    break
                off = min(off, t.off - size)
            assert off >= 0, "SBUF arena overflow (hi): %s" % name
        else:
            off = 0
            for t in self.live:
                if off + size <= t.off:
                    break
                off = max(off, t.off + t.size)
        if off + size > self.arena_bytes:
            print("ARENA:", [(t.name, t.off, t.size) for t in self.live])
        assert off + size <= self.arena_bytes, "SBUF arena overflow: %s %d" % (name, off + size)
        ap = self.arena[0:shape[0], off // 4:(off + size) // 4]
        if dt != F32:
            ap = ap.bitcast(dt)
        ap = ap[:, 0:n]
        if len(shape) == 3:
            ap = ap.rearrange("p (a b) -> p a b", a=shape[1], b=shape[2])
        elif len(shape) == 4:
            ap = ap.rearrange("p (a b c) -> p a b c", a=shape[1], b=shape[2], c=shape[3])
        tl = Tile(self, name, ap, off, size)
        keep = []
        for d in self.dead:
            if d.off < off + size and off < d.off + d.size:
                if d.b.w is not None:
                    tl.b.r.append(d.b.w)
                tl.b.r.extend(d.b.r)
            keep.append(d)
        self.dead = keep
        self.live.append(tl)
        return tl

    def free(self, *tiles):
        for t in tiles:
            self.live.remove(t)
            self.dead.append(t)

    def dram(self, name, shape, dt=F32, kind="ExternalInput"):
        t = self.nc.dram_tensor(name, list(shape), dt, kind=kind)
        return Tile(self, name, t.ap())

    def _infer(self, args, kw):
        out_ap = kw.get("out", args[0] if args else None)
        acc = kw.get("accum_out", None)
        reads, writes = [], []
        for a in list(args) + list(kw.values()):
            ent = self.reg.get(id(a))
            if ent is None:
                continue
            if a is out_ap or a is acc:
                if ent[1] not in writes:
                    writes.append(ent[1])
            else:
                if ent[1] not in reads:
                    reads.append(ent[1])
        return reads, writes

    def op(self, eng, meth, *args, r=(), w=(), **kw):
        reads, writes = self._infer(args, kw)
        reads = reads + [x.b if isinstance(x, Tile) else x for x in r]
        writes = writes + [x.b if isinstance(x, Tile) else x for x in w]
        return self.P.op(eng, lambda e: getattr(e, meth)(*args, **kw), reads, writes)

    def dve(self, meth, *a, **k):
        return self.op("dve", meth, *a, **k)

    def act(self, meth, *a, **k):
        return self.op("act", meth, *a, **k)

    def pool(self, meth, *a, **k):
        return self.op("pool", meth, *a, **k)

    def pe(self, meth, *a, **k):
        return self.op("pe", meth, *a, **k)

    def dma(self, q, out, in_, is_out=False):
        reads, writes = self._infer((out, in_), {})
        return self.P.op(q, lambda e: e.dma_start(out=out, in_=in_), reads, writes, dma=True, is_out=is_out)

    def finish(self):
        self.P.emit(self.es)


def bc_mid(ap, shape):
    return ap.unsqueeze(2).to_broadcast(list(shape))


def bc_heads(ap, shape):
    return ap.unsqueeze(1).to_broadcast(list(shape))


class Ctx:
    pass


def setup_consts(K):
    C = Ctx()
    C.ident = K.alloc("ident", [128, 128], F32)
    K.pool("memset", C.ident[:, :], 1.0)
    K.pool("affine_select", C.ident[:, :], C.ident[:, :], [[-1, 128]], ALU.is_equal, 0.0, base=0, channel_multiplier=1)
    C.ones = K.alloc("ones", [128, 128], F32)
    K.pool("memset", C.ones[:, :], 1.0)
    C.identb = K.alloc("identb", [128, 128], BF16)
    K.dve("tensor_copy", C.identb[:, :], C.ident[:, :])
    return C


def modulation_gen(K, C, d_c2, d_adaw, d_adab, d_adabT, need_vec, need_gate, res):
    c2 = K.alloc("c2", [128, 8, 2], F32, hi=True)
    K.dma("sp", c2[:, :, :], d_c2[:, :, :])
    sil = K.alloc("sil", [128, 8, 2], F32, hi=True)
    K.act("activation", sil[:, :, :], c2[:, :, :], AF.Silu)
    rep = [K.alloc("rep%d" % i, [128, 8, 128], F32, hi=True) for i in range(2)]
    for i in range(2):
        K.dve("tensor_copy", rep[i][:, :, :], sil.v(lambda a: a[:, :, i:i + 1].to_broadcast([128, 8, 128])))
    brow = K.alloc("adab_row", [1, 6144], F32, hi=True)
    K.dma("sp", brow[:, :], d_adab[:, :])
    bT = K.alloc("adabT", [128, 48], F32, hi=True)
    K.dma("sp", bT[:, :], d_adabT[:, :])
    modT = K.alloc("modT", [128, 48, 2], F32)
    wblk = [K.alloc("adaw%d" % i, [128, 8, 512], F32, hi=True) for i in range(2)]
    gates = {}
    mps = K.banks[7]
    n = 0
    for blk in range(12):
        if blk not in need_vec and blk not in need_gate:
            continue
        wb = wblk[n % 2]
        n += 1
        K.dma("sp", wb[:, :, :], d_adaw.v(lambda a: a.rearrange("(c p) n -> p c n", p=128)[:, :, blk * 512:(blk + 1) * 512]))
        if blk in need_vec:
            for sub in range(4):
                ec = blk * 4 + sub
                for kc in range(8):
                    K.pe("matmul", mps[:, ec * 2:ec * 2 + 2], wb[:, kc, sub * 128:(sub + 1) * 128], sil[:, kc, :],
                         start=(kc == 0), stop=(kc == 7))
            for sub in range(4):
                ec = blk * 4 + sub
                K.act("activation", modT[:, ec, :], mps[:, ec * 2:ec * 2 + 2], AF.Identity, bias=bT[:, ec:ec + 1], scale=1.0)
        else:
            for i in range(2):
                gp = K.banks[5 + i]
                for kc in range(8):
                    K.pe("matmul", gp[:, :], rep[i][:, kc, :], wb[:, kc, :], start=(kc == 0), stop=False)
                K.pe("matmul", gp[:, :], C.ones[0:1, :], brow[0:1, blk * 512:(blk + 1) * 512], start=False, stop=True)
                key = (blk // 2, i)
                if key not in gates:
                    gates[key] = K.alloc("gate%d_%d" % key, [128, 1024], F32)
                half = blk % 2
                K.act("activation", gates[key][:, half * 512:(half + 1) * 512], gp[:, :], AF.Copy)
        yield blk
    K.free(c2, rep[0], rep[1], brow, bT, wblk[0], wblk[1])
    res.extend([modT, gates, sil])


def modulation(K, C, d_c2, d_adaw, d_adab, d_adabT, need_vec, need_gate):
    res = []
    for _ in modulation_gen(K, C, d_c2, d_adaw, d_adab, d_adabT, need_vec, need_gate, res):
        pass
    return res[0], res[1], res[2]


def mod_cols(K, modT, d_g, sh_blk, sc_blk, name):
    gT = K.alloc(name + "_gT", [128, 8], F32, hi=True)
    K.dma("sp", gT[:, :], d_g[:, :])
    A = K.alloc(name + "_A", [128, 8, 2], F32)
    K.dve("tensor_scalar", A[:, :, :], modT[:, sc_blk * 4:sc_blk * 4 + 8, :], 1.0, None, ALU.add)
    K.dve("tensor_tensor", A[:, :, :], A[:, :, :], gT.v(lambda a: a[:, :].unsqueeze(2).to_broadcast([128, 8, 2])), ALU.mult)
    SH = K.alloc(name + "_SH", [128, 8, 2], F32)
    K.dve("tensor_copy", SH[:, :, :], modT[:, sh_blk * 4:sh_blk * 4 + 8, :])
    K.free(gT)
    return A, SH


class Front:
    def __init__(self, K, C, out_dt=BF16, nbuf=2, name="fr", share=None):
        self.K, self.C = K, C
        if share is not None:
            self.xn, self.junk, self.ss = share.xn[:nbuf], share.junk, share.ss[:nbuf]
        else:
            self.xn = [K.alloc(name + "_xn%d" % i, [128, 1024], F32) for i in range(nbuf)]
            self.junk = K.alloc(name + "_junk", [128, 1024], F32)
            self.ss = [K.alloc(name + "_ss%d" % i, [128, 4], F32) for i in range(nbuf)]
        self.hT = [K.alloc(name + "_hT%d" % i, [128, 8, 128], out_dt) for i in range(nbuf)]
        self.n = 0
        self.nbuf = nbuf

    def free(self):
        self.K.free(*(self.xn + self.ss + self.hT + [self.junk]))

    def run(self, xt_ap, A, SH, col, banks):
        K, C = self.K, self.C
        i = self.n % self.nbuf
        self.n += 1
        xn, ss, hT = self.xn[i], self.ss[i], self.hT[i]
        K.act("activation", self.junk[:, :], xt_ap, AF.Square, accum_out=ss[:, 0:1])
        K.act("activation", ss[:, 1:2], ss[:, 0:1], AF.Sqrt, bias=EPS, scale=1.0 / D)
        K.dve("reciprocal", ss[:, 2:3], ss[:, 1:2])
        K.act("activation", xn[:, :], xt_ap, AF.Copy, scale=ss[:, 2:3])
        for dc in range(8):
            bk = banks[dc // 4]
            K.pe("transpose", bk[:, (dc % 4) * 128:(dc % 4 + 1) * 128], xn[:, dc * 128:(dc + 1) * 128], C.ident[:, :])
        for dc in range(8):
            bk = banks[dc // 4]
            K.dve("tensor_scalar", hT[:, dc, :], bk[:, (dc % 4) * 128:(dc % 4 + 1) * 128],
                  A[:, dc, col:col + 1], SH[:, dc, col:col + 1], ALU.mult, ALU.add)
        return hT


def head_norm_rope(K, src_ap, H, G, cos_ap, sin_ap, out, tmp):
    n = H * 64
    if G is not None:
        sq, ssh, qn = tmp["sq"], tmp["ssh"], tmp["qn"]
        K.act("activation", sq[:, 0:n], src_ap, AF.Square)
        K.dve("tensor_reduce", ssh[:, 0:H], sq.v(lambda a: a[:, 0:n].rearrange("p (h d) -> p h d", d=64)), AX.X, ALU.add)
        K.act("activation", ssh[:, 8:8 + H], ssh[:, 0:H], AF.Sqrt, bias=EPS, scale=1.0 / 64)
        K.dve("reciprocal", ssh[:, 16:16 + H], ssh[:, 8:8 + H])
        dst = qn if cos_ap is not None else out
        K.dve("tensor_tensor", dst.v(lambda a: a[:, 0:n].rearrange("p (h d) -> p h d", d=64)),
              _reg_like(K, src_ap, src_ap.rearrange("p (h d) -> p h d", d=64)),
              ssh.v(lambda a: a[:, 16:16 + H].unsqueeze(2).to_broadcast([128, H, 64])), ALU.mult)
        K.pool("tensor_tensor", dst.v(lambda a: a[:, 0:n].rearrange("p (h d) -> p h d", d=64)),
               dst.v(lambda a: a[:, 0:n].rearrange("p (h d) -> p h d", d=64)),
               G.v(lambda a: a[:, :].unsqueeze(1).to_broadcast([128, H, 64])), ALU.mult)
    else:
        qn = tmp["qn"]
        dst = qn if cos_ap is not None else out
        K.act("activation", dst[:, 0:n], src_ap, AF.Copy)
    if cos_ap is None:
        return
    t1, t2 = tmp["t1"], tmp["t2"]

    def v4(t, half):
        return t.v(lambda a: a[:, 0:n].rearrange("p (h t d) -> p h t d", t=2, d=32)[:, :, half, :])

    def v3(t):
        return t.v(lambda a: a[:, 0:H * 32].rearrange("p (h d) -> p h d", d=32))
    cb = _reg_like(K, cos_ap, cos_ap.unsqueeze(1).to_broadcast([128, H, 32]))
    sb = _reg_like(K, sin_ap, sin_ap.unsqueeze(1).to_broadcast([128, H, 32]))
    K.pool("tensor_tensor", v3(t1), v4(qn, 0), cb, ALU.mult)
    K.pool("tensor_tensor", v3(t2), v4(qn, 1), sb, ALU.mult)
    K.dve("tensor_tensor", v4(out, 0), v3(t1), v3(t2), ALU.subtract)
    K.pool("tensor_tensor", v3(t1), v4(qn, 0), sb, ALU.mult)
    K.pool("tensor_tensor", v3(t2), v4(qn, 1), cb, ALU.mult)
    K.dve("tensor_tensor", v4(out, 1), v3(t1), v3(t2), ALU.add)


def _reg_like(K, base_ap, new_ap):
    ent = K.reg.get(id(base_ap))
    assert ent is not None
    K.reg[id(new_ap)] = (new_ap, ent[1])
    return new_ap


def attention(K, C, qT, NQ, QB, key_tiles, kT, Vaug, aT, PT, osb, rden, scale):
    sbanks = [K.banks[0], K.banks[1], K.banks[2]]
    obanks = [K.banks[3], K.banks[4]]
    bcb = K.banks[5]
    steps = []
    for h in range(8):
        for qb in range(NQ // QB):
            for i, kt in enumerate(key_tiles):
                steps.append((h, qb, i, kt))
    nk = len(key_tiles)

    def qk(n):
        h, qb, i, kt = steps[n]
        K.pe("matmul", sbanks[n % 3][:, 0:QB], kT[:, h // 4, kt * 128:(kt + 1) * 128], qT[:, h, qb * QB:(qb + 1) * QB],
             start=True, stop=True)
    LOOK = 2
    for n in range(min(LOOK, len(steps))):
        qk(n)
    for n, (h, qb, i, kt) in enumerate(steps):
        if n + LOOK < len(steps):
            qk(n + LOOK)
        pt = PT[n % 3]
        K.act("activation", pt[:, 0:QB], sbanks[n % 3][:, 0:QB], AF.Exp, scale=scale)
        ob = obanks[(h * (NQ // QB) + qb) % 2]
        K.pe("matmul", ob[0:65, 0:QB], Vaug[:, kt, h // 4, :], pt[:, 0:QB], start=(i == 0), stop=(i == nk - 1))
        if i == nk - 1:
            K.dve("reciprocal", rden[64:65, 0:QB], ob[64:65, 0:QB])
            K.pe("matmul", bcb[0:64, 0:QB], C.ones[64:65, 0:64], rden[64:65, 0:QB], start=True, stop=True)
            K.act("activation", osb[0:64, 0:QB], ob[0:64, 0:QB], AF.Copy)
            K.dve("tensor_tensor", aT[0:64, h, qb * QB:(qb + 1) * QB], osb[0:64, 0:QB], bcb[0:64, 0:QB], ALU.mult)


def softmax16(K, logits_ap, aff_out_ap, tmp):
    K.dve("tensor_reduce", tmp[:, 0:1], logits_ap, AX.X, ALU.max)
    K.dve("tensor_scalar", tmp[:, 1:2], tmp[:, 0:1], -1.0, None, ALU.mult)
    K.act("activation", tmp[:, 8:24], logits_ap, AF.Exp, bias=tmp[:, 1:2], scale=1.0, accum_out=tmp[:, 2:3])
    K.dve("reciprocal", tmp[:, 3:4], tmp[:, 2:3])
    K.dve("tensor_scalar", aff_out_ap, tmp[:, 8:24], tmp[:, 3:4], None, ALU.mult)


def build_stage1():
    nc = bass.Bass("TRN2", target_bir_lowering=False)
    es = contextlib.ExitStack()
    K = KB(nc, es)
    d = {}
    for name, shape in [("xb", [SEQ, D]), ("xo", [NOWN, D]), ("xh", [256, D]), ("ctx", [LC, D]), ("c2", [128, 8, 2]),
                        ("ada_w", [D, 6 * D]), ("ada_b", [1, 6 * D]), ("ada_bT", [128, 48]),
                        ("gmixT", [128, 8]), ("gffnT", [128, 8]), ("w_in", [D, 1280]), ("w_out", [D, D]),
                        ("qg", [128, 64]), ("kg", [128, 64]), ("gw", [4, 128, 128]), ("bsc", [128, 4]),
                        ("cosb", [SEQ, 32]), ("sinb", [SEQ, 32]), ("coso", [NOWN, 32]), ("sino", [NOWN, 32]),
                        ("band", [128, 60, 128]), ("rw", [D, 16])]:
        d[name] = K.dram(name, shape)
    o_x1 = K.dram("x1", [NOWN, D], kind="ExternalOutput")
    o_aff = K.dram("aff", [NOWN, 16], kind="ExternalOutput")
    o_c1 = K.dram("ctx1", [LC, D], kind="ExternalOutput")
    o_affc = K.dram("affc", [LC, 16], kind="ExternalOutput")

    C = setup_consts(K)
    modT, gates, sil = modulation(K, C, d["c2"], d["ada_w"], d["ada_b"], d["ada_bT"],
                                  need_vec=(0, 1, 2, 3, 6, 7, 8, 9), need_gate=(4, 5))
    A1, SH1 = mod_cols(K, modT, d["gmixT"], 0, 2, "m1")
    A2, SH2 = mod_cols(K, modT, d["gffnT"], 6, 8, "m2")
    G1 = [gates[(2, 0)], gates[(2, 1)]]
    K.free(sil)

    Win = K.alloc("Win", [128, 8, 1280], BF16, hi=True)
    K.dma("pool", Win[:, :, :], d["w_in"].v(lambda a: a.rearrange("(c p) n -> p c n", p=128)))
    QG = K.alloc("QG", [128, 64], F32)
    KG = K.alloc("KG", [128, 64], F32)
    K.dma("sp", QG[:, :], d["qg"][:, :])
    K.dma("sp", KG[:, :], d["kg"][:, :])
    band = K.alloc("band", [128, 60, 128], BF16, hi=True)
    K.dma("pool", band[:, :, :], d["band"][:, :, :])
    gw = K.alloc("gw", [128, 4, 128], BF16, hi=True)
    K.dma("pool", gw[:, :, :], d["gw"].v(lambda a: a.rearrange("g p n -> p g n")))
    bsc = K.alloc("bsc", [128, 4], F32)
    K.dma("sp", bsc[:, :], d["bsc"][:, :])
    coso = K.alloc("coso", [128, NT, 32], F32, hi=True)
    sino = K.alloc("sino", [128, NT, 32], F32, hi=True)
    K.dma("sp", coso[:, :, :], d["coso"].v(lambda a: a.rearrange("(j p) n -> p j n", p=128)))
    K.dma("sp", sino[:, :, :], d["sino"].v(lambda a: a.rearrange("(j p) n -> p j n", p=128)))

    qT = K.alloc("qT", [128, 8, NOWN], BF16)
    qTc = K.alloc("qTc", [128, 8, LC], BF16)
    K.pool("memset", qT[64:128, :, :], 0.0)
    K.pool("memset", qTc[64:128, :, :], 0.0)
    utok = K.alloc("utok", [128, 20, 512], BF16, hi=True)
    xt = [K.alloc("xt%d" % i, [128, 1024], F32) for i in range(2)]
    fr = Front(K, C)
    tmp = {"sq": K.alloc("sq", [128, 512], F32, hi=True), "ssh": K.alloc("ssh", [128, 24], F32, hi=True), "qn": K.alloc("qn", [128, 512], F32, hi=True),
           "t1": K.alloc("t1", [128, 256], F32, hi=True), "t2": K.alloc("t2", [128, 256], F32, hi=True)}
    qr = K.alloc("qr", [128, 512], F32, hi=True)
    trb = [K.banks[0], K.banks[1]]
    nx = 0

    passB = [("halo", 0), ("halo", 1)] + [("own", j) for j in range(NT)] + [("ctx", 0), ("ctx", 1)]
    def pB_front(n_, kind, j):
        nonlocal nx
        x_t = xt[nx % 2]
        nx += 1
        src = {"halo": d["xh"], "own": d["xo"], "ctx": d["ctx"]}[kind]
        K.dma("sp", x_t[:, :], src[j * 128:(j + 1) * 128, :])
        hT = fr.run(x_t[:, :], A1, SH1, 1 if kind == "ctx" else 0, trb)
        ub, qb = K.banks[3 + 4 * (n_ % 2)], K.banks[2 + 4 * (n_ % 2)]
        for dc in range(8):
            K.pe("matmul", ub[:, :], hT[:, dc, :], Win[:, dc, 768:1280], start=(dc == 0), stop=(dc == 7))
        if kind != "halo":
            for dc in range(8):
                K.pe("matmul", qb[:, :], hT[:, dc, :], Win[:, dc, 0:512], start=(dc == 0), stop=(dc == 7))

    def pB_back(n_, kind, j):
        ub, qb = K.banks[3 + 4 * (n_ % 2)], K.banks[2 + 4 * (n_ % 2)]
        ui = {"halo": 17 * j, "own": 1 + j, "ctx": 18 + j}[kind]
        K.act("activation", utok[:, ui, :], ub[:, :], AF.Copy)
        if kind == "halo":
            return
        if kind == "own":
            head_norm_rope(K, qb[:, :], 8, QG, coso[:, j, :], sino[:, j, :], qr, tmp)
            dst, off = qT, j * 128
        else:
            head_norm_rope(K, qb[:, :], 8, QG, None, None, qr, tmp)
            dst, off = qTc, j * 128
        for h in range(8):
            bk = K.banks[4 + h // 4]
            K.pe("transpose", bk[0:64, (h % 4) * 128:(h % 4 + 1) * 128], qr[:, h * 64:(h + 1) * 64], C.ident[:, :])
        for half in range(2):
            bk = K.banks[4 + half]
            K.act("activation", dst[0:64, half * 4:half * 4 + 4, off:off + 128],
                  bk.v(lambda a: a[0:64, :].rearrange("p (h t) -> p h t", t=128)), AF.Copy)
    pB_front(0, *passB[0])
    for n_, (kind, j) in enumerate(passB):
        if n_ + 1 < len(passB):
            pB_front(n_ + 1, *passB[n_ + 1])
        pB_back(n_, kind, j)

    bT = K.alloc("bT", [128, 4, NOWN], BF16)
    bTc = K.alloc("bTc", [128, 4, LC], BF16)
    dT = [K.alloc("dT%d" % i, [128, 128], BF16, hi=True) for i in range(2)]
    nd = 0
    jobs = [("own", j) for j in range(NT)] + [("ctx", 0), ("ctx", 1)]
    for kind, j in jobs:
        for g in range(4):
            if kind == "own":
                base = 12 if j == 0 else (24 if j == NT - 1 else 0)
                srcs = [(j + s, base + g * 3 + s) for s in range(3)]
                dst, off = bT, j * 128
            else:
                if j == 0:
                    srcs = [(18, 36 + g * 3 + 1), (19, 36 + g * 3 + 2)]
                else:
                    srcs = [(18, 48 + g * 3 + 0), (19, 48 + g * 3 + 1)]
                dst, off = bTc, j * 128
            pb = K.banks[6]
            for n, (ui, bi) in enumerate(srcs):
                K.pe("matmul", pb[:, 0:128], utok[:, ui, g * 128:(g + 1) * 128], band[:, bi, :],
                     start=(n == 0), stop=(n == len(srcs) - 1))
            dt_ = dT[nd % 2]
            nd += 1
            K.dve("tensor_copy", dt_[:, :], pb[:, 0:128])
            yb = K.banks[7]
            K.pe("matmul", yb[:, 0:128], gw[:, g, :], dt_[:, :], start=True, stop=True)
            K.act("activation", dst[:, g, off:off + 128], yb[:, 0:128], AF.Copy, scale=bsc[:, g:g + 1])
    K.free(utok, band, gw, dT[0], dT[1], qr)

    NKT = 2 + SEQ // 128
    kT = K.alloc("kT", [128, 2, NKT * 128], BF16)
    K.pool("memset", kT[64:128, :, :], 0.0)
    Vaug = K.alloc("Vaug", [128, NKT, 2, 65], BF16)
    K.pool("memset", Vaug.v(lambda a: a[:, :, :, 64:65]), 1.0)
    cosb = K.alloc("cosb", [128, SEQ // 128, 32], F32, hi=True)
    sinb = K.alloc("sinb", [128, SEQ // 128, 32], F32, hi=True)
    K.dma("sp", cosb[:, :, :], d["cosb"].v(lambda a: a.rearrange("(j p) n -> p j n", p=128)))
    K.dma("sp", sinb[:, :, :], d["sinb"].v(lambda a: a.rearrange("(j p) n -> p j n", p=128)))
    kr = K.alloc("kr", [128, 128], F32, hi=True)
    def passA_front(kt):
        nonlocal nx
        x_t = xt[nx % 2]
        nx += 1
        if kt < 2:
            K.dma("sp", x_t[:, :], d["ctx"][kt * 128:(kt + 1) * 128, :])
        else:
            K.dma("sp", x_t[:, :], d["xb"][(kt - 2) * 128:(kt - 1) * 128, :])
        hT = fr.run(x_t[:, :], A1, SH1, 1 if kt < 2 else 0, trb)
        kvb = K.banks[2 + kt % 2]
        for dc in range(8):
            K.pe("matmul", kvb[:, 0:256], hT[:, dc, :], Win[:, dc, 512:768], start=(dc == 0), stop=(dc == 7))

    def passA_back(kt):
        kvb = K.banks[2 + kt % 2]
        if kt < 2:
            head_norm_rope(K, kvb[:, 0:128], 2, KG, None, None, kr, tmp)
        else:
            head_norm_rope(K, kvb[:, 0:128], 2, KG, cosb[:, kt - 2, :], sinb[:, kt - 2, :], kr, tmp)
        K.act("activation", Vaug.v(lambda a: a[:, kt, :, 0:64]),
              kvb.v(lambda a: a[:, 128:256].rearrange("p (h d) -> p h d", d=64)), AF.Copy)
        tb = K.banks[4 + kt % 2]
        for h in range(2):
            K.pe("transpose", tb[0:64, h * 128:(h + 1) * 128], kr[:, h * 64:(h + 1) * 64], C.ident[:, :])
        K.dve("tensor_copy", kT[0:64, :, kt * 128:(kt + 1) * 128],
              tb.v(lambda a: a[0:64, 0:256].rearrange("p (h t) -> p h t", t=128)))
    passA_front(0)
    for kt in range(NKT):
        if kt + 1 < NKT:
            passA_front(kt + 1)
        passA_back(kt)
    K.free(cosb, sinb, kr, Win, coso, sino, *tmp.values())

    aT, aTc = qT, qTc
    PT = [K.alloc("PT%d" % i, [128, 512], BF16) for i in range(3)]
    osb = K.alloc("osb", [64, 512], F32)
    rden = K.alloc("rden", [65, 512], F32)
    attention(K, C, qTc, LC, LC, [0, 1], kT, Vaug, aTc, PT, osb, rden, 0.125)
    attention(K, C, qT, NOWN, 512, list(range(NKT)), kT, Vaug, aT, PT, osb, rden, 0.125)
    K.free(kT, Vaug, PT[0], PT[1], PT[2], osb, rden)

    WoA = K.alloc("WoA", [128, 8, D], BF16)
    K.pool("memset", WoA[64:128, :, :], 0.0)
    WoB = K.alloc("WoB", [128, 4, D], BF16)
    K.dma("pool", WoA[0:64, :, :], d["w_out"].v(lambda a: a[0:512, :].rearrange("(h p) n -> p h n", p=64)))
    K.dma("pool", WoB[:, :, :], d["w_out"].v(lambda a: a[512:1024, :].rearrange("(g p) n -> p g n", p=128)))
    rw = K.alloc("rw", [128, 8, 16], F32)
    K.dma("sp", rw[:, :, :], d["rw"].v(lambda a: a.rearrange("(c p) n -> p c n", p=128)))
    fr32 = Front(K, C, out_dt=F32, name="fr32")
    x1 = [K.alloc("x1_%d" % i, [128, 1024], F32) for i in range(2)]
    tg = K.alloc("tg", [128, 1024], F32)
    affo = K.alloc("affo", [128, NT + 2, 16], F32)
    smt = K.alloc("smt", [128, 24], F32)
    jobs = [("ctx", 0), ("ctx", 1)] + [("own", j) for j in range(NT)]
    tg2 = [tg, K.alloc("tg2", [128, 1024], F32)]

    def op_a(n, kind, j):
        nonlocal nx
        x_t = xt[nx % 2]
        nx += 1
        src, a_, b_, col, outd = (d["xo"], aT, bT, 0, o_x1) if kind == "own" else (d["ctx"], aTc, bTc, 1, o_c1)
        K.dma("sp", x_t[:, :], src[j * 128:(j + 1) * 128, :])
        x1t = x1[n % 2]
        tg_ = tg2[n % 2]
        for half in range(2):
            mb = K.banks[2 + 4 * (n % 2) + half]
            for h in range(8):
                K.pe("matmul", mb[:, :], a_[:, h, j * 128:(j + 1) * 128], WoA[:, h, half * 512:(half + 1) * 512],
                     start=(h == 0), stop=False)
            for g in range(4):
                K.pe("matmul", mb[:, :], b_[:, g, j * 128:(j + 1) * 128], WoB[:, g, half * 512:(half + 1) * 512],
                     start=False, stop=(g == 3))
            K.dve("tensor_tensor", tg_[:, half * 512:(half + 1) * 512], mb[:, :], G1[col][:, half * 512:(half + 1) * 512], ALU.mult)
        K.pool("tensor_tensor", x1t[:, :], tg_[:, :], x_t[:, :], ALU.add)
        K.dma("sp", outd[j * 128:(j + 1) * 128, :], x1t[:, :], is_out=True)

    def op_b(n, kind, j):
        col = 0 if kind == "own" else 1
        x1t = x1[n % 2]
        hT = fr32.run(x1t[:, :], A2, SH2, col, trb)
        lb = K.banks[4 + n % 2]
        for dc in range(8):
            K.pe("matmul", lb[:, 0:16], hT[:, dc, :], rw[:, dc, :], start=(dc == 0), stop=(dc == 7))
        ai = (NT + j) if kind == "ctx" else j
        softmax16(K, lb[:, 0:16], affo[:, ai, :], smt)
    op_a(0, *jobs[0])
    for n, (kind, j) in enumerate(jobs):
        if n + 1 < len(jobs):
            op_a(n + 1, *jobs[n + 1])
        op_b(n, kind, j)
    K.dma("sp", o_aff.v(lambda a: a.rearrange("(j p) e -> p j e", p=128)), affo[:, 0:NT, :], is_out=True)
    K.dma("sp", o_affc.v(lambda a: a.rearrange("(j p) e -> p j e", p=128)), affo[:, NT:NT + 2, :], is_out=True)
    K.finish()
    return nc, es


def rope_tables():
    rows = SEQ // 64
    row = np.repeat(np.arange(rows, dtype=np.float32), 64)
    col = np.tile(np.arange(64, dtype=np.float32), rows)
    freqs = (np.float32(10000.0) ** (-np.arange(16, dtype=np.float32) / np.float32(16))).astype(np.float32)
    ang = np.concatenate([row[:, None] * freqs, col[:, None] * freqs], axis=-1).astype(np.float32)
    return np.cos(ang).astype(np.float32), np.sin(ang).astype(np.float32)


def band_mats(T0, L):
    out = np.zeros((4, 3, 128, 128), np.float32)
    for g, w in enumerate((2, 4, 8, 16)):
        for tl in range(128):
            t = T0 + tl
            lo = min(max(t - w // 2, 0), L)
            hi = min(max(t - w // 2 + w, 0), L)
            for s in range(lo, hi):
                o = (s - T0) // 128 + 1
                out[g, o, (s - T0) % 128, tl] += 1.0 / (hi - lo)
            out[g, 1, tl, tl] -= 1.0
    return out


def fm(v):
    return np.ascontiguousarray(v.reshape(8, 128).T)


def prep_stage1(inp):
    cos, sin = rope_tables()
    mid = band_mats(1280, SEQ)
    first = band_mats(0, SEQ)
    last = band_mats(SEQ - 128, SEQ)
    c0 = band_mats(0, LC)
    c1 = band_mats(128, LC)
    maps = []
    for core in range(NCORES):
        b, r = core // 4, core % 4
        t0 = r * NOWN
        x = inp["x"][b]
        xh = np.zeros((256, D), np.float32)
        if r > 0:
            xh[0:128] = x[t0 - 128:t0]
        if r < 3:
            xh[128:256] = x[t0 + NOWN:t0 + NOWN + 128]
        bands = np.concatenate([mid.reshape(12, 128, 128), (first if r == 0 else mid).reshape(12, 128, 128),
                                (last if r == 3 else mid).reshape(12, 128, 128), c0.reshape(12, 128, 128),
                                c1.reshape(12, 128, 128)], axis=0)
        m = {
            "xb": x, "xo": x[t0:t0 + NOWN], "xh": xh, "ctx": inp["ctx"][b],
            "c2": np.stack([fm(inp["c"][b]), fm(inp["c_ctx"])], axis=-1),
            "ada_w": inp["ada_w"][0], "ada_b": inp["ada_b"][0][None, :],
            "ada_bT": np.ascontiguousarray(inp["ada_b"][0].reshape(48, 128).T),
            "gmixT": fm(inp["norm_mix_g"][0]), "gffnT": fm(inp["norm_ffn_g"][0]),
            "w_in": inp["ab_w_in"][0], "w_out": inp["ab_w_out"][0],
            "qg": np.broadcast_to(inp["a_q_norm_g"][0][None, :], (128, 64)),
            "kg": np.broadcast_to(inp["a_k_norm_g"][0][None, :], (128, 64)),
            "gw": inp["b_group_w"][0], "bsc": np.ascontiguousarray(inp["b_scale"][0].reshape(4, 128).T),
            "cosb": cos, "sinb": sin, "coso": cos[t0:t0 + NOWN], "sino": sin[t0:t0 + NOWN],
            "band": bands.transpose(1, 0, 2), "rw": inp["moe_router"][0],
        }
        maps.append({k: np.ascontiguousarray(v, dtype=np.float32) for k, v in m.items()})
    return maps


GCAP = 96
CCAP = 32
NSLOT = 4 * GCAP + CCAP


def build_moe(has_ctx, final):
    nc = bass.Bass("TRN2", target_bir_lowering=False)
    es = contextlib.ExitStack()
    K = KB(nc, es, arena_bytes=206 * 1024)
    d = {}
    ins = [("x1", [NOWN, D]), ("affb", [SEQ, 16]), ("affo", [NOWN, 16]), ("c2", [128, 8, 2]),
           ("ada_w", [D, 6 * D]), ("ada_b", [1, 6 * D]), ("ada_bT", [128, 48]), ("gffnT", [128, 8]),
           ("wg", [16, D, 2 * D]), ("wu", [16, D, 2 * D]), ("wd", [16, 2 * D, D])]
    if has_ctx:
        ins += [("ctx1", [LC, D]), ("affc", [LC, 16])]
    if final:
        ins += [("fg", [128, D])]
    for name, shape in ins:
        d[name] = K.dram(name, shape)
    o_x2 = K.dram("x2", [NOWN, D], kind="ExternalOutput")
    o_c2 = K.dram("ctx2", [LC, D], kind="ExternalOutput") if has_ctx else None
    NTL = NT + (2 if has_ctx else 0)
    NE2 = 32 if has_ctx else 16

    C = setup_consts(K)
    mres = []
    mgen = modulation_gen(K, C, d["c2"], d["ada_w"], d["ada_b"], d["ada_bT"], (6, 7, 8, 9), (10, 11), mres)
    next(mgen, None)

    X = [K.alloc("X%d" % j, [128, D], F32) for j in range(NTL)]
    H2 = [K.alloc("H2_%d" % j, [128, D], BF16) for j in range(NTL)]
    junk = K.alloc("junk", [128, D], F32, hi=True)
    ss = K.alloc("ss", [128, NTL, 4], F32)

    def load_tile(j):
        src = d["x1"][j * 128:(j + 1) * 128, :] if j < NT else d["ctx1"][(j - NT) * 128:(j - NT + 1) * 128, :]
        K.dma("sp", X[j][:, :], src)
        K.act("activation", junk[:, :], X[j][:, :], AF.Square, accum_out=ss[:, j, 0:1])
        K.act("activation", ss[:, j, 1:2], ss[:, j, 0:1], AF.Sqrt, bias=EPS, scale=1.0 / D)
        K.dve("reciprocal", ss[:, j, 2:3], ss[:, j, 1:2])
        K.act("activation", H2[j][:, :], X[j][:, :], AF.Copy, scale=ss[:, j, 2:3])

    affb = K.alloc("affb", [128, SEQ // 128, 16], F32, hi=True)
    K.dma("sp", affb[:, :, :], d["affb"].v(lambda a: a.rearrange("(j p) e -> p j e", p=128)))
    affo = K.alloc("affo", [128, NTL, 16], F32)
    K.dma("sp", affo[:, 0:NT, :], d["affo"].v(lambda a: a.rearrange("(j p) e -> p j e", p=128)))
    if has_ctx:
        K.dma("sp", affo[:, NT:NTL, :], d["affc"].v(lambda a: a.rearrange("(j p) e -> p j e", p=128)))
    lo = K.alloc("lo", [128, NE2], F32)
    capt = K.alloc("capt", [128, NE2], F32, hi=True)
    mid = K.alloc("mid", [128, NE2], F32, hi=True)
    cnt = K.alloc("cnt", [128, NE2], F32, hi=True)
    gtmp = K.alloc("gtmp", [128, NE2], F32, hi=True)
    mask = K.alloc("mask", [128, SEQ // 128 + 2, 16], F32, hi=True)
    K.dve("memset", lo[:, :], 0.0)
    K.dve("memset", capt[:, 0:16], float(2 * SEQ // 16))
    if has_ctx:
        K.dve("memset", capt[:, 16:32], float(2 * LC // 16))
    tb = K.banks[0]
    NJ = SEQ // 128
    for it in range(30):
        K.dve("tensor_scalar", mid[:, :], lo[:, :], float(2.0 ** -(it + 1)), None, ALU.add)
        K.dve("tensor_tensor", mask[:, 0:NJ, :], affb[:, :, :],
              mid.v(lambda a: a[:, 0:16].unsqueeze(1).to_broadcast([128, NJ, 16])), ALU.is_ge)
        K.dve("tensor_reduce", cnt[:, 0:16], mask.v(lambda a: a[:, 0:NJ, :].rearrange("p j e -> p e j")), AX.X, ALU.add)
        if has_ctx:
            K.dve("tensor_tensor", mask[:, NJ:NJ + 2, :], affo[:, NT:NTL, :],
                  mid.v(lambda a: a[:, 16:32].unsqueeze(1).to_broadcast([128, 2, 16])), ALU.is_ge)
            K.dve("tensor_reduce", cnt[:, 16:32], mask.v(lambda a: a[:, NJ:NJ + 2, :].rearrange("p j e -> p e j")), AX.X, ALU.add)
        K.pe("matmul", tb[:, 0:NE2], C.ones[:, :], cnt[:, :], start=True, stop=True)
        K.dve("tensor_tensor", gtmp[:, :], tb[:, 0:NE2], capt[:, :], ALU.is_ge)
        K.dve("tensor_tensor", gtmp[:, :], gtmp[:, :], mid[:, :], ALU.mult)
        K.dve("tensor_tensor", lo[:, :], lo[:, :], gtmp[:, :], ALU.max)
        if it < NTL:
            load_tile(it)
        if it % 3 == 1:
            next(mgen, None)
    for _ in mgen:
        pass
    modT, gates, sil = mres
    A2, SH2 = mod_cols(K, modT, d["gffnT"], 6, 8, "m2")
    G2 = [gates[(5, 0)], gates[(5, 1)]]
    K.free(sil, modT)
    if not has_ctx:
        K.free(G2[1])
    K.free(affb, capt, mid, cnt, gtmp, mask, junk)

    sel = K.alloc("sel", [128, NTL, 16], F32)
    gate = K.alloc("gate", [128, NTL, 16], F32)
    slot = K.alloc("slot", [128, NTL, 16], F32)
    offs = K.alloc("offs", [128, NTL, 16], F32)
    Lm = K.alloc("Lm", [128, 128], F32)
    K.pool("memset", Lm[:, :], 1.0)
    K.pool("affine_select", Lm[:, :], Lm[:, :], [[1, 128]], ALU.is_gt, 0.0, base=0, channel_multiplier=-1)
    iota = K.alloc("iota", [128, GCAP], F32)
    K.pool("iota", iota[:, :], [[1, GCAP]], base=0, channel_multiplier=0, allow_small_or_imprecise_dtypes=True)
    K.dve("tensor_tensor", sel[:, 0:NT, :], affo[:, 0:NT, :],
          lo.v(lambda a: a[:, 0:16].unsqueeze(1).to_broadcast([128, NT, 16])), ALU.is_ge)
    if has_ctx:
        K.dve("tensor_tensor", sel[:, NT:NTL, :], affo[:, NT:NTL, :],
              lo.v(lambda a: a[:, 16:32].unsqueeze(1).to_broadcast([128, 2, 16])), ALU.is_ge)
    K.dve("tensor_tensor", gate[:, :, :], affo[:, :, :], sel[:, :, :], ALU.mult)
    pb, tb2 = K.banks[1], K.banks[2]
    K.pe("matmul", pb[:, 0:NTL * 16], Lm[:, :], sel.v(lambda a: a.rearrange("p j e -> p (j e)")), start=True, stop=True)
    K.pe("matmul", tb2[:, 0:NTL * 16], C.ones[:, :], sel.v(lambda a: a.rearrange("p j e -> p (j e)")), start=True, stop=True)
    K.dve("memset", offs[:, :, :], 0.0)
    for j in range(NTL):
        first = (j % 4 == 0) if j < NT else (j == NT)
        if first:
            continue
        K.dve("tensor_tensor", offs[:, j, :], offs[:, j - 1, :], tb2[:, (j - 1) * 16:j * 16], ALU.add)
    K.dve("tensor_tensor", slot.v(lambda a: a.rearrange("p j e -> p (j e)")), offs.v(lambda a: a.rearrange("p j e -> p (j e)")),
          pb[:, 0:NTL * 16], ALU.add)
    K.dve("tensor_scalar", slot[:, :, :], slot[:, :, :], 1.0, None, ALU.add)
    K.dve("tensor_tensor", slot[:, :, :], slot[:, :, :], sel[:, :, :], ALU.mult)
    K.dve("tensor_scalar", slot[:, :, :], slot[:, :, :], -1.0, None, ALU.add)
    K.free(sel, offs, Lm, affo, lo)

    S = K.alloc("S", [128, NTL, GCAP], BF16)
    Sg = K.alloc("Sg", [128, NTL, GCAP], BF16)
    ST = K.alloc("ST", [GCAP, NTL, 128], BF16)
    xsT = K.alloc("xsT", [128, 8, NSLOT], BF16)
    actT = K.alloc("actT", [128, 16, NSLOT], BF16)
    silt = [K.alloc("silt%d" % i, [128, NSLOT], F32) for i in range(2)]
    ysb = [K.alloc("ysb%d" % i, [GCAP, D], BF16) for i in range(5 if has_ctx else 4)]
    Wg = [K.alloc("Wg%d" % i, [128, 8, 256], BF16) for i in range(2)]
    Wu = [K.alloc("Wu%d" % i, [128, 8, 256], BF16) for i in range(2)]
    Wd = [K.alloc("Wd%d" % i, [128, 16, 256], BF16) for i in range(2)]
    groups = [(g, list(range(4 * g, 4 * g + 4)), g * GCAP, GCAP, 0) for g in range(4)]
    if has_ctx:
        groups.append((4, [NT, NT + 1], 4 * GCAP, CCAP, 1))
    NS = 4 * GCAP + (CCAP if has_ctx else 0)
    nw = 0
    nd = 0
    st_ = {"nw": 0, "nd": 0}

    def build_S(e):
        K.dve("tensor_tensor", S[:, :, :], iota.v(lambda a: a[:, :].unsqueeze(1).to_broadcast([128, NTL, GCAP])),
              slot.v(lambda a: a[:, :, e:e + 1].to_broadcast([128, NTL, GCAP])), ALU.is_equal)
        K.dve("tensor_tensor", Sg[:, :, :], S[:, :, :],
              gate.v(lambda a: a[:, :, e:e + 1].to_broadcast([128, NTL, GCAP])), ALU.mult)

    def gather(e):
        for dc in range(8):
            xb_ = K.banks[dc % 2]
            for (g, tiles, c0, ncol, col) in groups:
                for n, j in enumerate(tiles):
                    K.pe("matmul", xb_[:, c0:c0 + ncol], H2[j][:, dc * 128:(dc + 1) * 128], S[:, j, 0:ncol],
                         start=(n == 0), stop=(n == len(tiles) - 1))
            K.dve("tensor_scalar", xsT[:, dc, 0:4 * GCAP], xb_[:, 0:4 * GCAP], A2[:, dc, 0:1], SH2[:, dc, 0:1], ALU.mult, ALU.add)
            if has_ctx:
                K.dve("tensor_scalar", xsT[:, dc, 4 * GCAP:NS], xb_[:, 4 * GCAP:NS], A2[:, dc, 1:2], SH2[:, dc, 1:2], ALU.mult, ALU.add)

    def st_transposes(e):
        stb_t = K.banks[6]
        for j0 in range(0, NTL, 8):
            js = list(range(j0, min(j0 + 8, NTL)))
            for n, j in enumerate(js):
                o_ap = stb_t.v(lambda a: a.bitcast(BF16)[0:GCAP, n * 128:(n + 1) * 128])
                K.pe("transpose", o_ap, Sg[:, j, :], C.identb[:, :])
            K.act("activation", ST[:, j0:j0 + len(js), :],
                  stb_t.v(lambda a: a.bitcast(BF16)[0:GCAP, 0:len(js) * 128].rearrange("p (j t) -> p j t", t=128)), AF.Copy)

    def gate_up(e, hook=None):
        for q in range(8):
            wg_, wu_ = Wg[st_["nw"] % 2], Wu[st_["nw"] % 2]
            st_["nw"] += 1
            K.dma("pool", wg_[:, :, :], d["wg"].v(lambda a: a[e].rearrange("(c p) n -> p c n", p=128)[:, :, q * 256:(q + 1) * 256]))
            K.dma("pool", wu_[:, :, :], d["wu"].v(lambda a: a[e].rearrange("(c p) n -> p c n", p=128)[:, :, q * 256:(q + 1) * 256]))
            for f2 in range(2):
                fc = q * 2 + f2
                ab, ub = K.banks[2 + fc % 2], K.banks[4 + fc % 2]
                for dc in range(8):
                    K.pe("matmul", ab[:, 0:NS], wg_[:, dc, f2 * 128:(f2 + 1) * 128], xsT[:, dc, 0:NS], start=(dc == 0), stop=(dc == 7))
                for dc in range(8):
                    K.pe("matmul", ub[:, 0:NS], wu_[:, dc, f2 * 128:(f2 + 1) * 128], xsT[:, dc, 0:NS], start=(dc == 0), stop=(dc == 7))
                sl_ = silt[fc % 2]
                K.act("activation", sl_[:, 0:NS], ab[:, 0:NS], AF.Silu)
                K.dve("tensor_tensor", actT[:, fc, 0:NS], sl_[:, 0:NS], ub[:, 0:NS], ALU.mult)
            if q == 0 and hook is not None:
                hook()

    def down(e):
        for dq in range(4):
            wd_ = Wd[st_["nd"] % 2]
            st_["nd"] += 1
            K.dma("pool", wd_[:, :, :], d["wd"].v(lambda a: a[e].rearrange("(c p) n -> p c n", p=128)[:, :, dq * 256:(dq + 1) * 256]))
            for (g, tiles, c0, ncol, col) in groups:
                yb = K.banks[6 + g % 2]
                for fc in range(16):
                    K.pe("matmul", yb[0:ncol, 0:256], actT[:, fc, c0:c0 + ncol], wd_[:, fc, :], start=(fc == 0), stop=(fc == 15))
                K.dve("tensor_tensor", ysb[g][0:ncol, dq * 256:(dq + 1) * 256], yb[0:ncol, 0:256],
                      G2[col][0:ncol, dq * 256:(dq + 1) * 256], ALU.mult)

    def scatter(e):
        for (g, tiles, c0, ncol, col) in groups:
            for j in tiles:
                for half in range(2):
                    ob = K.banks[2 + (j % 2) * 2 + half]
                    K.pe("matmul", ob[:, :], ST[0:ncol, j, :], ysb[g][0:ncol, half * 512:(half + 1) * 512], start=True, stop=True)
                    K.dve("tensor_tensor", X[j][:, half * 512:(half + 1) * 512], X[j][:, half * 512:(half + 1) * 512], ob[:, :], ALU.add)

    build_S(0)
    gather(0)
    st_transposes(0)
    for e in range(16):
        gate_up(e, hook=(lambda e=e: build_S(e + 1)) if e + 1 < 16 else None)
        if e + 1 < 16:
            gather(e + 1)
        down(e)
        scatter(e)
        if e + 1 < 16:
            st_transposes(e + 1)

    if final:
        fg = K.alloc("fg", [128, D], F32, hi=True)
        K.dma("sp", fg[:, :], d["fg"][:, :])
        junk = K.alloc("junk2", [128, D], F32, hi=True)
    for j in range(NTL):
        dst = o_x2[j * 128:(j + 1) * 128, :] if j < NT else o_c2[(j - NT) * 128:(j - NT + 1) * 128, :]
        if final:
            K.act("activation", junk[:, :], X[j][:, :], AF.Square, accum_out=ss[:, j, 0:1])
            K.act("activation", ss[:, j, 1:2], ss[:, j, 0:1], AF.Sqrt, bias=EPS, scale=1.0 / D)
            K.dve("reciprocal", ss[:, j, 2:3], ss[:, j, 1:2])
            K.act("activation", X[j][:, :], X[j][:, :], AF.Copy, scale=ss[:, j, 2:3])
            K.dve("tensor_tensor", X[j][:, :], X[j][:, :], fg[:, :], ALU.mult)
        K.dma("sp", dst, X[j][:, :], is_out=True)
    K.finish()
    return nc, es


def prep_moe(inp, layer, x1, aff, ctx1=None, affc=None, final=False):
    maps = []
    for core in range(NCORES):
        b, r = core // 4, core % 4
        t0 = r * NOWN
        m = {
            "x1": x1[b, t0:t0 + NOWN], "affb": aff[b], "affo": aff[b, t0:t0 + NOWN],
            "c2": np.stack([fm(inp["c"][b]), fm(inp["c_ctx"])], axis=-1),
            "ada_w": inp["ada_w"][layer], "ada_b": inp["ada_b"][layer][None, :],
            "ada_bT": np.ascontiguousarray(inp["ada_b"][layer].reshape(48, 128).T),
            "gffnT": fm(inp["norm_ffn_g"][layer]),
            "wg": inp["moe_w_gate"][layer], "wu": inp["moe_w_up"][layer], "wd": inp["moe_w_down"][layer],
        }
        if ctx1 is not None:
            m["ctx1"] = ctx1[b]
            m["affc"] = affc[b]
        if final:
            m["fg"] = np.broadcast_to(inp["final_norm_g"][None, :], (128, D))
        maps.append({k: np.ascontiguousarray(v, dtype=np.float32) for k, v in m.items()})
    return maps


LN2 = 0.6931471805599453
STAGE3_DEBUG = False
NKB = 2 + SEQ // 128


def build_stage3():
    nc = bass.Bass("TRN2", target_bir_lowering=False)
    es = contextlib.ExitStack()
    K = KB(nc, es, arena_bytes=206 * 1024)
    d = {}
    for name, shape in [("xb", [SEQ, D]), ("xo", [NOWN, D]), ("xh", [256, D]), ("ctx", [LC, D]), ("c2", [128, 8, 2]),
                        ("ada_w", [D, 6 * D]), ("ada_b", [1, 6 * D]), ("ada_bT", [128, 48]),
                        ("gmixT", [128, 8]), ("gffnT", [128, 8]), ("w_in", [D, 2304]), ("w_out", [D, D]),
                        ("dec", [128, 8]), ("gng", [128, 512]), ("sink", [128, 8]),
                        ("cosb", [SEQ, 32]), ("sinb", [SEQ, 32]), ("coso", [NOWN, 32]), ("sino", [NOWN, 32]),
                        ("cosh", [256, 32]), ("sinh", [256, 32]),
                        ("EF", [128, NKB]), ("EB", [128, NKB]), ("MF", [128, NKB]), ("MB", [128, NKB]),
                        ("wm", [128, 3, 384]), ("rw", [D, 16])]:
        d[name] = K.dram(name, shape)
    o_x3 = K.dram("x3", [NOWN, D], kind="ExternalOutput")
    o_aff = K.dram("aff", [NOWN, 16], kind="ExternalOutput")
    DBG = STAGE3_DEBUG
    if DBG:
        o_cat = K.dram("dbg_cat", [NOWN, D], kind="ExternalOutput")
        o_rf = K.dram("dbg_rf", [64, 1024], kind="ExternalOutput")
        o_z = K.dram("dbg_z", [128, 2 * NKB * 4], kind="ExternalOutput")

    C = setup_consts(K)
    modT, gates, sil = modulation(K, C, d["c2"], d["ada_w"], d["ada_b"], d["ada_bT"],
                                  need_vec=(0, 1, 2, 3, 6, 7, 8, 9), need_gate=(4, 5))
    A1, SH1 = mod_cols(K, modT, d["gmixT"], 0, 2, "m1")
    A2, SH2 = mod_cols(K, modT, d["gffnT"], 6, 8, "m2")
    G1 = gates[(2, 0)]
    K.free(sil, modT, gates[(2, 1)])

    dec = K.alloc("dec", [128, 8], F32)
    K.dma("sp", dec[:, :], d["dec"][:, :])
    lgt = K.alloc("lgt", [128, 8], F32)
    K.act("activation", dec[:, :], dec[:, :], AF.Exp, scale=-LN2)
    K.act("activation", lgt[:, :], dec[:, :], AF.Ln, scale=-1.0, bias=1.0)
    gT = K.alloc("gT", [128, 8], F32)
    K.act("activation", gT[:, :], lgt[:, :], AF.Exp, scale=128.0)
    pcol = K.alloc("pcol", [128, 2], F32)
    K.pool("iota", pcol[:, 0:1], [[1, 1]], base=127, channel_multiplier=-1, allow_small_or_imprecise_dtypes=True)
    K.pool("iota", pcol[:, 1:2], [[1, 1]], base=0, channel_multiplier=1, allow_small_or_imprecise_dtypes=True)
    zloc = K.alloc("zloc", [128, 8], F32)
    for h in range(4):
        K.act("activation", zloc[:, h:h + 1], pcol[:, 0:1], AF.Exp, scale=lgt[:, h:h + 1])
        K.act("activation", zloc[:, 4 + h:5 + h], pcol[:, 1:2], AF.Exp, scale=lgt[:, 4 + h:5 + h])
    K.dve("tensor_scalar", zloc[:, :], zloc[:, :], 0.125, None, ALU.mult)
    diff = K.alloc("diff", [128, 128], F32, hi=True)
    K.pool("iota", diff[:, :], [[1, 128]], base=0, channel_multiplier=-1, allow_small_or_imprecise_dtypes=True)
    dpos = K.alloc("dpos", [128, 128], F32, hi=True)
    dneg = K.alloc("dneg", [128, 128], F32, hi=True)
    K.dve("tensor_scalar", dpos[:, :], diff[:, :], 0.0, None, ALU.max)
    K.dve("tensor_scalar", dneg[:, :], diff[:, :], -1.0, 0.0, ALU.mult, ALU.max)
    DmT = K.alloc("DmT", [128, 4, 128], F32)
    tmpd = K.alloc("tmpd", [128, 128], F32, hi=True)
    for h in range(4):
        K.dve("tensor_scalar", tmpd[:, :], dpos[:, :], lgt[:, h:h + 1], None, ALU.mult)
        K.dve("scalar_tensor_tensor", tmpd[:, :], dneg[:, :], lgt[:, 4 + h:5 + h], tmpd[:, :], ALU.mult, ALU.add)
        K.act("activation", DmT[:, h, :], tmpd[:, :], AF.Exp)
    trow = K.alloc("trow", [128, 2, 128], F32, hi=True)
    K.pool("iota", trow[:, 0, :], [[1, 128]], base=1, channel_multiplier=0, allow_small_or_imprecise_dtypes=True)
    K.pool("iota", trow[:, 1, :], [[-1, 128]], base=128, channel_multiplier=0, allow_small_or_imprecise_dtypes=True)
    Xi = K.alloc("Xi", [64, 2, 4, 128], F32)
    for h in range(4):
        K.act("activation", Xi[:, 0, h, :], trow[0:64, 0, :], AF.Exp, scale=lgt[0:64, h:h + 1])
        K.act("activation", Xi[:, 1, h, :], trow[0:64, 1, :], AF.Exp, scale=lgt[0:64, 4 + h:5 + h])
    K.free(diff, dpos, dneg, tmpd, trow)
    EF = K.alloc("EF", [128, 2, NKB], F32, hi=True)
    MF = K.alloc("MF", [128, 2, NKB], F32, hi=True)
    K.dma("sp", EF[:, 0, :], d["EF"][:, :])
    K.dma("sp", EF[:, 1, :], d["EB"][:, :])
    K.dma("sp", MF[:, 0, :], d["MF"][:, :])
    K.dma("sp", MF[:, 1, :], d["MB"][:, :])
    Z = K.alloc("Z", [128, 2, NKB, 4], F32, hi=True)
    for dr in range(2):
        for h in range(4):
            K.act("activation", Z[:, dr, :, h], EF[:, dr, :], AF.Exp, scale=lgt[:, dr * 4 + h:dr * 4 + h + 1])
        K.dve("scalar_tensor_tensor", Z[:, dr, :, :], Z[:, dr, :, :], 0.125,
              MF.v(lambda a: a[:, dr, :].unsqueeze(2).to_broadcast([128, NKB, 4])), ALU.mult, ALU.mult)
    K.free(EF, MF)
    if DBG:
        K.dma("sp", o_z[:, :], Z.v(lambda a: a.rearrange("p a b c -> p (a b c)")), is_out=True)

    Wkv = K.alloc("Wkv", [128, 8, 1024], BF16, hi=True)
    K.dma("pool", Wkv[:, :, 0:768], d["w_in"].v(lambda a: a.rearrange("(c p) n -> p c n", p=128)[:, :, 256:1024]))
    K.dma("pool", Wkv[:, :, 768:1024], d["w_in"].v(lambda a: a.rearrange("(c p) n -> p c n", p=128)[:, :, 2048:2304]))
    cosb = K.alloc("cosb", [128, SEQ // 128, 32], F32, hi=True)
    sinb = K.alloc("sinb", [128, SEQ // 128, 32], F32, hi=True)
    K.dma("sp", cosb[:, :, :], d["cosb"].v(lambda a: a.rearrange("(j p) n -> p j n", p=128)))
    K.dma("sp", sinb[:, :, :], d["sinb"].v(lambda a: a.rearrange("(j p) n -> p j n", p=128)))
    xt = [K.alloc("xt%d" % i, [128, 1024], F32) for i in range(2)]
    fr = Front(K, C)
    tmp = {"qn": K.alloc("qn", [128, 512], F32, hi=True), "t1": K.alloc("t1", [128, 256], F32, hi=True),
           "t2": K.alloc("t2", [128, 256], F32, hi=True)}
    kr = K.alloc("kr", [128, 256], F32, hi=True)
    kz = [K.alloc("kz%d" % i, [128, 2, 4, 64], BF16, hi=True) for i in range(2)]
    Vt = [K.alloc("Vt%d" % i, [128, 512], BF16, hi=True) for i in range(2)]
    trb = [K.banks[0], K.banks[1]]
    RFp, RBp = K.banks[6], K.banks[7]
    nx = 0
    def p1_front(i):
        nonlocal nx
        x_t = xt[nx % 2]
        nx += 1
        if i < 2:
            K.dma("sp", x_t[:, :], d["ctx"][i * 128:(i + 1) * 128, :])
        else:
            K.dma("sp", x_t[:, :], d["xb"][(i - 2) * 128:(i - 1) * 128, :])
        hT = fr.run(x_t[:, :], A1, SH1, 1 if i < 2 else 0, trb)
        kb, vb = K.banks[2 + i % 2], K.banks[4 + i % 2]
        for dc in range(8):
            K.pe("matmul", kb[:, 0:256], hT[:, dc, :], Wkv[:, dc, 0:256], start=(dc == 0), stop=(dc == 7))
        for dc in range(8):
            K.pe("matmul", vb[:, :], hT[:, dc, :], Wkv[:, dc, 256:768], start=(dc == 0), stop=(dc == 7))

    def p1_back(i):
        kb, vb = K.banks[2 + i % 2], K.banks[4 + i % 2]
        if i < 2:
            head_norm_rope(K, kb[:, 0:256], 4, None, None, None, kr, tmp)
        else:
            head_norm_rope(K, kb[:, 0:256], 4, None, cosb[:, i - 2, :], sinb[:, i - 2, :], kr, tmp)
        kz_, vt_ = kz[i % 2], Vt[i % 2]
        for dr in range(2):
            K.dve("tensor_tensor", kz_[:, dr, :, :], kr.v(lambda a: a[:, :].rearrange("p (h d) -> p h d", d=64)),
                  Z.v(lambda a: a[:, dr, i, :].unsqueeze(2).to_broadcast([128, 4, 64])), ALU.mult)
        K.act("activation", vt_[:, :], vb[:, :], AF.Copy)
        for dr, bank in ((0, RFp), (1, RBp)):
            for h in range(4):
                K.pe("matmul", bank[0:64, h * 128:(h + 1) * 128], kz_[:, dr, h, :], vt_[:, h * 128:(h + 1) * 128],
                     start=(i == 0 and h == 0), stop=(i == NKB - 1 and h == 3))
    p1_front(0)
    for i in range(NKB):
        if i + 1 < NKB:
            p1_front(i + 1)
        p1_back(i)
    Rf = K.alloc("Rf", [64, 512], F32)
    Rb = K.alloc("Rb", [64, 512], F32)
    K.act("activation", Rf[:, :], RFp[0:64, :], AF.Copy)
    K.act("activation", Rb[:, :], RBp[0:64, :], AF.Copy)
    if DBG:
        K.dma("sp", o_rf[:, 0:512], Rf[:, :], is_out=True)
        K.dma("sp", o_rf[:, 512:1024], Rb[:, :], is_out=True)
    K.free(cosb, sinb, Z)

    coso = K.alloc("coso", [128, NT + 2, 32], F32)
    sino = K.alloc("sino", [128, NT + 2, 32], F32)
    K.dma("sp", coso[:, 0:NT, :], d["coso"].v(lambda a: a.rearrange("(j p) n -> p j n", p=128)))
    K.dma("sp", sino[:, 0:NT, :], d["sino"].v(lambda a: a.rearrange("(j p) n -> p j n", p=128)))
    K.dma("sp", coso[:, NT:NT + 2, :], d["cosh"].v(lambda a: a.rearrange("(j p) n -> p j n", p=128)))
    K.dma("sp", sino[:, NT:NT + 2, :], d["sinh"].v(lambda a: a.rearrange("(j p) n -> p j n", p=128)))
    kTr = K.alloc("kTr", [128, NT, 4, 128], BF16)
    K.pool("memset", kTr[64:128, :, :, :], 0.0)
    Vr = K.alloc("Vr", [128, NT, 512], BF16)
    kzb = K.alloc("kzb", [128, NT, 4, 64], BF16)
    Rfb = K.alloc("Rfb", [128, NT, 512], BF16)
    Rbb = K.alloc("Rbb", [128, NT, 512], BF16)
    K.pool("memset", Rfb[64:128, :, :], 0.0)
    K.pool("memset", Rbb[64:128, :, :], 0.0)
    kTd = K.alloc("kTd", [128, 2, 20 * 128], BF16)
    K.pool("memset", kTd[64:128, :, :], 0.0)
    Vd = K.alloc("Vd", [128, 20, 2, 64], BF16)
    kzf = [K.alloc("kzf%d" % i, [128, 4, 64], BF16, hi=True) for i in range(2)]
    krd = K.alloc("krd", [128, 128], F32, hi=True)
    sweep = [("halo", 0), ("halo", 1), ("ctx", 0), ("ctx", 1)] + [("own", n) for n in range(NT)]
    def p2_front(n_, kind, n):
        nonlocal nx
        x_t = xt[nx % 2]
        nx += 1
        src = {"halo": d["xh"], "own": d["xo"], "ctx": d["ctx"]}[kind]
        K.dma("sp", x_t[:, :], src[n * 128:(n + 1) * 128, :])
        hT = fr.run(x_t[:, :], A1, SH1, 1 if kind == "ctx" else 0, trb)
        ab = K.banks[2 if n_ % 2 == 0 else 7]
        if kind == "own":
            for dc in range(8):
                K.pe("matmul", ab[:, 0:256], hT[:, dc, :], Wkv[:, dc, 0:256], start=(dc == 0), stop=(dc == 7))
        for dc in range(8):
            K.pe("matmul", ab[:, 256:512], hT[:, dc, :], Wkv[:, dc, 768:1024], start=(dc == 0), stop=(dc == 7))
        if kind == "own":
            vb = K.banks[4 + n_ % 2]
            for dc in range(8):
                K.pe("matmul", vb[:, :], hT[:, dc, :], Wkv[:, dc, 256:768], start=(dc == 0), stop=(dc == 7))

    def p2_back(n_, kind, n):
        idx = {"halo": 17 * n, "own": 1 + n, "ctx": 18 + n}[kind]
        ci = {"halo": NT + n, "own": n, "ctx": None}[kind]
        ab = K.banks[2 if n_ % 2 == 0 else 7]
        if ci is None:
            head_norm_rope(K, ab[:, 256:384], 2, None, None, None, krd, tmp)
        else:
            head_norm_rope(K, ab[:, 256:384], 2, None, coso[:, ci, :], sino[:, ci, :], krd, tmp)
        K.act("activation", Vd[:, idx, :, :], ab.v(lambda a: a[:, 384:512].rearrange("p (h d) -> p h d", d=64)), AF.Copy)
        tb = K.banks[3]
        for h in range(2):
            K.pe("transpose", tb[0:64, h * 128:(h + 1) * 128], krd[:, h * 64:(h + 1) * 64], C.ident[:, :])
        K.dve("tensor_copy", kTd[0:64, :, idx * 128:(idx + 1) * 128],
              tb.v(lambda a: a[0:64, 0:256].rearrange("p (h t) -> p h t", t=128)))
        if kind != "own":
            return
        vb = K.banks[4 + n_ % 2]
        head_norm_rope(K, ab[:, 0:256], 4, None, coso[:, n, :], sino[:, n, :], kr, tmp)
        K.act("activation", Vr[:, n, :], vb[:, :], AF.Copy)
        kzf_ = kzf[n % 2]
        K.dve("tensor_tensor", kzf_[:, :, :], kr.v(lambda a: a[:, :].rearrange("p (h d) -> p h d", d=64)),
              zloc.v(lambda a: a[:, 0:4].unsqueeze(2).to_broadcast([128, 4, 64])), ALU.mult)
        K.dve("tensor_tensor", kzb[:, n, :, :], kr.v(lambda a: a[:, :].rearrange("p (h d) -> p h d", d=64)),
              zloc.v(lambda a: a[:, 4:8].unsqueeze(2).to_broadcast([128, 4, 64])), ALU.mult)
        tk = K.banks[3]
        for h in range(4):
            K.pe("transpose", tk[0:64, h * 128:(h + 1) * 128], kr[:, h * 64:(h + 1) * 64], C.ident[:, :])
        K.act("activation", kTr[0:64, n, :, :], tk.v(lambda a: a[0:64, :].rearrange("p (h t) -> p h t", t=128)), AF.Copy, scale=0.125)
        K.act("activation", Rfb[0:64, n, :], Rf[:, :], AF.Copy)
        sb_ = K.banks[6]
        for h in range(4):
            K.pe("matmul", sb_[0:64, h * 128:(h + 1) * 128], kzf_[:, h, :], Vr[:, n, h * 128:(h + 1) * 128], start=True, stop=True)
        for h in range(4):
            K.dve("scalar_tensor_tensor", Rf[:, h * 128:(h + 1) * 128], Rf[:, h * 128:(h + 1) * 128], gT[0:64, h:h + 1],
                  sb_[0:64, h * 128:(h + 1) * 128], ALU.mult, ALU.add)
    p2_front(0, *sweep[0])
    for n_, (kind, n) in enumerate(sweep):
        if n_ + 1 < len(sweep):
            p2_front(n_ + 1, *sweep[n_ + 1])
        p2_back(n_, kind, n)
    for n in range(NT - 1, -1, -1):
        K.act("activation", Rbb[0:64, n, :], Rb[:, :], AF.Copy)
        sb_ = K.banks[6 + n % 2]
        for h in range(4):
            K.pe("matmul", sb_[0:64, h * 128:(h + 1) * 128], kzb[:, n, h, :], Vr[:, n, h * 128:(h + 1) * 128], start=True, stop=True)
        for h in range(4):
            K.dve("scalar_tensor_tensor", Rb[:, h * 128:(h + 1) * 128], Rb[:, h * 128:(h + 1) * 128], gT[0:64, 4 + h:5 + h],
                  sb_[0:64, h * 128:(h + 1) * 128], ALU.mult, ALU.add)
    K.free(Wkv, kr, kz[0], kz[1], Vt[0], Vt[1], kzf[0], kzf[1], krd, kzb, Rf, Rb)

    Wq = K.alloc("Wq", [128, 8, 1280], BF16)
    K.dma("pool", Wq[:, :, 0:256], d["w_in"].v(lambda a: a.rearrange("(c p) n -> p c n", p=128)[:, :, 0:256]))
    K.dma("pool", Wq[:, :, 256:1280], d["w_in"].v(lambda a: a.rearrange("(c p) n -> p c n", p=128)[:, :, 1024:2048]))
    Wo = K.alloc("Wo", [128, 8, D], BF16)
    K.dma("pool", Wo[:, :, :], d["w_out"].v(lambda a: a.rearrange("(c p) n -> p c n", p=128)))
    for dc in range(8):
        K.dve("tensor_tensor", Wo[:, dc, :], Wo[:, dc, :], G1[:, :], ALU.mult)
    gng = K.alloc("gng", [128, 512], F32)
    K.dma("sp", gng[:, :], d["gng"][:, :])
    sink = K.alloc("sink", [128, 8], F32)
    K.dma("sp", sink[:, :], d["sink"][:, :])
    wm = K.alloc("wm", [128, 3, 384], F32)
    K.dma("sp", wm[:, :, :], d["wm"][:, :, :])
    rw = K.alloc("rw", [128, 8, 16], F32)
    K.dma("sp", rw[:, :, :], d["rw"].v(lambda a: a.rearrange("(c p) n -> p c n", p=128)))
    fr32 = Front(K, C, out_dt=F32, name="fr32", nbuf=1, share=fr)
    qr = K.alloc("qr", [128, 512], F32)
    qT = K.alloc("qT", [128, 3, 4, 128], BF16)
    K.pool("memset", qT[64:128, :, :, :], 0.0)
    PTr = K.alloc("PTr", [128, 512], BF16)
    cat = K.alloc("cat", [128, D], F32)
    catT = K.alloc("catT", [128, 8, 128], BF16)
    osb = K.alloc("osb", [128, 512], F32)
    sq = K.alloc("sq", [128, 512], F32)
    sg = sq
    st = K.alloc("st", [128, 32], F32)
    qTd = K.alloc("qTd", [128, 8, 128], BF16)
    K.pool("memset", qTd[64:128, :, :], 0.0)
    Pld = [K.alloc("Pl%d" % i, [128, 384], BF16) for i in range(2)]
    Pmd = [K.alloc("Pm%d" % i, [128, 640], BF16) for i in range(2)]
    PTdd = [K.alloc("PTd%d" % i, [128, 5, 128], BF16) for i in range(2)]
    smd = [K.alloc("sm%d" % i, [128, 16], F32) for i in range(2)]
    tg = cat
    affo = K.alloc("affo", [128, NT, 16], F32)
    smt = K.alloc("smt", [128, 24], F32)
    cat2 = [cat, K.alloc("cat_b", [128, D], F32)]
    qTd2 = [qTd, K.alloc("qTd_b", [128, 8, 128], BF16)]
    K.pool("memset", qTd2[1][64:128, :, :], 0.0)
    xsave = {}

    def h1(n):
        nonlocal nx
        cat_, qTd_ = cat2[n % 2], qTd2[n % 2]
        x_t = xt[nx % 2]
        nx += 1
        K.dma("sp", x_t[:, :], d["xo"][n * 128:(n + 1) * 128, :])
        hT = fr.run(x_t[:, :], A1, SH1, 0, trb)
        cqb, cgb, dqb = K.banks[2], K.banks[3], K.banks[4]
        for dc in range(8):
            K.pe("matmul", cqb[:, 0:256], hT[:, dc, :], Wq[:, dc, 0:256], start=(dc == 0), stop=(dc == 7))
        for dc in range(8):
            K.pe("matmul", cgb[:, :], hT[:, dc, :], Wq[:, dc, 256:768], start=(dc == 0), stop=(dc == 7))
        for dc in range(8):
            K.pe("matmul", dqb[:, :], hT[:, dc, :], Wq[:, dc, 768:1280], start=(dc == 0), stop=(dc == 7))
        head_norm_rope(K, cqb[:, 0:256], 4, None, coso[:, n, :], sino[:, n, :], qr, tmp)
        tq = K.banks[5]
        for h in range(4):
            K.pe("transpose", tq[0:64, h * 128:(h + 1) * 128], qr[:, h * 64:(h + 1) * 64], C.ident[:, :])
        K.act("activation", qT[0:64, 0, :, :], tq.v(lambda a: a[0:64, :].rearrange("p (h t) -> p h t", t=128)), AF.Copy)
        for v_ in range(2):
            K.dve("tensor_tensor", qT[0:64, 1 + v_, :, :], tq.v(lambda a: a[0:64, :].rearrange("p (h t) -> p h t", t=128)),
                  Xi[:, v_, :, :], ALU.mult)
        atb = K.banks[6]
        for h in range(4):
            K.pe("matmul", atb[:, h * 128:(h + 1) * 128], kTr[:, n, h, :], qT[:, 0, h, :], start=True, stop=True)
        K.dve("tensor_tensor", PTr[:, :], atb[:, :], DmT.v(lambda a: a.rearrange("p h t -> p (h t)")), ALU.mult)
        ob = K.banks[7]
        for h in range(4):
            hs = slice(h * 128, (h + 1) * 128)
            K.pe("matmul", ob[:, hs], PTr[:, hs], Vr[:, n, hs], start=True, stop=False)
            K.pe("matmul", ob[:, hs], qT[:, 1, h, :], Rfb[:, n, hs], start=False, stop=False)
            K.pe("matmul", ob[:, hs], qT[:, 2, h, :], Rbb[:, n, hs], start=False, stop=True)
        K.act("activation", osb[:, :], ob[:, :], AF.Copy)
        K.act("activation", sq[:, :], ob[:, :], AF.Square)
        K.dve("tensor_reduce", st[:, 0:4], osb.v(lambda a: a.rearrange("p (h v) -> p h v", v=128)), AX.X, ALU.add)
        K.dve("tensor_reduce", st[:, 4:8], sq.v(lambda a: a.rearrange("p (h v) -> p h v", v=128)), AX.X, ALU.add)
        K.dve("tensor_scalar", st[:, 8:12], st[:, 0:4], 1.0 / 128, None, ALU.mult)
        K.dve("tensor_tensor", st[:, 12:16], st[:, 8:12], st[:, 8:12], ALU.mult)
        K.dve("scalar_tensor_tensor", st[:, 16:20], st[:, 4:8], 1.0 / 128, st[:, 12:16], ALU.mult, ALU.subtract)
        K.act("activation", st[:, 20:24], st[:, 16:20], AF.Sqrt, bias=1e-5, scale=1.0)
        K.dve("reciprocal", st[:, 24:28], st[:, 20:24])
        for h in range(4):
            hs = slice(h * 128, (h + 1) * 128)
            K.dve("tensor_scalar", osb[:, hs], osb[:, hs], st[:, 8 + h:9 + h], st[:, 24 + h:25 + h], ALU.subtract, ALU.mult)
        K.act("activation", sg[:, :], cgb[:, :], AF.Silu)
        K.pool("tensor_tensor", osb[:, :], osb[:, :], gng[:, :], ALU.mult)
        K.pool("tensor_tensor", cat_[:, 0:512], osb[:, :], sg[:, :], ALU.mult)
        head_norm_rope(K, dqb[:, :], 8, None, coso[:, n, :], sino[:, n, :], qr, tmp)
        for h in range(8):
            bk = K.banks[5 + h // 4]
            K.pe("transpose", bk[0:64, (h % 4) * 128:(h % 4 + 1) * 128], qr[:, h * 64:(h + 1) * 64], C.ident[:, :])
        for half in range(2):
            bk = K.banks[5 + half]
            K.act("activation", qTd_[0:64, half * 4:half * 4 + 4, :], bk.v(lambda a: a[0:64, :].rearrange("p (h t) -> p h t", t=128)), AF.Copy)
        xsave[n] = x_t

    def h2(n):
        cat_, qTd_ = cat2[n % 2], qTd2[n % 2]
        x_t = xsave[n]
        mi = 0 if n == 0 else (2 if n == NT - 1 else 1)
        odb = K.banks[7]

        def win_a(h):
            kv = h // 4
            sm_, Pl_, Pm_ = smd[h % 2], Pld[h % 2], Pmd[h % 2]
            s1, s2 = K.banks[2 + h % 2], K.banks[4 + h % 2]
            K.pe("matmul", s1[:, 0:384], qTd_[:, h, :], kTd[:, kv, n * 128:(n + 3) * 128], start=True, stop=True)
            K.pe("matmul", s2[:, 0:256], qTd_[:, h, :], kTd[:, kv, 18 * 128:20 * 128], start=True, stop=True)
            K.dve("tensor_reduce", sm_[:, 0:1], s1[:, 0:384], AX.X, ALU.max)
            K.dve("tensor_reduce", sm_[:, 1:2], s2[:, 0:256], AX.X, ALU.max)
            K.dve("tensor_tensor", sm_[:, 2:3], sm_[:, 0:1], sm_[:, 1:2], ALU.max)
            K.dve("scalar_tensor_tensor", sm_[:, 3:4], sm_[:, 2:3], 0.125, sink[:, h:h + 1], ALU.mult, ALU.max)
            K.dve("tensor_scalar", sm_[:, 4:5], sm_[:, 3:4], -1.0, None, ALU.mult)
            K.act("activation", Pl_[:, 0:384], s1[:, 0:384], AF.Exp, bias=sm_[:, 4:5], scale=0.125)
            K.act("activation", Pm_[:, 384:640], s2[:, 0:256], AF.Exp, bias=sm_[:, 4:5], scale=0.125, accum_out=sm_[:, 5:6])
            K.act("activation", sm_[:, 6:7], sink[:, h:h + 1], AF.Exp, bias=sm_[:, 4:5], scale=1.0)
            K.dve("scalar_tensor_tensor", Pm_[:, 0:384], Pl_[:, 0:384], 1.0, wm[:, mi, :], ALU.mult, ALU.mult, accum_out=sm_[:, 7:8])
            K.dve("tensor_tensor", sm_[:, 8:9], sm_[:, 5:6], sm_[:, 6:7], ALU.add)
            K.dve("tensor_tensor", sm_[:, 8:9], sm_[:, 8:9], sm_[:, 7:8], ALU.add)
            K.dve("reciprocal", sm_[:, 9:10], sm_[:, 8:9])

        def win_b(h):
            kv = h // 4
            sm_, Pm_, PTd_ = smd[h % 2], Pmd[h % 2], PTdd[h % 2]
            ptb = K.banks[6]
            for i in range(5):
                K.pe("transpose", ptb.v(lambda a: a.bitcast(BF16)[:, i * 128:(i + 1) * 128]), Pm_[:, i * 128:(i + 1) * 128], C.identb[:, :])
            K.act("activation", PTd_[:, :, :], ptb.v(lambda a: a.bitcast(BF16)[:, 0:640].rearrange("p (i t) -> p i t", t=128)), AF.Copy)
            for i in range(5):
                vidx = (n + i) if i < 3 else (18 + i - 3)
                K.pe("matmul", odb[:, h * 64:(h + 1) * 64], PTd_[:, i, :], Vd[:, vidx, kv, :], start=(i == 0), stop=(i == 4))
            K.dve("tensor_scalar", cat_[:, 512 + h * 64:512 + (h + 1) * 64], odb[:, h * 64:(h + 1) * 64], sm_[:, 9:10], None, ALU.mult)
        win_a(0)
        for h in range(8):
            if h + 1 < 8:
                win_a(h + 1)
            win_b(h)
        if DBG:
            K.dma("sp", o_cat[n * 128:(n + 1) * 128, :], cat_[:, :], is_out=True)
        for dc in range(8):
            bk = trb[dc // 4]
            K.pe("transpose", bk[:, (dc % 4) * 128:(dc % 4 + 1) * 128], cat_[:, dc * 128:(dc + 1) * 128], C.ident[:, :])
        for half in range(2):
            K.act("activation", catT[:, half * 4:half * 4 + 4, :], trb[half].v(lambda a: a[:, :].rearrange("p (c t) -> p c t", t=128)), AF.Copy)
        x3t = cat_
        for half in range(2):
            mb = K.banks[2 + half]
            for dc in range(8):
                K.pe("matmul", mb[:, :], catT[:, dc, :], Wo[:, dc, half * 512:(half + 1) * 512], start=(dc == 0), stop=(dc == 7))
            K.dve("tensor_tensor", cat_[:, half * 512:(half + 1) * 512], mb[:, :], x_t[:, half * 512:(half + 1) * 512], ALU.add)
        K.dma("sp", o_x3[n * 128:(n + 1) * 128, :], x3t[:, :], is_out=True)
        hT32 = fr32.run(x3t[:, :], A2, SH2, 0, trb)
        lb = K.banks[4]
        for dc in range(8):
            K.pe("matmul", lb[:, 0:16], hT32[:, dc, :], rw[:, dc, :], start=(dc == 0), stop=(dc == 7))
        softmax16(K, lb[:, 0:16], affo[:, n, :], smt)
    h1(0)
    for n in range(NT):
        if n + 1 < NT:
            h1(n + 1)
        h2(n)
    K.dma("sp", o_aff.v(lambda a: a.rearrange("(j p) e -> p j e", p=128)), affo[:, :, :], is_out=True)
    K.finish()
    return nc, es


def prep_stage3(inp, x2, ctx2):
    cos, sin = rope_tables()
    p = np.arange(128)
    tq, tk = p[:, None], p[None, :]
    prev_std = (tq <= tk).astype(np.float32)
    next_std = (tk <= tq).astype(np.float32)
    ones = np.ones((128, 128), np.float32)
    zeros = np.zeros((128, 128), np.float32)
    maps = []
    for core in range(NCORES):
        b, r = core // 4, core % 4
        t0, t1 = r * NOWN, (r + 1) * NOWN
        x = x2[b]
        xh = np.zeros((256, D), np.float32)
        ch = np.zeros((256, 32), np.float32)
        sh = np.zeros((256, 32), np.float32)
        if r > 0:
            xh[0:128] = x[t0 - 128:t0]
            ch[0:128], sh[0:128] = cos[t0 - 128:t0], sin[t0 - 128:t0]
        if r < 3:
            xh[128:256] = x[t1:t1 + 128]
            ch[128:256], sh[128:256] = cos[t1:t1 + 128], sin[t1:t1 + 128]
        EF = np.zeros((128, NKB), np.float32)
        EB = np.zeros((128, NKB), np.float32)
        MF = np.zeros((128, NKB), np.float32)
        MB = np.zeros((128, NKB), np.float32)
        for i in range(NKB):
            if i < 2:
                m = i * 128 + p
                EF[:, i] = t0 - 1 + LC - m
                MF[:, i] = 1.0
                EB[:, i] = SEQ + m - t1
                MB[:, i] = 1.0
            else:
                pos = (i - 2) * 128 + p
                if pos[0] < t0:
                    EF[:, i] = t0 - 1 - pos
                    MF[:, i] = 1.0
                elif pos[0] >= t1:
                    EB[:, i] = pos - t1
                    MB[:, i] = 1.0
        wm = np.stack([np.concatenate([prev_std if r > 0 else zeros, ones, next_std], axis=1),
                       np.concatenate([prev_std, ones, next_std], axis=1),
                       np.concatenate([prev_std, ones, next_std if r < 3 else zeros], axis=1)], axis=1)
        m = {
            "xb": x, "xo": x[t0:t1], "xh": xh, "ctx": ctx2[b],
            "c2": np.stack([fm(inp["c"][b]), fm(inp["c_ctx"])], axis=-1),
            "ada_w": inp["ada_w"][1], "ada_b": inp["ada_b"][1][None, :],
            "ada_bT": np.ascontiguousarray(inp["ada_b"][1].reshape(48, 128).T),
            "gmixT": fm(inp["norm_mix_g"][1]), "gffnT": fm(inp["norm_ffn_g"][1]),
            "w_in": inp["cd_w_in"][0], "w_out": inp["cd_w_out"][0],
            "dec": np.broadcast_to(np.concatenate([inp["c_decay_fwd"][0], inp["c_decay_bwd"][0]])[None, :], (128, 8)),
            "gng": np.broadcast_to(inp["c_norm_g"][0][None, :], (128, 512)),
            "sink": np.broadcast_to(inp["d_sink"][0][None, :], (128, 8)),
            "cosb": cos, "sinb": sin, "coso": cos[t0:t1], "sino": sin[t0:t1], "cosh": ch, "sinh": sh,
            "EF": EF, "EB": EB, "MF": MF, "MB": MB, "wm": wm, "rw": inp["moe_router"][1],
        }
        maps.append({k: np.ascontiguousarray(v, dtype=np.float32) for k, v in m.items()})
    return maps


def _run(builder, maps):
    nc, es = builder()
    es.close()
    res = run_bass_kernel_spmd(nc, maps, core_ids=list(range(NCORES)))
    return res.results


def _gather(results, key):
    return np.stack([np.concatenate([np.asarray(results[b * 4 + r][key]) for r in range(4)], axis=0) for b in range(2)])


def kernel(**inputs):
    inp = {k: np.asarray(v) for k, v in inputs.items()}
    r1 = _run(build_stage1, prep_stage1(inp))
    x1, aff0 = _gather(r1, "x1"), _gather(r1, "aff")
    ctx1 = np.stack([np.asarray(r1[0]["ctx1"]), np.asarray(r1[4]["ctx1"])])
    affc = np.stack([np.asarray(r1[0]["affc"]), np.asarray(r1[4]["affc"])])
    r2 = _run(lambda: build_moe(True, False), prep_moe(inp, 0, x1, aff0, ctx1, affc))
    x2 = _gather(r2, "x2")
    ctx2 = np.stack([np.asarray(r2[0]["ctx2"]), np.asarray(r2[4]["ctx2"])])
    r3 = _run(build_stage3, prep_stage3(inp, x2, ctx2))
    x3, aff1 = _gather(r3, "x3"), _gather(r3, "aff")
    r4 = _run(lambda: build_moe(False, True), prep_moe(inp, 1, x3, aff1, final=True))
    return _gather(r4, "x2").astype(np.float32)
```
